# Optimizing a Trainium2 kernel written in Bass

```python
import math
import jax, jax.numpy as jnp
from jax import lax
import numpy as np

D_MODEL = 2048
BATCH = 2
SEQ = 4096
DEPTH = 1

HEAD_DIM = 128
MIX_WIDTH = D_MODEL
MOBA_WIDTH = MIX_WIDTH // 2
MOBA_HEADS = MOBA_WIDTH // HEAD_DIM
MOBA_BLOCK = 256
MOBA_TOPK = 3
MOBA_QCHUNK = 32
DIFF_WIDTH = MIX_WIDTH - MOBA_WIDTH
DIFF_V_DIM = 2 * HEAD_DIM
DIFF_HEADS = DIFF_WIDTH // DIFF_V_DIM
DIFF_QK_DIM = HEAD_DIM
DIFF_QK_WIDTH = DIFF_HEADS * 2 * DIFF_QK_DIM
DIFF_QBLOCK = 128
IN_SIZES = (MOBA_WIDTH, MOBA_WIDTH, MOBA_WIDTH, DIFF_QK_WIDTH, DIFF_QK_WIDTH, DIFF_WIDTH)
IN_WIDTH = sum(IN_SIZES)
N_GROUPS = 4
EXPERTS_PER_GROUP = 8
N_EXPERTS = N_GROUPS * EXPERTS_PER_GROUP
EXPERT_TOPK = 2
D_EXPERT = D_MODEL // 4
RMS_EPS = 1e-6
ALIBI_MAX_BIAS = 8.0

kernel_name = "hymba_moba_diffattn_hmoe"


def rms_norm(x, w):
    xf = x.astype(jnp.float32)
    y = xf * lax.rsqrt(jnp.mean(xf * xf, axis=-1, keepdims=True) + RMS_EPS)
    return (y * w.astype(jnp.float32)).astype(x.dtype)


def alibi_slopes(n):
    i = jnp.arange(n, dtype=jnp.float32) + 1.0
    return jnp.exp2(-ALIBI_MAX_BIAS * i / n)


def to_heads(t, n, d):
    b, s, _ = t.shape
    return t.reshape(b, s, n, d).transpose(0, 2, 1, 3)


def moba_attention(q, k, v, slopes):
    B, H, S, Dh = q.shape
    nb = -(-S // MOBA_BLOCK)
    sp = nb * MOBA_BLOCK
    n_top = min(MOBA_TOPK, nb)
    pad = ((0, 0), (0, 0), (0, sp - S), (0, 0))
    kf = jnp.pad(k.astype(jnp.float32), pad)
    vf = jnp.pad(v.astype(jnp.float32), pad)
    kb = kf.reshape(B, H, nb, MOBA_BLOCK, Dh)
    vb = vf.reshape(B, H, nb, MOBA_BLOCK, Dh)
    k_mean = jnp.mean(kb, axis=3)
    qf = q.astype(jnp.float32) * (Dh ** -0.5)
    b_idx = jnp.arange(B)[:, None, None, None]
    h_idx = jnp.arange(H)[None, :, None, None]
    blk = jnp.arange(nb)
    in_blk = jnp.arange(MOBA_BLOCK)
    n_chunks = S // MOBA_QCHUNK

    def chunk(c):
        t0 = c * MOBA_QCHUNK
        qc = lax.dynamic_slice_in_dim(qf, t0, MOBA_QCHUNK, axis=2)
        t = t0 + jnp.arange(MOBA_QCHUNK)
        own = t0 // MOBA_BLOCK
        gate = jnp.einsum('bhqd,bhnd->bhqn', qc, k_mean)
        gate = jnp.where(blk < own, gate, -jnp.inf)
        _, sel = lax.top_k(gate, n_top)
        sel_valid = sel < own
        k_sel = kb[b_idx, h_idx, sel]
        v_sel = vb[b_idx, h_idx, sel]
        pos_sel = sel[..., None] * MOBA_BLOCK + in_blk
        dist_sel = (t[None, None, :, None, None] - pos_sel).astype(jnp.float32)
        s_sel = jnp.einsum('bhqd,bhqjkd->bhqjk', qc, k_sel) - slopes[None, :, None, None, None] * dist_sel
        s_sel = jnp.where(sel_valid[..., None], s_sel, -jnp.inf)
        k_own = lax.dynamic_index_in_dim(kb, own, axis=2, keepdims=False)
        v_own = lax.dynamic_index_in_dim(vb, own, axis=2, keepdims=False)
        pos_own = own * MOBA_BLOCK + in_blk
        dist_own = (t[:, None] - pos_own[None, :]).astype(jnp.float32)
        s_own = jnp.einsum('bhqd,bhkd->bhqk', qc, k_own) - slopes[None, :, None, None] * dist_own
        s_own = jnp.where(pos_own[None, :] <= t[:, None], s_own, -jnp.inf)
        n_sel = n_top * MOBA_BLOCK
        s_all = jnp.concatenate([s_sel.reshape(B, H, MOBA_QCHUNK, n_sel), s_own], axis=-1)
        p = jax.nn.softmax(s_all, axis=-1)
        p_sel = p[..., :n_sel].reshape(B, H, MOBA_QCHUNK, n_top, MOBA_BLOCK)
        p_own = p[..., n_sel:]
        out = (jnp.einsum('bhqjk,bhqjkd->bhqd', p_sel, v_sel)
               + jnp.einsum('bhqk,bhkd->bhqd', p_own, v_own))
        return out.astype(q.dtype)

    outs = lax.map(chunk, jnp.arange(n_chunks))
    return outs.transpose(1, 2, 0, 3, 4).reshape(B, H, S, Dh)


def diff_attention(q1, q2, k1, k2, v, lam, slopes):
    B, H, S, Dq = q1.shape
    scale = Dq ** -0.5
    q1f = q1.astype(jnp.float32) * scale
    q2f = q2.astype(jnp.float32) * scale
    k1f, k2f, vf = k1.astype(jnp.float32), k2.astype(jnp.float32), v.astype(jnp.float32)
    s_pos = jnp.arange(S)
    n_blocks = S // DIFF_QBLOCK

    def block(c):
        t0 = c * DIFF_QBLOCK
        t = t0 + jnp.arange(DIFF_QBLOCK)
        causal = s_pos[None, :] <= t[:, None]
        bias = -slopes[:, None, None] * (t[:, None] - s_pos[None, :]).astype(jnp.float32)

        def probs(qf, kf):
            qc = lax.dynamic_slice_in_dim(qf, t0, DIFF_QBLOCK, axis=2)
            s = jnp.einsum('bhqd,bhkd->bhqk', qc, kf) + bias[None]
            return jax.nn.softmax(jnp.where(causal, s, -jnp.inf), axis=-1)

        a = probs(q1f, k1f) - lam * probs(q2f, k2f)
        return jnp.einsum('bhqk,bhkd->bhqd', a, vf)

    outs = lax.map(block, jnp.arange(n_blocks))
    return outs.transpose(1, 2, 0, 3, 4).reshape(B, H, S, vf.shape[-1])


def hier_moe(h, w_rg, b_rg, w_re, b_re, w_gate, w_up, w_down):
    B, S, D = h.shape
    t = h.reshape(B * S, D)
    tf = t.astype(jnp.float32)
    g_probs = jax.nn.softmax(tf @ w_rg.astype(jnp.float32) + b_rg.astype(jnp.float32), axis=-1)
    g_p, g_top = lax.top_k(g_probs, 1)
    e_logits = (tf @ w_re.astype(jnp.float32) + b_re.astype(jnp.float32)).reshape(-1, N_GROUPS, EXPERTS_PER_GROUP)
    e_in = jnp.take_along_axis(e_logits, g_top[:, :, None], axis=1)[:, 0]
    e_p, e_top = lax.top_k(jax.nn.softmax(e_in, axis=-1), EXPERT_TOPK)
    e_p = e_p / jnp.sum(e_p, axis=-1, keepdims=True)
    weights = g_p * e_p
    expert_ids = g_top * EXPERTS_PER_GROUP + e_top
    combine = jnp.sum(jax.nn.one_hot(expert_ids, N_EXPERTS, dtype=jnp.float32) * weights[..., None], axis=1)
    combine = combine.astype(t.dtype)
    out = jnp.zeros_like(t)
    for g in range(N_GROUPS):
        sl = slice(g * EXPERTS_PER_GROUP, (g + 1) * EXPERTS_PER_GROUP)
        a = jnp.einsum('td,edf->tef', t, w_gate[sl])
        u = jnp.einsum('td,edf->tef', t, w_up[sl])
        act = jax.nn.silu(a) * u * combine[:, sl, None]
        out = out + jnp.einsum('tef,efd->td', act, w_down[sl])
    return out.reshape(B, S, D)


def setup_inputs(seed: int = 0) -> dict:
    key = jax.random.key(seed)
    ks = jax.random.split(key, 20)
    f32 = jnp.float32
    nrm = lambda k, shape, s: jax.random.normal(k, shape, f32) * s
    L = DEPTH
    return {
        "x": jax.random.normal(ks[0], (BATCH, SEQ, D_MODEL), f32),
        "norm_mix_w": 1.0 + nrm(ks[1], (L, D_MODEL), 0.02),
        "w_in": nrm(ks[2], (L, D_MODEL, IN_WIDTH), D_MODEL ** -0.5),
        "lambda_q1": nrm(ks[3], (L, DIFF_QK_DIM), 0.1),
        "lambda_k1": nrm(ks[4], (L, DIFF_QK_DIM), 0.1),
        "lambda_q2": nrm(ks[5], (L, DIFF_QK_DIM), 0.1),
        "lambda_k2": nrm(ks[6], (L, DIFF_QK_DIM), 0.1),
        "diff_subln_w": 1.0 + nrm(ks[7], (L, DIFF_V_DIM), 0.02),
        "w_out": nrm(ks[8], (L, MIX_WIDTH, D_MODEL), MIX_WIDTH ** -0.5),
        "norm_ffn_w": 1.0 + nrm(ks[9], (L, D_MODEL), 0.02),
        "w_router_group": nrm(ks[10], (L, D_MODEL, N_GROUPS), D_MODEL ** -0.5),
        "b_router_group": nrm(ks[11], (L, N_GROUPS), 0.01),
        "w_router_expert": nrm(ks[12], (L, D_MODEL, N_EXPERTS), D_MODEL ** -0.5),
        "b_router_expert": nrm(ks[13], (L, N_EXPERTS), 0.01),
        "w_gate": nrm(ks[14], (L, N_EXPERTS, D_MODEL, D_EXPERT), D_MODEL ** -0.5),
        "w_up": nrm(ks[15], (L, N_EXPERTS, D_MODEL, D_EXPERT), D_MODEL ** -0.5),
        "w_down": nrm(ks[16], (L, N_EXPERTS, D_EXPERT, D_MODEL), D_EXPERT ** -0.5),
        "norm_final_w": 1.0 + nrm(ks[17], (D_MODEL,), 0.02),
    }


def reference(x, norm_mix_w, w_in, lambda_q1, lambda_k1, lambda_q2, lambda_k2, diff_subln_w,
              w_out, norm_ffn_w, w_router_group, b_router_group, w_router_expert, b_router_expert,
              w_gate, w_up, w_down, norm_final_w):
    B, S, _ = x.shape
    slopes_a = alibi_slopes(MOBA_HEADS)
    slopes_b = alibi_slopes(DIFF_HEADS)
    offsets = [int(o) for o in np.cumsum(IN_SIZES)[:-1]]
    for l in range(DEPTH):
        h = rms_norm(x, norm_mix_w[l])
        proj = h @ w_in[l]
        qa, ka, va, qb, kb, vb = jnp.split(proj, offsets, axis=-1)
        out_a = moba_attention(to_heads(qa, MOBA_HEADS, HEAD_DIM), to_heads(ka, MOBA_HEADS, HEAD_DIM),
                               to_heads(va, MOBA_HEADS, HEAD_DIM), slopes_a)
        out_a = out_a.transpose(0, 2, 1, 3).reshape(B, S, MOBA_WIDTH).astype(x.dtype)
        qb = qb.reshape(B, S, DIFF_HEADS, 2, DIFF_QK_DIM).transpose(0, 2, 3, 1, 4)
        kb = kb.reshape(B, S, DIFF_HEADS, 2, DIFF_QK_DIM).transpose(0, 2, 3, 1, 4)
        vbh = to_heads(vb, DIFF_HEADS, DIFF_V_DIM)
        lambda_init = 0.8 - 0.6 * math.exp(-0.3 * l)
        lam = (jnp.exp(jnp.sum(lambda_q1[l].astype(jnp.float32) * lambda_k1[l].astype(jnp.float32)))
               - jnp.exp(jnp.sum(lambda_q2[l].astype(jnp.float32) * lambda_k2[l].astype(jnp.float32)))
               + lambda_init)
        out_b = diff_attention(qb[:, :, 0], qb[:, :, 1], kb[:, :, 0], kb[:, :, 1], vbh, lam, slopes_b)
        out_b = rms_norm(out_b, diff_subln_w[l]) * (1.0 - lambda_init)
        out_b = out_b.transpose(0, 2, 1, 3).reshape(B, S, DIFF_WIDTH).astype(x.dtype)
        mixed = jnp.concatenate([out_a, out_b], axis=-1)
        x = x + mixed @ w_out[l]
        h = rms_norm(x, norm_ffn_w[l])
        x = x + hier_moe(h, w_router_group[l], b_router_group[l], w_router_expert[l], b_router_expert[l],
                         w_gate[l], w_up[l], w_down[l])
    return rms_norm(x, norm_final_w)
```

```python
import numpy as np
import ml_dtypes
from contextlib import ExitStack
import concourse.bass as bass
import concourse.mybir as mybir
from concourse.bass_utils import run_bass_kernel_spmd

F32 = mybir.dt.float32
BF16 = mybir.dt.bfloat16
AF = mybir.ActivationFunctionType
ALU = mybir.AluOpType
AX = mybir.AxisListType

D = 2048
S = 4096
NT = 32
EPS = 1e-6
NEG = -30000.0
N_EXP = 32
DE = 512
SEM_BLK = 2000


class Buf:
    __slots__ = ("name", "last_w", "readers", "dma_sem", "dma_cnt")

    def __init__(self, name):
        self.name = name
        self.last_w = None
        self.readers = {}
        self.dma_sem = None
        self.dma_cnt = 0


class Op:
    __slots__ = ("eng", "fn", "deps", "kind", "buf", "dma_val", "sig", "semidx", "idx")


class Prog:
    def __init__(self, nc):
        self.nc = nc
        self.ops = []
        self.engs = {"pe": nc.tensor, "act": nc.scalar, "dve": nc.vector, "pool": nc.gpsimd, "sp": nc.sync}

    def add(self, eng, fn, reads=(), writes=(), kind="c", buf=None):
        op = Op()
        op.eng, op.fn, op.kind, op.buf = eng, fn, kind, buf
        op.sig = False
        op.semidx = 0
        op.idx = len(self.ops)
        deps = set()
        for b in reads:
            if b.last_w is not None:
                deps.add(b.last_w)
        for b in writes:
            if b.last_w is not None:
                deps.add(b.last_w)
            for r in b.readers.values():
                if isinstance(r, list):
                    deps.update(r)
                else:
                    deps.add(r)
        deps.discard(op.idx)
        op.deps = deps
        key = eng if kind == "c" else "dma"
        for b in reads:
            if key == "dma":
                b.readers.setdefault("dma", []).append(op.idx)
            else:
                b.readers[key] = op.idx
        for b in writes:
            b.last_w = op.idx
            b.readers = {}
        if kind == "d":
            buf.dma_cnt += 1
            op.dma_val = 16 * buf.dma_cnt
        elif kind == "cc":
            buf.dma_cnt += 1
            op.dma_val = buf.dma_cnt
        self.ops.append(op)
        return op

    def dma(self, eng, out, in_, reads=(), writes=(), buf=None):
        return self.add(eng, lambda e: e.dma_start(out=out, in_=in_), reads, writes, kind="d", buf=buf)

    def emit(self, final_bufs):
        nc = self.nc
        ops = self.ops
        for op in ops:
            for d in op.deps:
                p = ops[d]
                if p.kind == "c":
                    if p.eng == op.eng and op.kind == "c" and op.eng == "pe":
                        continue
                    p.sig = True
        cnt = {e: 0 for e in self.engs}
        for op in ops:
            if op.kind == "c" and op.sig:
                cnt[op.eng] += 1
                op.semidx = cnt[op.eng]
        sems = {}

        def eng_sem(e, idx):
            blk = (idx - 1) // SEM_BLK
            k = (e, blk)
            if k not in sems:
                sems[k] = nc.alloc_semaphore(f"s_{e}_{blk}")
            return sems[k], (idx - 1) % SEM_BLK + 1

        def buf_sem(b):
            if b.dma_sem is None:
                b.dma_sem = nc.alloc_semaphore(f"d_{b.name}")
            return b.dma_sem

        waited_eng = {e: {p: 0 for p in self.engs} for e in self.engs}
        waited_buf = {e: {} for e in self.engs}
        for op in ops:
            e = self.engs[op.eng]
            need_eng = {}
            need_buf = {}
            for d in op.deps:
                p = ops[d]
                if p.kind == "c":
                    if p.eng == op.eng and op.kind == "c" and op.eng == "pe":
                        continue
                    if p.semidx > need_eng.get(p.eng, 0):
                        need_eng[p.eng] = p.semidx
                else:
                    if p.dma_val > need_buf.get(id(p.buf), (None, 0))[1]:
                        need_buf[id(p.buf)] = (p.buf, p.dma_val)
            for pe_, idx in need_eng.items():
                if waited_eng[op.eng][pe_] < idx:
                    s, v = eng_sem(pe_, idx)
                    e.wait_ge(s, v)
                    waited_eng[op.eng][pe_] = idx
            for bid, (b, v) in need_buf.items():
                if waited_buf[op.eng].get(bid, 0) < v:
                    e.wait_ge(buf_sem(b), v)
                    waited_buf[op.eng][bid] = v
            ins = op.fn(e)
            if op.kind == "c":
                if op.sig:
                    s, v = eng_sem(op.eng, op.semidx)
                    ins.then_inc(s, 1)
            elif op.kind == "d":
                ins.then_inc(buf_sem(op.buf), 16)
            else:
                ins.then_inc(buf_sem(op.buf))
        sp = nc.sync
        for b in final_bufs:
            sp.wait_ge(buf_sem(b), 16 * b.dma_cnt)


class T:
    def __init__(self, ap, name, nbuf=1):
        self.ap = ap
        self.b = Buf(name)
        self.bs = [Buf(f"{name}_{i}") for i in range(nbuf)] if nbuf > 1 else [self.b]


def build(stage=99, dbg=False):
    nc = bass.Bass("TRN2", target_bir_lowering=False)
    P = Prog(nc)
    def din(name, shape, dt=F32):
        return nc.dram_tensor(name, shape, dt, kind="ExternalInput").ap()

    xb = din("xb", [S, D])
    xres = din("xres", [1024, D])
    w_in_c = din("w_in_c", [D, 1536])
    w_out_p = din("w_out_p", [D, D])
    w_r = din("w_r", [D, 36])
    if stage == 99:
        w_gate = din("w_gate", [N_EXP, D, DE])
        w_up = din("w_up", [N_EXP, D, DE])
        w_down = din("w_down", [N_EXP, DE, D])
    vecs = din("vecs", [128, 16 + 16 + 96])
    lamv = din("lamv", [128, 512])
    subw = din("subw", [128, 256])
    rbias = din("rbias", [128, 36])
    wfin = din("wfin", [128, D])
    pastm = din("pastm", [128, 512])
    cbf = din("cbf", [128, 256 + 512], BF16)
    lrb_d = din("lrb", [128, 2 * 16 * 128], BF16)
    rbt_d = din("rbt", [2, S], BF16)
    out = nc.dram_tensor("out", [1024, D], F32, kind="ExternalOutput").ap()
    bounce = [nc.dram_tensor(f"bounce{j}", [1024, 512], BF16) for j in range(4)]
    agbig = nc.dram_tensor("agbig", [4 * 4096, 512], BF16)
    idxg_d = nc.dram_tensor("idxg", [128, 32], mybir.dt.int32, kind="ExternalInput").ap()
    bounce_b = [Buf(f"bounce{j}") for j in range(4)]
    agout_b = [Buf(f"agout{j}") for j in range(4)]
    out_b = Buf("outd")
    dbg_outs = {}

    PA = [T(nc.alloc_psum_tensor(f"pa{i}", [128, 512], F32).ap(), f"pa{i}") for i in range(8)]
    PT = []
    for i in range(2):
        v = T(PA[6 + i].ap.bitcast(BF16), f"ptv{i}")
        v.b = PA[6 + i].b
        v.bs = [v.b]
        PT.append(v)

    es_all = ExitStack()

    def sb(es, name, shape, dt, nbuf=1):
        h = es.enter_context(nc.sbuf_tensor("sb_" + name, shape, dt))
        return T(h.ap() if hasattr(h, "ap") and callable(h.ap) else h, name, nbuf)

    vecs_t = sb(es_all, "vecs", [128, 128], F32)
    cbf_t = sb(es_all, "cbf", [128, 768], BF16)
    P.dma("sp", vecs_t.ap, vecs, writes=[vecs_t.b], buf=vecs_t.b)
    P.dma("sp", cbf_t.ap, cbf, writes=[cbf_t.b], buf=cbf_t.b)
    epsT = sb(es_all, "epsT", [128, 1], F32)
    bar_t = sb(es_all, "bar_t", [128, 8], F32)
    P.add("dve", lambda e: e.memset(epsT.ap, EPS), writes=[epsT.b])
    wmix = vecs_t.ap[:, 0:16]
    wffn = vecs_t.ap[:, 16:32]
    ident = cbf_t.ap[:, 0:128]
    tri = cbf_t.ap[:, 128:256]
    CONST = [vecs_t.b, cbf_t.b]

    es1 = ExitStack()
    QT = [sb(es1, f"qt{i}", [128, S], BF16, nbuf=8) for i in range(4)]
    KT = [sb(es1, f"kt{i}", [128, S], BF16, nbuf=8) for i in range(4)]
    VA = [sb(es1, f"va{i}", [128, NT, 129], BF16, nbuf=8) for i in range(2)]
    VD = sb(es1, "vd", [128, NT, 257], BF16, nbuf=8)
    ksum = sb(es1, "ksum", [128, 2, 16], F32)
    ss1 = sb(es1, "ss1", [128, NT], F32, nbuf=NT)
    rstd1 = sb(es1, "rstd1", [128, NT], F32, nbuf=NT)

    P.add("dve", lambda e: e.memset(ksum.ap, 0.0), writes=[ksum.b])
    P.add("pool", lambda e: e.memset(VA[0].ap[:, :, 128:129], 1.0), writes=VA[0].bs)
    P.add("pool", lambda e: e.memset(VA[1].ap[:, :, 128:129], 1.0), writes=VA[1].bs)
    P.add("pool", lambda e: e.memset(VD.ap[:, :, 256:257], 1.0), writes=VD.bs)

    es1a = ExitStack()
    w_in_sb = sb(es1a, "w_in_sb", [128, 16, 1536], BF16, nbuf=9)
    xts = [sb(es1a, f"xt{i}", [128, D], F32) for i in range(2)]
    xns = [sb(es1a, f"xn{i}", [128, D], BF16) for i in range(2)]
    hTs = [sb(es1a, f"hT{i}", [128, 16, 512], BF16) for i in range(2)]

    w_in_v = w_in_c.rearrange("(k p) c -> p k c", p=128)
    for c in range(8):
        P.dma("pool", w_in_sb.ap[:, :, c * 128:(c + 1) * 128], w_in_v[:, :, c * 128:(c + 1) * 128],
              writes=[w_in_sb.bs[c]], buf=w_in_sb.bs[c])
    P.dma("pool", w_in_sb.ap[:, :, 1024:1536], w_in_v[:, :, 1024:1536], writes=[w_in_sb.bs[8]], buf=w_in_sb.bs[8])

    QSCALE = 128.0 ** -0.5
    pa_ctr = [0]

    def norm_T(g, tt):
        hT = hTs[g % 2]
        t = g * 4 + tt
        xt = xts[t % 2]
        xn = xns[t % 2]
        P.dma("sp", xt.ap, xb[t * 128:(t + 1) * 128, :], writes=[xt.b], buf=xt.b)
        P.add("act", lambda e: e.activation(out=xn.ap, in_=xt.ap, func=AF.Square, accum_out=ss1.ap[:, t:t + 1]),
              reads=[xt.b], writes=[xn.b, ss1.bs[t]])
        P.add("act", lambda e: e.activation(out=rstd1.ap[:, t:t + 1], in_=ss1.ap[:, t:t + 1], func=AF.Sqrt,
                                            scale=1.0 / D, bias=epsT.ap),
              reads=[ss1.bs[t], epsT.b], writes=[rstd1.bs[t]])
        P.add("dve", lambda e: e.reciprocal(out=rstd1.ap[:, t:t + 1], in_=rstd1.ap[:, t:t + 1]),
              reads=[rstd1.bs[t]], writes=[rstd1.bs[t]])
        P.add("dve", lambda e: e.tensor_scalar(out=xn.ap, in0=xt.ap, scalar1=rstd1.ap[:, t:t + 1], scalar2=None,
                                               op0=ALU.mult), reads=[xt.b, rstd1.bs[t]], writes=[xn.b])

    def norm_pe(g, tt):
        hT = hTs[g % 2]
        t = g * 4 + tt
        xn = xns[t % 2]
        for r in range(2):
            pt = PT[r]
            for k in range(8):
                P.add("pe", lambda e, pt=pt, k=k, r=r: e.transpose(
                    out=pt.ap[:, k * 128:(k + 1) * 128], in_=xn.ap[:, (r * 8 + k) * 128:(r * 8 + k + 1) * 128],
                    identity=ident), reads=[xn.b] + CONST, writes=[pt.b])
            P.add("dve", lambda e, pt=pt, r=r: e.tensor_tensor(
                out=hT.ap[:, r * 8:(r + 1) * 8, tt * 128:(tt + 1) * 128],
                in0=pt.ap.rearrange("p (k n) -> p k n", k=8),
                in1=wmix[:, r * 8:(r + 1) * 8].unsqueeze(2).to_broadcast([128, 8, 128]),
                op=ALU.mult), reads=[pt.b] + CONST, writes=[hT.b])

    def chunk_T(g, c):
        hT = hTs[g % 2]
        pa = PA[pa_ctr[0] % 2]
        pa_ctr[0] += 1
        for k in range(16):
            P.add("pe", lambda e, k=k: e.matmul(
                pa.ap, lhsT=w_in_sb.ap[:, k, c * 128:(c + 1) * 128], rhs=hT.ap[:, k, :],
                start=(k == 0), stop=(k == 15)), reads=[hT.b, w_in_sb.bs[c]], writes=[pa.b])
        cols = slice(g * 512, (g + 1) * 512)
        if c in (0, 1, 4, 5):
            dst = QT[c if c < 2 else c - 2]
            P.add("act", lambda e: e.activation(out=dst.ap[:, cols], in_=pa.ap, func=AF.Copy, scale=QSCALE),
                  reads=[pa.b], writes=[dst.bs[g]])
        elif c in (2, 3):
            dst = KT[c - 2]
            for hf in range(2):
                P.add("act", lambda e, hf=hf: e.activation(
                    out=dst.ap[:, g * 512 + hf * 256:g * 512 + (hf + 1) * 256],
                    in_=pa.ap[:, hf * 256:(hf + 1) * 256], func=AF.Identity,
                    accum_out=ksum.ap[:, c - 2, 2 * g + hf:2 * g + hf + 1]),
                    reads=[pa.b], writes=[dst.bs[g], ksum.b])
        else:
            dst = KT[c - 4]
            P.add("act", lambda e: e.activation(out=dst.ap[:, cols], in_=pa.ap, func=AF.Copy),
                  reads=[pa.b], writes=[dst.bs[g]])

    def chunk_V(g, tt):
        hT = hTs[g % 2]
        t = g * 4 + tt
        pa = PA[pa_ctr[0] % 2]
        pa_ctr[0] += 1
        for k in range(16):
            P.add("pe", lambda e, k=k: e.matmul(
                pa.ap, lhsT=hT.ap[:, k, tt * 128:(tt + 1) * 128], rhs=w_in_sb.ap[:, k, 1024:1536],
                start=(k == 0), stop=(k == 15)), reads=[hT.b, w_in_sb.bs[8]], writes=[pa.b])
        P.add("dve", lambda e: e.tensor_copy(out=VA[0].ap[:, t, 0:128], in_=pa.ap[:, 0:128]),
              reads=[pa.b], writes=[VA[0].bs[g]])
        P.add("dve", lambda e: e.tensor_copy(out=VA[1].ap[:, t, 0:128], in_=pa.ap[:, 128:256]),
              reads=[pa.b], writes=[VA[1].bs[g]])
        P.add("dve", lambda e: e.tensor_copy(out=VD.ap[:, t, 0:256], in_=pa.ap[:, 256:512]),
              reads=[pa.b], writes=[VD.bs[g]])

    norm_T(0, 0)
    norm_T(0, 1)
    norm_pe(0, 0)
    norm_T(0, 2)
    norm_pe(0, 1)
    norm_T(0, 3)
    norm_pe(0, 2)
    norm_pe(0, 3)
    for g in range(8):
        ci = 0
        for c in range(12):
            if c < 8:
                chunk_T(g, c)
            else:
                chunk_V(g, c - 8)
            ci += 1
            if g + 1 < 8:
                if ci in (1, 4, 7, 10):
                    norm_T(g + 1, (ci - 1) // 3)
                if ci in (3, 6, 9, 12):
                    norm_pe(g + 1, (ci - 3) // 3)
    es1a.close()

    if dbg and stage == 1:
        for nm, tt_ in (("qt0", QT[0]), ("kt1", KT[1]), ("qt3", QT[3])):
            d = nc.dram_tensor("dbg_" + nm, [128, S], BF16, kind="ExternalOutput").ap()
            b = Buf("dbg_" + nm)
            P.dma("sp", d, tt_.ap, reads=tt_.bs, writes=[b], buf=b)
            dbg_outs[nm] = b
        d = nc.dram_tensor("dbg_vd", [128, NT * 257], BF16, kind="ExternalOutput").ap()
        b = Buf("dbg_vd")
        P.dma("sp", d, VD.ap.rearrange("p t c -> p (t c)"), reads=VD.bs, writes=[b], buf=b)
        dbg_outs["vd"] = b
        d = nc.dram_tensor("dbg_ksum", [128, 32], F32, kind="ExternalOutput").ap()
        b = Buf("dbg_ksum")
        P.dma("sp", d, ksum.ap.rearrange("p a c -> p (a c)"), reads=[ksum.b], writes=[b], buf=b)
        dbg_outs["ksum"] = b
        P.emit(list(dbg_outs.values()))
        return nc

    es1b = ExitStack()
    RB = [sb(es1b, f"rb{h}", [128, S], BF16) for h in range(2)]
    lrb = sb(es1b, "lrb", [128, 2 * 16 * 128], BF16)
    pTs = [sb(es1b, f"pT{i}", [128, 512], BF16) for i in range(5)]
    O1n = sb(es1b, "o1n", [128, NT, 256], F32, nbuf=NT)
    mixed = sb(es1b, "mixed", [128, NT, 512], BF16, nbuf=NT)
    lam_t = sb(es1b, "lam_t", [128, 512], F32)
    subw_t = sb(es1b, "subw_t", [128, 256], F32)
    pastm_t = sb(es1b, "pastm_t", [128, 512], F32)
    small = sb(es1b, "small", [128, 64], F32)
    gall = sb(es1b, "gall", [128, 32, 16], F32)
    max8 = sb(es1b, "max8", [128, 32, 8], F32)
    selb = sb(es1b, "selb", [128, 32, 16], BF16)
    selb2 = sb(es1b, "selb2", [128, 32, 16], F32)
    km = sb(es1b, "km", [128, 4, 16], F32)
    kmb = sb(es1b, "kmb", [128, 2, 16], BF16)
    recs = sb(es1b, "recs", [128, 8], F32, nbuf=8)
    dtmp = sb(es1b, "dtmp", [128, 2, 256], F32, nbuf=2)
    djunk = sb(es1b, "djunk", [128, 256], F32)

    old1 = list(w_in_sb.bs)
    for tl in xts + xns + hTs:
        old1.append(tl.b)
    new1 = []
    for tl in RB + [lrb] + pTs + [O1n, mixed, lam_t, subw_t, pastm_t, small, gall, max8, selb, selb2, km, kmb, recs,
                                  dtmp, djunk]:
        new1 += tl.bs
        if tl.b not in new1:
            new1.append(tl.b)
    P.add("dve", lambda e: e.memset(bar_t.ap[:, 6:7], 0.0), reads=[], writes=old1 + [bar_t.b])
    P.add("dve", lambda e: e.memset(bar_t.ap[:, 7:8], 0.0), reads=[bar_t.b], writes=new1)

    P.dma("sp", lam_t.ap, lamv, writes=[lam_t.b], buf=lam_t.b)
    P.dma("sp", subw_t.ap, subw, writes=[subw_t.b], buf=subw_t.b)
    P.dma("sp", pastm_t.ap, pastm, writes=[pastm_t.b], buf=pastm_t.b)
    P.dma("sp", lrb.ap, lrb_d, writes=[lrb.b], buf=lrb.b)
    for h in range(2):
        P.add("pool", lambda e, h=h: e.memset(RB[h].ap, 0.0), writes=[RB[h].b])
        P.dma("sp", RB[h].ap[16:18, :], rbt_d, reads=[], writes=[RB[h].b], buf=RB[h].b)

    P.add("dve", lambda e: e.tensor_tensor(out=djunk.ap[:, 0:128], in0=lam_t.ap[:, 0:128], in1=lam_t.ap[:, 128:256],
                                           op=ALU.mult), reads=[lam_t.b], writes=[djunk.b])
    P.add("dve", lambda e: e.reduce_sum(out=small.ap[:, 0:1], in_=djunk.ap[:, 0:128], axis=AX.X),
          reads=[djunk.b], writes=[small.b])
    P.add("dve", lambda e: e.tensor_tensor(out=djunk.ap[:, 128:256], in0=lam_t.ap[:, 256:384], in1=lam_t.ap[:, 384:512],
                                           op=ALU.mult), reads=[lam_t.b], writes=[djunk.b])
    P.add("dve", lambda e: e.reduce_sum(out=small.ap[:, 1:2], in_=djunk.ap[:, 128:256], axis=AX.X),
          reads=[djunk.b], writes=[small.b])
    P.add("act", lambda e: e.activation(out=small.ap[:, 2:4], in_=small.ap[:, 0:2], func=AF.Exp),
          reads=[small.b], writes=[small.b])
    P.add("dve", lambda e: e.scalar_tensor_tensor(out=small.ap[:, 4:5], in0=small.ap[:, 3:4], scalar=-0.2,
                                                  in1=small.ap[:, 2:3], op0=ALU.add, op1=ALU.subtract),
          reads=[small.b], writes=[small.b])
    neglam = small.ap[:, 4:5]


    def _dbg_exit(tl, shape2):
        d = nc.dram_tensor("dbg_x", shape2, tl.ap.dtype, kind="ExternalOutput").ap()
        b = Buf("dbg_x")
        src = tl.ap
        if len(src.shape) == 3:
            src = src.rearrange("p a b -> p (a b)")
        P.dma("sp", d, src, reads=tl.bs + [tl.b], writes=[b], buf=b)
        P.emit([b])
        return nc

    if dbg and stage == 11:
        return _dbg_exit(small, [128, 64])
    kb_tab = [vecs_t.ap[:, 32 + 32 * i:64 + 32 * i] for i in range(3)]

    for h in range(2):
        P.add("dve", lambda e, h=h: e.tensor_scalar(out=km.ap[:, 0, :], in0=ksum.ap[:, h, :], scalar1=1.0 / 256,
                                                    scalar2=None, op0=ALU.mult), reads=[ksum.b], writes=[km.b])
        P.add("dve", lambda e: e.tensor_copy(out=kmb.ap[:, 0, :], in_=km.ap[:, 0, :]), reads=[km.b], writes=[kmb.b])
        P.add("dve", lambda e: e.tensor_copy(out=km.ap[:, 1, :], in_=kmb.ap[:, 0, :]), reads=[kmb.b], writes=[km.b])
        P.add("dve", lambda e: e.tensor_tensor(out=km.ap[:, 2, :], in0=km.ap[:, 0, :], in1=km.ap[:, 1, :],
                                               op=ALU.subtract), reads=[km.b], writes=[km.b])
        P.add("dve", lambda e: e.tensor_copy(out=kmb.ap[:, 1, :], in_=km.ap[:, 2, :]), reads=[km.b], writes=[kmb.b])
        pg = PA[2]
        for i in range(NT):
            for part in range(2):
                P.add("pe", lambda e, i=i, part=part, h=h: e.matmul(
                    pg.ap[:, i * 16:(i + 1) * 16], lhsT=QT[h].ap[:, i * 128:(i + 1) * 128], rhs=kmb.ap[:, part, :],
                    start=(part == 0), stop=(part == 1)), reads=[QT[h].bs[i // 4], kmb.b], writes=[pg.b])
        P.add("dve", lambda e: e.tensor_tensor(out=gall.ap.rearrange("p a b -> p (a b)"), in0=pg.ap, in1=pastm_t.ap,
                                               op=ALU.add), reads=[pg.b, pastm_t.b], writes=[gall.b])
        if dbg and stage == 12:
            return _dbg_exit(gall, [128, 512])
        for i in range(NT):
            P.add("dve", lambda e, i=i: e.max(out=max8.ap[:, i, :], in_=gall.ap[:, i, :]),
                  reads=[gall.b], writes=[max8.b])
        P.add("dve", lambda e: e.tensor_tensor(out=selb2.ap, in0=gall.ap,
                                               in1=max8.ap[:, :, 3:4].to_broadcast([128, 32, 16]),
                                               op=ALU.is_lt), reads=[gall.b, max8.b], writes=[selb2.b])
        P.add("dve", lambda e: e.tensor_scalar(out=selb.ap, in0=selb2.ap, scalar1=NEG, scalar2=None, op0=ALU.mult),
              reads=[selb2.b], writes=[selb.b])
        if dbg and stage == 13:
            return _dbg_exit(selb, [128, 512])
        for r in range(4):
            pt = PT[r % 2]
            for k in range(8):
                i = r * 8 + k
                P.add("pe", lambda e, pt=pt, k=k, i=i: e.transpose(
                    out=pt.ap[0:16, k * 128:(k + 1) * 128], in_=selb.ap[:, i, :], identity=ident),
                    reads=[selb.b] + CONST, writes=[pt.b])
            P.add("act", lambda e, pt=pt, r=r, h=h: e.activation(
                out=RB[h].ap[0:16, r * 1024:(r + 1) * 1024], in_=pt.ap[0:16, :], func=AF.Copy),
                reads=[pt.b], writes=[RB[h].b])

    if dbg and stage == 15:
        d = nc.dram_tensor("dbg_rb", [18, S], BF16, kind="ExternalOutput").ap()
        b = Buf("dbg_rb")
        P.dma("sp", d, RB[1].ap, reads=[RB[1].b], writes=[b], buf=b)
        d2 = nc.dram_tensor("dbg_gall", [128, 512], F32, kind="ExternalOutput").ap()
        b2 = Buf("dbg_gall")
        P.dma("sp", d2, gall.ap.rearrange("p a b -> p (a b)"), reads=[gall.b], writes=[b2], buf=b2)
        P.emit([b, b2])
        return nc

    sc_i = [0]

    tasks = []
    SCB = [PA[0], PA[1], PA[6], PA[7]]

    def attn_pass(qT, kT, V, dv1, kbias, rb, lrb_h, out_fn, qts=range(8)):
        Oacc = PA[2:6]
        for qt in qts:
            nkt = 4 * qt + 4
            for kt in range(nkt):
                j = kt - 4 * qt
                q0 = 128 * j if j > 0 else 0
                ps = SCB[sc_i[0] % 4]
                pT = pTs[sc_i[0] % 5]
                sc_i[0] += 1
                qcols = slice(qt * 512 + q0, (qt + 1) * 512)
                more = (rb is not None) or (j >= 0)

                def qk(ps=ps, kt=kt, q0=q0, qcols=qcols, more=more, j=j, qt=qt):
                    P.add("pe", lambda e: e.matmul(
                        ps.ap[:, q0:512], lhsT=kT.ap[:, kt * 128:(kt + 1) * 128], rhs=qT.ap[:, qcols],
                        start=True, stop=not more), reads=[kT.bs[kt // 4], qT.bs[qt]], writes=[ps.b])
                    if rb is not None:
                        n = kt // 2
                        P.add("pe", lambda e: e.matmul(
                            ps.ap[:, q0:512], lhsT=lrb.ap[:, (lrb_h * 16 + n) * 128:(lrb_h * 16 + n + 1) * 128],
                            rhs=rb.ap[:, qcols], start=False, stop=(j < 0)), reads=[rb.b, lrb.b], writes=[ps.b])
                    if j >= 0:
                        P.add("pe", lambda e: e.matmul(
                            ps.ap[:, q0:q0 + 128], lhsT=ident, rhs=tri, start=False, stop=True),
                            reads=CONST, writes=[ps.b])

                def rest(ps=ps, pT=pT, kt=kt, q0=q0, j=j, qt=qt):
                    m = j + 28
                    P.add("act", lambda e: e.activation(
                        out=pT.ap[:, q0:512], in_=ps.ap[:, q0:512], func=AF.Exp, bias=kbias[:, m:m + 1], scale=1.0),
                        reads=[ps.b] + CONST, writes=[pT.b])
                    for sub in range(q0 // 128, 4):
                        last = (kt == 4 * qt + sub)
                        P.add("pe", lambda e, sub=sub, last=last: e.matmul(
                            Oacc[sub].ap[:, 0:dv1], lhsT=pT.ap[:, sub * 128:(sub + 1) * 128], rhs=V.ap[:, kt, 0:dv1],
                            start=(kt == 0), stop=last), reads=[pT.b, V.bs[kt // 4]], writes=[Oacc[sub].b])
                        if last:
                            out_fn(qt * 4 + sub, Oacc[sub])

                tasks.append((qk, rest))

    DEPTH = 3

    def run_tasks():
        n = len(tasks)
        for i in range(min(DEPTH, n)):
            tasks[i][0]()
        for i in range(n):
            if i + DEPTH < n:
                tasks[i + DEPTH][0]()
            tasks[i][1]()
        tasks.clear()

    def moba_out(h):
        def f(tile, oa):
            rb_ = recs.bs[tile % 8]
            rc = recs.ap[:, tile % 8:tile % 8 + 1]
            P.add("dve", lambda e: e.reciprocal(out=rc, in_=oa.ap[:, 128:129]), reads=[oa.b], writes=[rb_])
            P.add("dve", lambda e: e.tensor_scalar(out=mixed.ap[:, tile, h * 128:(h + 1) * 128], in0=oa.ap[:, 0:128],
                                                   scalar1=rc, scalar2=None, op0=ALU.mult),
                  reads=[oa.b, rb_], writes=[mixed.bs[tile]])
        return f

    def diff1_out(tile, oa):
        rb_ = recs.bs[tile % 8]
        rc = recs.ap[:, tile % 8:tile % 8 + 1]
        P.add("dve", lambda e: e.reciprocal(out=rc, in_=oa.ap[:, 256:257]), reads=[oa.b], writes=[rb_])
        P.add("dve", lambda e: e.tensor_scalar(out=O1n.ap[:, tile, :], in0=oa.ap[:, 0:256], scalar1=rc, scalar2=None,
                                               op0=ALU.mult), reads=[oa.b, rb_], writes=[O1n.bs[tile]])

    def diff2_out(tile, oa):
        rb_ = recs.bs[tile % 8]
        rc = recs.ap[:, tile % 8:tile % 8 + 1]
        P.add("dve", lambda e: e.reciprocal(out=rc, in_=oa.ap[:, 256:257]), reads=[oa.b], writes=[rb_])
        P.add("dve", lambda e: e.tensor_tensor(out=rc, in0=rc, in1=neglam, op=ALU.mult),
              reads=[rb_, small.b], writes=[rb_])
        P.add("dve", lambda e: e.scalar_tensor_tensor(out=O1n.ap[:, tile, :], in0=oa.ap[:, 0:256], scalar=rc,
                                                      in1=O1n.ap[:, tile, :], op0=ALU.mult, op1=ALU.add),
              reads=[oa.b, rb_, O1n.bs[tile]], writes=[O1n.bs[tile]])
        if tile % 8 == 7:
            t0 = tile - 7
            sb_ = dtmp.bs[(tile // 8) % 2]
            sq = dtmp.ap[:, (tile // 8) % 2, 0:8]
            rs = dtmp.ap[:, (tile // 8) % 2, 8:16]
            for t in range(t0, t0 + 8):
                P.add("act", lambda e, t=t: e.activation(out=djunk.ap, in_=O1n.ap[:, t, :], func=AF.Square,
                                                         accum_out=sq[:, t - t0:t - t0 + 1]),
                      reads=[O1n.bs[t]], writes=[djunk.b, sb_])
            P.add("act", lambda e: e.activation(out=rs, in_=sq, func=AF.Ln, scale=1.0 / 256, bias=epsT.ap),
                  reads=[sb_, epsT.b], writes=[sb_])
            P.add("act", lambda e: e.activation(out=rs, in_=rs, func=AF.Exp, scale=-0.5), reads=[sb_], writes=[sb_])
            P.add("dve", lambda e: e.tensor_scalar(out=rs, in0=rs, scalar1=0.8, scalar2=None, op0=ALU.mult),
                  reads=[sb_], writes=[sb_])
            for t in range(t0, t0 + 8):
                P.add("dve", lambda e, t=t: e.scalar_tensor_tensor(
                    out=mixed.ap[:, t, 256:512], in0=O1n.ap[:, t, :], scalar=rs[:, t - t0:t - t0 + 1],
                    in1=subw_t.ap, op0=ALU.mult, op1=ALU.mult),
                    reads=[O1n.bs[t], sb_, subw_t.b], writes=[mixed.bs[t]])

    def exchange(j):
        P.dma("sp", bounce[j].ap().rearrange("(t p) f -> p t f", p=128), mixed.ap[:, j * 8:(j + 1) * 8, :],
              reads=mixed.bs[j * 8:(j + 1) * 8], writes=[bounce_b[j]], buf=bounce_b[j])
        P.add("pool", lambda e: e.collective_compute(
            "AllGather", ALU.bypass, replica_groups=[[0, 1, 2, 3], [4, 5, 6, 7]],
            ins=[bounce[j].ap().opt()], outs=[agbig.ap()[j * 4096:(j + 1) * 4096, :].opt()]),
            reads=[bounce_b[j]], writes=[agout_b[j]], kind="cc", buf=agout_b[j])

    passes = [(QT[0], KT[0], VA[0], 129, kb_tab[0], RB[0], 0, moba_out(0)),
              (QT[1], KT[1], VA[1], 129, kb_tab[1], RB[1], 1, moba_out(1)),
              (QT[2], KT[2], VD, 257, kb_tab[2], None, 0, diff1_out),
              (QT[3], KT[3], VD, 257, kb_tab[2], None, 0, diff2_out)]
    if not (dbg and stage == 17):
        for j in range(4):
            for pz in passes:
                attn_pass(*pz, qts=(2 * j, 2 * j + 1))
            if not (dbg and stage == 2):
                tasks.append((lambda: None, lambda j=j: exchange(j)))
        run_tasks()
    if dbg and stage == 17:
        attn_pass(QT[0], KT[0], VA[0], 129, kb_tab[0], RB[0], 0, moba_out(0))
        run_tasks()
        d = nc.dram_tensor("dbg_mixed", [128, NT * 512], BF16, kind="ExternalOutput").ap()
        b = Buf("dbg_mixed")
        P.dma("sp", d, mixed.ap.rearrange("p t c -> p (t c)"), reads=mixed.bs, writes=[b], buf=b)
        P.emit([b])
        return nc

    if dbg and stage == 2:
        d = nc.dram_tensor("dbg_mixed", [128, NT * 512], BF16, kind="ExternalOutput").ap()
        b = Buf("dbg_mixed")
        P.dma("sp", d, mixed.ap.rearrange("p t c -> p (t c)"), reads=mixed.bs, writes=[b], buf=b)
        P.emit([b])
        return nc

    es1b.close()
    es1.close()

    allb = Buf("phase_barrier")

    def barrier():
        for en in ("pe", "act", "dve", "pool", "sp"):
            pass

    es2 = ExitStack()
    x1 = sb(es2, "x1", [128, 8, D], F32, nbuf=8)
    ss2 = sb(es2, "ss2", [128, 16], F32, nbuf=16)
    selI = cbf_t.ap[:, 256:768]

    es2a = ExitStack()
    mixT = sb(es2a, "mixT", [128, 16, 1024], BF16, nbuf=16)
    agb = [sb(es2a, f"agb{i}", [128, 32, 512], BF16) for i in range(1)]
    wob = [sb(es2a, f"wob{i}", [128, 16, 512], BF16) for i in range(2)]

    p1_bufs = []
    for tl in QT + KT + VA + [VD, ksum, ss1, rstd1, mixed, O1n, lam_t, subw_t, pastm_t, small, gall, max8, selb, selb2,
                              km, kmb, recs, dtmp, djunk, lrb] + RB + pTs + xts + xns + hTs + [w_in_sb]:
        p1_bufs += tl.bs
        if tl.b not in p1_bufs:
            p1_bufs.append(tl.b)
    P.add("dve", lambda e: e.memset(bar_t.ap[:, 0:1], 0.0), reads=[], writes=p1_bufs + [bar_t.b])
    new_bufs = x1.bs + ss2.bs + mixT.bs + [t_.b for t_ in agb + wob]
    P.add("dve", lambda e: e.memset(bar_t.ap[:, 1:2], 0.0), reads=[bar_t.b], writes=new_bufs)

    for i in range(8):
        P.dma("sp", x1.ap[:, i, :], xres[i * 128:(i + 1) * 128, :], writes=[x1.bs[i]], buf=x1.bs[i])

    idx_t = sb(es2a, "idx_t", [128, 32], mybir.dt.int32)
    P.add("dve", lambda e: e.memset(bar_t.ap[:, 2:3], 0.0), reads=[bar_t.b], writes=[idx_t.b])
    P.dma("sp", idx_t.ap, idxg_d, writes=[idx_t.b], buf=idx_t.b)
    ab = agb[0]
    for ri in range(32):
        P.add("pool", lambda e, ri=ri: e.indirect_dma_start(
            out=ab.ap[:, ri, :], out_offset=None, in_=agbig.ap(),
            in_offset=bass.IndirectOffsetOnAxis(ap=idx_t.ap[:, ri:ri + 1], axis=0)),
            reads=[idx_t.b] + agout_b, writes=[ab.b], kind="d", buf=ab.b)
    ev = 0
    for r in range(4):
        for fc in range(4):
            pt = PT[ev % 2]
            for i in range(8):
                P.add("pe", lambda e, pt=pt, r=r, fc=fc, i=i: e.transpose(
                    out=pt.ap[:, i * 128:(i + 1) * 128], in_=ab.ap[:, r * 8 + i, fc * 128:(fc + 1) * 128],
                    identity=ident), reads=[ab.b] + CONST, writes=[pt.b])
            if ev % 2 == 0:
                P.add("act", lambda e, pt=pt, r=r, fc=fc: e.activation(
                    out=mixT.ap[:, r * 4 + fc, :], in_=pt.ap, func=AF.Copy), reads=[pt.b],
                    writes=[mixT.bs[r * 4 + fc]])
            else:
                P.add("dve", lambda e, pt=pt, r=r, fc=fc: e.tensor_copy(out=mixT.ap[:, r * 4 + fc, :], in_=pt.ap),
                      reads=[pt.b], writes=[mixT.bs[r * 4 + fc]])
            ev += 1

    pa_i = 0
    w_out_v = w_out_p.rearrange("(k p) c -> p k c", p=128)
    for dc in range(4):
        wb = wob[dc % 2]
        P.dma("pool", wb.ap, w_out_v[:, :, dc * 512:(dc + 1) * 512], writes=[wb.b], buf=wb.b)
        for i in range(8):
            pa = PA[pa_i % 2]
            pa_i += 1
            for k in range(16):
                P.add("pe", lambda e, pa=pa, k=k, i=i, wb=wb: e.matmul(
                    pa.ap, lhsT=mixT.ap[:, k, i * 128:(i + 1) * 128], rhs=wb.ap[:, k, :],
                    start=(k == 0), stop=(k == 15)), reads=[mixT.bs[k], wb.b], writes=[pa.b])
            P.add("dve", lambda e, pa=pa, i=i, dc=dc: e.tensor_tensor(
                out=x1.ap[:, i, dc * 512:(dc + 1) * 512], in0=pa.ap, in1=x1.ap[:, i, dc * 512:(dc + 1) * 512],
                op=ALU.add), reads=[pa.b, x1.bs[i]], writes=[x1.bs[i]])
    es2a.close()

    if dbg and stage == 3:
        d = nc.dram_tensor("dbg_x1", [128, 8 * D], F32, kind="ExternalOutput").ap()
        b = Buf("dbg_x1")
        P.dma("sp", d, x1.ap.rearrange("p t c -> p (t c)"), reads=x1.bs, writes=[b], buf=b)
        P.emit([b])
        return nc

    es2b = ExitStack()
    h2T = sb(es2b, "h2T", [128, 16, 1024], BF16, nbuf=8)
    xn2 = [sb(es2b, f"xn2_{i}", [128, D], BF16) for i in range(2)]
    wr_sb = sb(es2b, "wr_sb", [128, 16, 36], BF16)
    rb_sb = sb(es2b, "rb_sb", [128, 36], F32)
    lg = sb(es2b, "lg", [128, 8, 36], F32, nbuf=8)
    rt = sb(es2b, "rt", [128, 8, 64], F32, nbuf=8)
    comb = sb(es2b, "comb", [128, 8, 32], F32, nbuf=8)
    es2w = ExitStack()
    wg = [sb(es2w, f"wg{i}", [128, 16, DE], BF16) for i in range(2)]
    wu = [sb(es2w, f"wu{i}", [128, 16, DE], BF16) for i in range(2)]
    wd = [sb(es2w, "wd0", [128, 4, D], BF16)] * 2
    sa = [sb(es2w, f"sa{i}", [128, 512], BF16) for i in range(2)]
    actT = [sb(es2w, f"actT{i}", [128, 4, 512], BF16, nbuf=4) for i in range(2)]
    new_bufs = h2T.bs + lg.bs + rt.bs + comb.bs + [wr_sb.b, rb_sb.b]
    for tl in xn2 + wg + wu + wd + sa:
        new_bufs.append(tl.b)
    for tl in actT:
        new_bufs += tl.bs
    old = mixT.bs + [t_.b for t_ in agb + wob]
    P.add("dve", lambda e: e.memset(bar_t.ap[:, 2:3], 0.0), reads=[], writes=old + [bar_t.b])
    P.add("dve", lambda e: e.memset(bar_t.ap[:, 3:4], 0.0), reads=[bar_t.b], writes=new_bufs)

    P.dma("pool", wr_sb.ap, w_r.rearrange("(k p) c -> p k c", p=128), writes=[wr_sb.b], buf=wr_sb.b)
    P.dma("sp", rb_sb.ap, rbias, writes=[rb_sb.b], buf=rb_sb.b)

    def load_expert(e_):
        P.dma("pool", wg[e_ % 2].ap, w_gate[e_].rearrange("(k p) f -> p k f", p=128), writes=[wg[e_ % 2].b],
              buf=wg[e_ % 2].b)
        P.dma("pool", wu[e_ % 2].ap, w_up[e_].rearrange("(k p) f -> p k f", p=128), writes=[wu[e_ % 2].b],
              buf=wu[e_ % 2].b)

    def load_down(e_):
        P.dma("pool", wd[0].ap, w_down[e_].rearrange("(k p) d -> p k d", p=128), writes=[wd[0].b], buf=wd[0].b)

    load_expert(0)
    load_down(0)
    load_expert(1)

    def rms_rstd(ssap, ssb, n):
        P.add("act", lambda e: e.activation(out=ssap, in_=ssap, func=AF.Sqrt, scale=1.0 / n, bias=epsT.ap),
              reads=[ssb, epsT.b], writes=[ssb])
        P.add("dve", lambda e: e.reciprocal(out=ssap, in_=ssap), reads=[ssb], writes=[ssb])

    for i in range(8):
        xn = xn2[i % 2]
        ssap = ss2.ap[:, i:i + 1]
        P.add("act", lambda e, xn=xn, i=i, ssap=ssap: e.activation(out=xn.ap, in_=x1.ap[:, i, :], func=AF.Square,
                                                                  accum_out=ssap),
              reads=[x1.bs[i]], writes=[xn.b, ss2.bs[i]])
        rms_rstd(ssap, ss2.bs[i], D)
        P.add("dve", lambda e, xn=xn, i=i, ssap=ssap: e.tensor_scalar(out=xn.ap, in0=x1.ap[:, i, :], scalar1=ssap,
                                                                     scalar2=None, op0=ALU.mult),
              reads=[x1.bs[i], ss2.bs[i]], writes=[xn.b])
        for r in range(2):
            pt = PT[r]
            for k in range(8):
                P.add("pe", lambda e, pt=pt, xn=xn, k=k, r=r: e.transpose(
                    out=pt.ap[:, k * 128:(k + 1) * 128], in_=xn.ap[:, (r * 8 + k) * 128:(r * 8 + k + 1) * 128],
                    identity=ident), reads=[xn.b] + CONST, writes=[pt.b])
            P.add("dve", lambda e, pt=pt, r=r, i=i: e.tensor_tensor(
                out=h2T.ap[:, r * 8:(r + 1) * 8, i * 128:(i + 1) * 128],
                in0=pt.ap.rearrange("p (k n) -> p k n", k=8),
                in1=wffn[:, r * 8:(r + 1) * 8].unsqueeze(2).to_broadcast([128, 8, 128]),
                op=ALU.mult), reads=[pt.b] + CONST, writes=[h2T.bs[i]])

    for i in range(8):
        pa = PA[2 + i % 2]
        for k in range(16):
            P.add("pe", lambda e, pa=pa, k=k, i=i: e.matmul(
                pa.ap[:, 0:36], lhsT=h2T.ap[:, k, i * 128:(i + 1) * 128], rhs=wr_sb.ap[:, k, :],
                start=(k == 0), stop=(k == 15)), reads=[h2T.bs[i], wr_sb.b], writes=[pa.b])
        L = lg.ap[:, i, :]
        Lb = lg.bs[i]
        R = rt.ap[:, i, :]
        Rb = rt.bs[i]
        C = comb.ap[:, i, :]
        Cb = comb.bs[i]
        P.add("dve", lambda e, pa=pa, L=L: e.tensor_tensor(out=L, in0=pa.ap[:, 0:36], in1=rb_sb.ap, op=ALU.add),
              reads=[pa.b, rb_sb.b], writes=[Lb])
        P.add("dve", lambda e, L=L, R=R: e.reduce_max(out=R[:, 0:1], in_=L[:, 0:4], axis=AX.X), reads=[Lb], writes=[Rb])
        P.add("dve", lambda e, L=L, R=R: e.tensor_scalar(out=R[:, 4:8], in0=L[:, 0:4], scalar1=R[:, 0:1], scalar2=None,
                                                         op0=ALU.is_ge), reads=[Lb, Rb], writes=[Rb])
        P.add("dve", lambda e, L=L, R=R: e.tensor_scalar(out=R[:, 8:12], in0=L[:, 0:4], scalar1=R[:, 0:1], scalar2=None,
                                                         op0=ALU.subtract), reads=[Lb, Rb], writes=[Rb])
        P.add("act", lambda e, R=R: e.activation(out=R[:, 8:12], in_=R[:, 8:12], func=AF.Exp, accum_out=R[:, 1:2]),
              reads=[Rb], writes=[Rb])
        P.add("dve", lambda e, L=L, R=R: e.tensor_scalar(out=R[:, 16:24], in0=L[:, 4:12], scalar1=R[:, 4:5], scalar2=None,
                                                         op0=ALU.mult), reads=[Lb, Rb], writes=[Rb])
        for g_ in range(1, 4):
            P.add("dve", lambda e, L=L, R=R, g_=g_: e.scalar_tensor_tensor(
                out=R[:, 16:24], in0=L[:, 4 + 8 * g_:12 + 8 * g_], scalar=R[:, 4 + g_:5 + g_], in1=R[:, 16:24],
                op0=ALU.mult, op1=ALU.add), reads=[Lb, Rb], writes=[Rb])
        P.add("dve", lambda e, R=R: e.reduce_max(out=R[:, 2:3], in_=R[:, 16:24], axis=AX.X), reads=[Rb], writes=[Rb])
        P.add("dve", lambda e, R=R: e.tensor_scalar(out=R[:, 24:32], in0=R[:, 16:24], scalar1=R[:, 2:3], scalar2=None,
                                                    op0=ALU.is_ge), reads=[Rb], writes=[Rb])
        P.add("dve", lambda e, R=R: e.scalar_tensor_tensor(out=R[:, 32:40], in0=R[:, 24:32], scalar=-1e30,
                                                           in1=R[:, 16:24], op0=ALU.mult, op1=ALU.add),
              reads=[Rb], writes=[Rb])
        P.add("dve", lambda e, R=R: e.reduce_max(out=R[:, 3:4], in_=R[:, 32:40], axis=AX.X), reads=[Rb], writes=[Rb])
        P.add("dve", lambda e, R=R: e.tensor_scalar(out=R[:, 40:48], in0=R[:, 32:40], scalar1=R[:, 3:4], scalar2=None,
                                                    op0=ALU.is_ge), reads=[Rb], writes=[Rb])
        P.add("dve", lambda e, R=R: e.tensor_tensor(out=R[:, 48:49], in0=R[:, 3:4], in1=R[:, 2:3], op=ALU.subtract),
              reads=[Rb], writes=[Rb])
        P.add("act", lambda e, R=R: e.activation(out=R[:, 49:50], in_=R[:, 48:49], func=AF.Exp), reads=[Rb], writes=[Rb])
        P.add("dve", lambda e, R=R: e.tensor_scalar(out=R[:, 50:51], in0=R[:, 49:50], scalar1=1.0, scalar2=None,
                                                    op0=ALU.add), reads=[Rb], writes=[Rb])
        P.add("dve", lambda e, R=R: e.tensor_tensor(out=R[:, 50:51], in0=R[:, 50:51], in1=R[:, 1:2], op=ALU.mult),
              reads=[Rb], writes=[Rb])
        P.add("dve", lambda e, R=R: e.reciprocal(out=R[:, 51:52], in_=R[:, 50:51]), reads=[Rb], writes=[Rb])
        P.add("dve", lambda e, R=R: e.tensor_tensor(out=R[:, 52:53], in0=R[:, 51:52], in1=R[:, 49:50], op=ALU.mult),
              reads=[Rb], writes=[Rb])
        P.add("dve", lambda e, R=R: e.tensor_scalar(out=R[:, 56:64], in0=R[:, 24:32], scalar1=R[:, 51:52], scalar2=None,
                                                    op0=ALU.mult), reads=[Rb], writes=[Rb])
        P.add("dve", lambda e, R=R: e.scalar_tensor_tensor(out=R[:, 56:64], in0=R[:, 40:48], scalar=R[:, 52:53],
                                                           in1=R[:, 56:64], op0=ALU.mult, op1=ALU.add),
              reads=[Rb], writes=[Rb])
        for g_ in range(4):
            P.add("dve", lambda e, R=R, C=C, g_=g_: e.tensor_scalar(out=C[:, 8 * g_:8 * g_ + 8], in0=R[:, 56:64],
                                                                   scalar1=R[:, 4 + g_:5 + g_], scalar2=None,
                                                                   op0=ALU.mult), reads=[Rb], writes=[Cb])
    it = 0
    for ex in range(N_EXP):
        wgb, wub, wdb = wg[ex % 2], wu[ex % 2], wd[ex % 2]
        for tg in range(2):
            aT = actT[tg]
            for fc in range(4):
                pa_g = PA[0]
                pa_u = PA[1]
                s_ = sa[it % 2]
                it += 1
                for k in range(16):
                    P.add("pe", lambda e, k=k, fc=fc, tg=tg, wgb=wgb: e.matmul(
                        pa_g.ap, lhsT=wgb.ap[:, k, fc * 128:(fc + 1) * 128], rhs=h2T.ap[:, k, tg * 512:(tg + 1) * 512],
                        start=(k == 0), stop=(k == 15)), reads=[wgb.b] + h2T.bs[tg * 4:tg * 4 + 4], writes=[pa_g.b])
                for k in range(16):
                    P.add("pe", lambda e, k=k, fc=fc, tg=tg, wub=wub: e.matmul(
                        pa_u.ap, lhsT=wub.ap[:, k, fc * 128:(fc + 1) * 128], rhs=h2T.ap[:, k, tg * 512:(tg + 1) * 512],
                        start=(k == 0), stop=(k == 15)), reads=[wub.b] + h2T.bs[tg * 4:tg * 4 + 4], writes=[pa_u.b])
                P.add("act", lambda e, s_=s_: e.activation(out=s_.ap, in_=pa_g.ap, func=AF.Silu),
                      reads=[pa_g.b], writes=[s_.b])
                P.add("dve", lambda e, s_=s_, aT=aT, fc=fc: e.tensor_tensor(
                    out=aT.ap[:, fc, :], in0=pa_u.ap, in1=s_.ap, op=ALU.mult), reads=[pa_u.b, s_.b], writes=[aT.bs[fc]])
            for ii in range(4):
                i = tg * 4 + ii
                for dc in range(4):
                    pa = PA[2 + (ii * 4 + dc) % 4]
                    for fc in range(4):
                        P.add("pe", lambda e, pa=pa, fc=fc, ii=ii, dc=dc, aT=aT, wdb=wdb: e.matmul(
                            pa.ap, lhsT=aT.ap[:, fc, ii * 128:(ii + 1) * 128], rhs=wdb.ap[:, fc, dc * 512:(dc + 1) * 512],
                            start=(fc == 0), stop=(fc == 3)), reads=[aT.bs[fc], wdb.b], writes=[pa.b])
                    P.add("dve", lambda e, pa=pa, i=i, dc=dc, ex=ex: e.scalar_tensor_tensor(
                        out=x1.ap[:, i, dc * 512:(dc + 1) * 512], in0=pa.ap, scalar=comb.ap[:, i, ex:ex + 1],
                        in1=x1.ap[:, i, dc * 512:(dc + 1) * 512], op0=ALU.mult, op1=ALU.add),
                        reads=[pa.b, x1.bs[i], comb.bs[i]], writes=[x1.bs[i]])
        if ex + 1 < N_EXP:
            load_down(ex + 1)
        if ex + 2 < N_EXP:
            load_expert(ex + 2)

    oldw = [wg[0].b, wg[1].b, wu[0].b, wu[1].b, wd[0].b, sa[0].b, sa[1].b] + actT[0].bs + actT[1].bs
    P.add("dve", lambda e: e.memset(bar_t.ap[:, 4:5], 0.0), reads=[], writes=oldw + [bar_t.b])
    es2w.close()
    es2c = ExitStack()
    wfin_t = sb(es2c, "wfin_t", [128, D], F32)
    P.add("dve", lambda e: e.memset(bar_t.ap[:, 5:6], 0.0), reads=[bar_t.b], writes=[wfin_t.b])
    P.dma("sp", wfin_t.ap, wfin, writes=[wfin_t.b], buf=wfin_t.b)
    for i in range(8):
        xn = xn2[i % 2]
        ssap = ss2.ap[:, 8 + i:9 + i]
        P.add("act", lambda e, xn=xn, i=i, ssap=ssap: e.activation(out=xn.ap, in_=x1.ap[:, i, :], func=AF.Square,
                                                                  accum_out=ssap),
              reads=[x1.bs[i]], writes=[xn.b, ss2.bs[8 + i]])
        rms_rstd(ssap, ss2.bs[8 + i], D)
        P.add("dve", lambda e, i=i, ssap=ssap: e.scalar_tensor_tensor(
            out=x1.ap[:, i, :], in0=x1.ap[:, i, :], scalar=ssap, in1=wfin_t.ap, op0=ALU.mult, op1=ALU.mult),
            reads=[x1.bs[i], ss2.bs[8 + i], wfin_t.b], writes=[x1.bs[i]])
        P.dma("sp", out[i * 128:(i + 1) * 128, :], x1.ap[:, i, :], reads=[x1.bs[i]], writes=[out_b], buf=out_b)
    P.emit([out_b])
    return nc


def _bf(a):
    return np.asarray(a, dtype=np.float32).astype(ml_dtypes.bfloat16)


def _consts(g):
    slopes_a = [2.0 ** -(i + 1) for i in range(8)]
    slopes_b = [2.0 ** (-2 * (i + 1)) for i in range(4)]
    p = np.arange(128, dtype=np.float64)[:, None]
    m = np.arange(32, dtype=np.float64)[None, :]
    kb = []
    for h in range(2):
        kb.append(slopes_a[2 * g + h] * (p + 128.0 * (m - 28)))
    kb.append(slopes_b[g] * (p + 128.0 * (m - 28) - 256.0))
    kb = np.concatenate(kb, axis=1).astype(np.float32)
    ident = np.eye(128, dtype=np.float32)
    kk = np.arange(128)[:, None]
    qq = np.arange(128)[None, :]
    tri = np.where(kk > qq, NEG, 0.0).astype(np.float32)
    selI = np.zeros((128, 4, 128), np.float32)
    selI[:, g, :] = ident
    cbf = _bf(np.concatenate([ident, tri, selI.reshape(128, 512)], axis=1))
    lrb = np.zeros((128, 2, 16, 128), np.float32)
    for h in range(2):
        for n in range(16):
            lrb[n, h, n, :] = 1.0
        lrb[16:18, h, :, :] = -slopes_a[2 * g + h]
    lrb = _bf(lrb.reshape(128, 2 * 16 * 128))
    t = np.arange(S) % 512
    rbt = _bf(np.stack([t % 256, t - t % 256]).astype(np.float32))
    pm = np.zeros((32, 16), np.float32)
    for i in range(32):
        j = i // 2
        pm[i, j] = 1e30
        pm[i, j + 1:] = -1e30
    pastm = np.broadcast_to(pm.reshape(1, 512), (128, 512)).copy()
    selE = np.zeros((32, N_EXP, 128), np.float32)
    for e in range(N_EXP):
        selE[e, e, :] = 1.0
    selE = _bf(selE.reshape(32, N_EXP * 128))
    return kb, cbf, lrb, rbt, pastm, selE


def make_in_maps(x, norm_mix_w, w_in, lambda_q1, lambda_k1, lambda_q2, lambda_k2, diff_subln_w,
                 w_out, norm_ffn_w, w_router_group, b_router_group, w_router_expert, b_router_expert,
                 w_gate, w_up, w_down, norm_final_w):
    f = lambda a: np.ascontiguousarray(np.asarray(a, dtype=np.float32))
    x = f(x); w_in = f(w_in)[0]; w_out = f(w_out)[0]
    w_gate = f(w_gate)[0]; w_up = f(w_up)[0]; w_down = f(w_down)[0]
    wmix = f(norm_mix_w)[0].reshape(16, 128).T
    wffn = f(norm_ffn_w)[0].reshape(16, 128).T
    lam = np.concatenate([f(lambda_q1)[0], f(lambda_k1)[0], f(lambda_q2)[0], f(lambda_k2)[0]])
    lamv = np.ascontiguousarray(np.broadcast_to(lam[None, :], (128, 512)))
    subw = np.ascontiguousarray(np.broadcast_to(f(diff_subln_w)[0][None, :], (128, 256)))
    rbias = np.concatenate([f(b_router_group)[0], f(b_router_expert)[0]])
    rbias = np.ascontiguousarray(np.broadcast_to(rbias[None, :], (128, 36)))
    wfin = np.ascontiguousarray(np.broadcast_to(f(norm_final_w)[None, :], (128, D)))
    w_r = np.ascontiguousarray(np.concatenate([f(w_router_group)[0], f(w_router_expert)[0]], axis=1))
    in_maps = []
    for c in range(8):
        b, g = c // 4, c % 4
        cols = []
        for h in (2 * g, 2 * g + 1):
            cols.append(np.arange(h * 128, (h + 1) * 128))
        for h in (2 * g, 2 * g + 1):
            cols.append(1024 + np.arange(h * 128, (h + 1) * 128))
        cols.append(3072 + g * 256 + np.arange(256))
        cols.append(4096 + g * 256 + np.arange(256))
        for h in (2 * g, 2 * g + 1):
            cols.append(2048 + np.arange(h * 128, (h + 1) * 128))
        cols.append(5120 + g * 256 + np.arange(256))
        cols = np.concatenate(cols)
        w_in_c = np.ascontiguousarray(w_in[:, cols])
        rows = np.concatenate([np.concatenate([np.arange(256 * r, 256 * r + 256), 1024 + np.arange(256 * r, 256 * r + 256)])
                               for r in range(4)])
        kb, cbf, lrb, rbt, pastm, selE = _consts(g)
        ri = np.arange(32)[None, :]
        idxg = (g * 4096 + (ri // 8) * 1024 + (ri % 8) * 128 + np.arange(128)[:, None]).astype(np.int32)
        vecs = np.ascontiguousarray(np.concatenate([wmix, wffn, kb], axis=1).astype(np.float32))
        in_maps.append({
            "xb": x[b], "xres": np.ascontiguousarray(x[b, g * 1024:(g + 1) * 1024]),
            "w_in_c": w_in_c, "w_out_p": np.ascontiguousarray(w_out[rows]), "w_r": w_r,
            "w_gate": w_gate, "w_up": w_up, "w_down": w_down,
            "vecs": vecs, "lamv": lamv, "subw": subw, "rbias": rbias, "wfin": wfin, "pastm": pastm,
            "cbf": cbf, "lrb": lrb, "rbt": rbt, "idxg": np.ascontiguousarray(idxg),
        })
    return in_maps


_NC = None


def kernel(**inputs):
    global _NC
    in_maps = make_in_maps(**inputs)
    if _NC is None:
        _NC = build()
    res = run_bass_kernel_spmd(_NC, in_maps, core_ids=list(range(8)))
    outp = np.empty((2, S, D), np.float32)
    for c in range(8):
        b, g = c // 4, c % 4
        outp[b, g * 1024:(g + 1) * 1024] = res.results[c]["out"]
    return outp
```

```python
import numpy as np
import ml_dtypes
from contextlib import ExitStack
import concourse.bass as bass
import concourse.mybir as mybir
from concourse.bass_utils import run_bass_kernel_spmd

F32 = mybir.dt.float32
BF16 = mybir.dt.bfloat16
AF = mybir.ActivationFunctionType
ALU = mybir.AluOpType
AX = mybir.AxisListType

D = 2048
S = 4096
NT = 32
EPS = 1e-6
NEG = -30000.0
N_EXP = 32
DE = 512
SEM_BLK = 2000


class Buf:
    __slots__ = ("name", "last_w", "readers", "dma_sem", "dma_cnt")

    def __init__(self, name):
        self.name = name
        self.last_w = None
        self.readers = {}
        self.dma_sem = None
        self.dma_cnt = 0


class Op:
    __slots__ = ("eng", "fn", "deps", "kind", "buf", "dma_val", "sig", "semidx", "idx")


class Prog:
    def __init__(self, nc):
        self.nc = nc
        self.ops = []
        self.engs = {"pe": nc.tensor, "act": nc.scalar, "dve": nc.vector, "pool": nc.gpsimd, "sp": nc.sync}

    def add(self, eng, fn, reads=(), writes=(), kind="c", buf=None):
        op = Op()
        op.eng, op.fn, op.kind, op.buf = eng, fn, kind, buf
        op.sig = False
        op.semidx = 0
        op.idx = len(self.ops)
        deps = set()
        for b in reads:
            if b.last_w is not None:
                deps.add(b.last_w)
        for b in writes:
            if b.last_w is not None:
                deps.add(b.last_w)
            for r in b.readers.values():
                if isinstance(r, list):
                    deps.update(r)
                else:
                    deps.add(r)
        deps.discard(op.idx)
        op.deps = deps
        key = eng if kind == "c" else "dma"
        for b in reads:
            if key == "dma":
                b.readers.setdefault("dma", []).append(op.idx)
            else:
                b.readers[key] = op.idx
        for b in writes:
            b.last_w = op.idx
            b.readers = {}
        if kind == "d":
            buf.dma_cnt += 1
            op.dma_val = 16 * buf.dma_cnt
        elif kind == "cc":
            buf.dma_cnt += 1
            op.dma_val = buf.dma_cnt
        self.ops.append(op)
        return op

    def dma(self, eng, out, in_, reads=(), writes=(), buf=None):
        return self.add(eng, lambda e: e.dma_start(out=out, in_=in_), reads, writes, kind="d", buf=buf)

    def emit(self, final_bufs):
        nc = self.nc
        ops = self.ops
        for op in ops:
            for d in op.deps:
                p = ops[d]
                if p.kind == "c":
                    if p.eng == op.eng and op.kind == "c" and op.eng == "pe":
                        continue
                    p.sig = True
        cnt = {e: 0 for e in self.engs}
        for op in ops:
            if op.kind == "c" and op.sig:
                cnt[op.eng] += 1
                op.semidx = cnt[op.eng]
        sems = {}

        def eng_sem(e, idx):
            blk = (idx - 1) // SEM_BLK
            k = (e, blk)
            if k not in sems:
                sems[k] = nc.alloc_semaphore(f"s_{e}_{blk}")
            return sems[k], (idx - 1) % SEM_BLK + 1

        def buf_sem(b):
            if b.dma_sem is None:
                b.dma_sem = nc.alloc_semaphore(f"d_{b.name}")
            return b.dma_sem

        waited_eng = {e: {p: 0 for p in self.engs} for e in self.engs}
        waited_buf = {e: {} for e in self.engs}
        for op in ops:
            e = self.engs[op.eng]
            need_eng = {}
            need_buf = {}
            for d in op.deps:
                p = ops[d]
                if p.kind == "c":
                    if p.eng == op.eng and op.kind == "c" and op.eng == "pe":
                        continue
                    if p.semidx > need_eng.get(p.eng, 0):
                        need_eng[p.eng] = p.semidx
                else:
                    if p.dma_val > need_buf.get(id(p.buf), (None, 0))[1]:
                        need_buf[id(p.buf)] = (p.buf, p.dma_val)
            for pe_, idx in need_eng.items():
                if waited_eng[op.eng][pe_] < idx:
                    s, v = eng_sem(pe_, idx)
                    e.wait_ge(s, v)
                    waited_eng[op.eng][pe_] = idx
            for bid, (b, v) in need_buf.items():
                if waited_buf[op.eng].get(bid, 0) < v:
                    e.wait_ge(buf_sem(b), v)
                    waited_buf[op.eng][bid] = v
            ins = op.fn(e)
            if op.kind == "c":
                if op.sig:
                    s, v = eng_sem(op.eng, op.semidx)
                    ins.then_inc(s, 1)
            elif op.kind == "d":
                ins.then_inc(buf_sem(op.buf), 16)
            else:
                ins.then_inc(buf_sem(op.buf))
        sp = nc.sync
        for b in final_bufs:
            sp.wait_ge(buf_sem(b), 16 * b.dma_cnt)


class T:
    def __init__(self, ap, name, nbuf=1):
        self.ap = ap
        self.b = Buf(name)
        self.bs = [Buf(f"{name}_{i}") for i in range(nbuf)] if nbuf > 1 else [self.b]


def build(stage=99, dbg=False):
    nc = bass.Bass("TRN2", target_bir_lowering=False)
    P = Prog(nc)
    def din(name, shape, dt=F32):
        return nc.dram_tensor(name, shape, dt, kind="ExternalInput").ap()

    xb = din("xb", [S, D])
    xres = din("xres", [1024, D])
    w_in_c = din("w_in_c", [D, 1536])
    w_out_p = din("w_out_p", [D, D])
    w_r = din("w_r", [D, 36])
    if stage == 99:
        w_gate = din("w_gate", [N_EXP, D, DE])
        w_up = din("w_up", [N_EXP, D, DE])
        w_down = din("w_down", [N_EXP, DE, D])
    vecs = din("vecs", [128, 16 + 16 + 96])
    lamv = din("lamv", [128, 512])
    subw = din("subw", [128, 256])
    rbias = din("rbias", [128, 36])
    wfin = din("wfin", [128, D])
    pastm = din("pastm", [128, 512])
    cbf = din("cbf", [128, 256 + 512], BF16)
    lrb_d = din("lrb", [128, 2 * 16 * 128], BF16)
    rbt_d = din("rbt", [2, S], BF16)
    out = nc.dram_tensor("out", [1024, D], F32, kind="ExternalOutput").ap()
    bounce = [nc.dram_tensor(f"bounce{j}", [1024, 512], BF16) for j in range(4)]
    agbig = nc.dram_tensor("agbig", [4 * 4096, 512], BF16)
    idxg_d = nc.dram_tensor("idxg", [128, 32], mybir.dt.int32, kind="ExternalInput").ap()
    bounce_b = [Buf(f"bounce{j}") for j in range(4)]
    agout_b = [Buf(f"agout{j}") for j in range(4)]
    out_b = Buf("outd")
    dbg_outs = {}

    PA = [T(nc.alloc_psum_tensor(f"pa{i}", [128, 512], F32).ap(), f"pa{i}") for i in range(8)]
    PT = []
    for i in range(2):
        v = T(PA[6 + i].ap.bitcast(BF16), f"ptv{i}")
        v.b = PA[6 + i].b
        v.bs = [v.b]
        PT.append(v)

    es_all = ExitStack()

    def sb(es, name, shape, dt, nbuf=1):
        h = es.enter_context(nc.sbuf_tensor("sb_" + name, shape, dt))
        return T(h.ap() if hasattr(h, "ap") and callable(h.ap) else h, name, nbuf)

    vecs_t = sb(es_all, "vecs", [128, 128], F32)
    cbf_t = sb(es_all, "cbf", [128, 768], BF16)
    P.dma("sp", vecs_t.ap, vecs, writes=[vecs_t.b], buf=vecs_t.b)
    P.dma("sp", cbf_t.ap, cbf, writes=[cbf_t.b], buf=cbf_t.b)
    epsT = sb(es_all, "epsT", [128, 1], F32)
    bar_t = sb(es_all, "bar_t", [128, 8], F32)
    P.add("dve", lambda e: e.memset(epsT.ap, EPS), writes=[epsT.b])
    wmix = vecs_t.ap[:, 0:16]
    wffn = vecs_t.ap[:, 16:32]
    ident = cbf_t.ap[:, 0:128]
    tri = cbf_t.ap[:, 128:256]
    CONST = [vecs_t.b, cbf_t.b]

    es1 = ExitStack()
    QT = [sb(es1, f"qt{i}", [128, S], BF16, nbuf=8) for i in range(4)]
    KT = [sb(es1, f"kt{i}", [128, S], BF16, nbuf=8) for i in range(4)]
    VA = [sb(es1, f"va{i}", [128, NT, 129], BF16, nbuf=8) for i in range(2)]
    VD = sb(es1, "vd", [128, NT, 257], BF16, nbuf=8)
    ksum = sb(es1, "ksum", [128, 2, 16], F32)
    ss1 = sb(es1, "ss1", [128, NT], F32, nbuf=NT)
    rstd1 = sb(es1, "rstd1", [128, NT], F32, nbuf=NT)

    P.add("dve", lambda e: e.memset(ksum.ap, 0.0), writes=[ksum.b])
    P.add("pool", lambda e: e.memset(VA[0].ap[:, :, 128:129], 1.0), writes=VA[0].bs)
    P.add("pool", lambda e: e.memset(VA[1].ap[:, :, 128:129], 1.0), writes=VA[1].bs)
    P.add("pool", lambda e: e.memset(VD.ap[:, :, 256:257], 1.0), writes=VD.bs)

    es1a = ExitStack()
    w_in_sb = sb(es1a, "w_in_sb", [128, 16, 1536], BF16, nbuf=9)
    xts = [sb(es1a, f"xt{i}", [128, D], F32) for i in range(2)]
    xns = [sb(es1a, f"xn{i}", [128, D], BF16) for i in range(2)]
    hTs = [sb(es1a, f"hT{i}", [128, 16, 512], BF16) for i in range(2)]

    w_in_v = w_in_c.rearrange("(k p) c -> p k c", p=128)
    for c in range(8):
        P.dma("pool", w_in_sb.ap[:, :, c * 128:(c + 1) * 128], w_in_v[:, :, c * 128:(c + 1) * 128],
              writes=[w_in_sb.bs[c]], buf=w_in_sb.bs[c])
    P.dma("pool", w_in_sb.ap[:, :, 1024:1536], w_in_v[:, :, 1024:1536], writes=[w_in_sb.bs[8]], buf=w_in_sb.bs[8])

    QSCALE = 128.0 ** -0.5
    pa_ctr = [0]

    def norm_T(g, tt):
        hT = hTs[g % 2]
        t = g * 4 + tt
        xt = xts[t % 2]
        xn = xns[t % 2]
        P.dma("sp", xt.ap, xb[t * 128:(t + 1) * 128, :], writes=[xt.b], buf=xt.b)
        P.add("act", lambda e: e.activation(out=xn.ap, in_=xt.ap, func=AF.Square, accum_out=ss1.ap[:, t:t + 1]),
              reads=[xt.b], writes=[xn.b, ss1.bs[t]])
        P.add("act", lambda e: e.activation(out=rstd1.ap[:, t:t + 1], in_=ss1.ap[:, t:t + 1], func=AF.Sqrt,
                                            scale=1.0 / D, bias=epsT.ap),
              reads=[ss1.bs[t], epsT.b], writes=[rstd1.bs[t]])
        P.add("dve", lambda e: e.reciprocal(out=rstd1.ap[:, t:t + 1], in_=rstd1.ap[:, t:t + 1]),
              reads=[rstd1.bs[t]], writes=[rstd1.bs[t]])
        P.add("dve", lambda e: e.tensor_scalar(out=xn.ap, in0=xt.ap, scalar1=rstd1.ap[:, t:t + 1], scalar2=None,
                                               op0=ALU.mult), reads=[xt.b, rstd1.bs[t]], writes=[xn.b])

    def norm_pe(g, tt):
        hT = hTs[g % 2]
        t = g * 4 + tt
        xn = xns[t % 2]
        for r in range(2):
            pt = PT[r]
            for k in range(8):
                P.add("pe", lambda e, pt=pt, k=k, r=r: e.transpose(
                    out=pt.ap[:, k * 128:(k + 1) * 128], in_=xn.ap[:, (r * 8 + k) * 128:(r * 8 + k + 1) * 128],
                    identity=ident), reads=[xn.b] + CONST, writes=[pt.b])
            P.add("dve", lambda e, pt=pt, r=r: e.tensor_tensor(
                out=hT.ap[:, r * 8:(r + 1) * 8, tt * 128:(tt + 1) * 128],
                in0=pt.ap.rearrange("p (k n) -> p k n", k=8),
                in1=wmix[:, r * 8:(r + 1) * 8].unsqueeze(2).to_broadcast([128, 8, 128]),
                op=ALU.mult), reads=[pt.b] + CONST, writes=[hT.b])

    def chunk_T(g, c):
        hT = hTs[g % 2]
        pa = PA[pa_ctr[0] % 2]
        pa_ctr[0] += 1
        for k in range(16):
            P.add("pe", lambda e, k=k: e.matmul(
                pa.ap, lhsT=w_in_sb.ap[:, k, c * 128:(c + 1) * 128], rhs=hT.ap[:, k, :],
                start=(k == 0), stop=(k == 15)), reads=[hT.b, w_in_sb.bs[c]], writes=[pa.b])
        cols = slice(g * 512, (g + 1) * 512)
        if c in (0, 1, 4, 5):
            dst = QT[c if c < 2 else c - 2]
            P.add("act", lambda e: e.activation(out=dst.ap[:, cols], in_=pa.ap, func=AF.Copy, scale=QSCALE),
                  reads=[pa.b], writes=[dst.bs[g]])
        elif c in (2, 3):
            dst = KT[c - 2]
            for hf in range(2):
                P.add("act", lambda e, hf=hf: e.activation(
                    out=dst.ap[:, g * 512 + hf * 256:g * 512 + (hf + 1) * 256],
                    in_=pa.ap[:, hf * 256:(hf + 1) * 256], func=AF.Identity,
                    accum_out=ksum.ap[:, c - 2, 2 * g + hf:2 * g + hf + 1]),
                    reads=[pa.b], writes=[dst.bs[g], ksum.b])
        else:
            dst = KT[c - 4]
            P.add("act", lambda e: e.activation(out=dst.ap[:, cols], in_=pa.ap, func=AF.Copy),
                  reads=[pa.b], writes=[dst.bs[g]])

    def chunk_V(g, tt):
        hT = hTs[g % 2]
        t = g * 4 + tt
        pa = PA[pa_ctr[0] % 2]
        pa_ctr[0] += 1
        for k in range(16):
            P.add("pe", lambda e, k=k: e.matmul(
                pa.ap, lhsT=hT.ap[:, k, tt * 128:(tt + 1) * 128], rhs=w_in_sb.ap[:, k, 1024:1536],
                start=(k == 0), stop=(k == 15)), reads=[hT.b, w_in_sb.bs[8]], writes=[pa.b])
        P.add("dve", lambda e: e.tensor_copy(out=VA[0].ap[:, t, 0:128], in_=pa.ap[:, 0:128]),
              reads=[pa.b], writes=[VA[0].bs[g]])
        P.add("dve", lambda e: e.tensor_copy(out=VA[1].ap[:, t, 0:128], in_=pa.ap[:, 128:256]),
              reads=[pa.b], writes=[VA[1].bs[g]])
        P.add("dve", lambda e: e.tensor_copy(out=VD.ap[:, t, 0:256], in_=pa.ap[:, 256:512]),
              reads=[pa.b], writes=[VD.bs[g]])

    norm_T(0, 0)
    norm_T(0, 1)
    norm_pe(0, 0)
    norm_T(0, 2)
    norm_pe(0, 1)
    norm_T(0, 3)
    norm_pe(0, 2)
    norm_pe(0, 3)
    for g in range(8):
        ci = 0
        for c in range(12):
            if c < 8:
                chunk_T(g, c)
            else:
                chunk_V(g, c - 8)
            ci += 1
            if g + 1 < 8:
                if ci in (1, 4, 7, 10):
                    norm_T(g + 1, (ci - 1) // 3)
                if ci in (3, 6, 9, 12):
                    norm_pe(g + 1, (ci - 3) // 3)
    es1a.close()

    if dbg and stage == 1:
        for nm, tt_ in (("qt0", QT[0]), ("kt1", KT[1]), ("qt3", QT[3])):
            d = nc.dram_tensor("dbg_" + nm, [128, S], BF16, kind="ExternalOutput").ap()
            b = Buf("dbg_" + nm)
            P.dma("sp", d, tt_.ap, reads=tt_.bs, writes=[b], buf=b)
            dbg_outs[nm] = b
        d = nc.dram_tensor("dbg_vd", [128, NT * 257], BF16, kind="ExternalOutput").ap()
        b = Buf("dbg_vd")
        P.dma("sp", d, VD.ap.rearrange("p t c -> p (t c)"), reads=VD.bs, writes=[b], buf=b)
        dbg_outs["vd"] = b
        d = nc.dram_tensor("dbg_ksum", [128, 32], F32, kind="ExternalOutput").ap()
        b = Buf("dbg_ksum")
        P.dma("sp", d, ksum.ap.rearrange("p a c -> p (a c)"), reads=[ksum.b], writes=[b], buf=b)
        dbg_outs["ksum"] = b
        P.emit(list(dbg_outs.values()))
        return nc

    es1b = ExitStack()
    RB = [sb(es1b, f"rb{h}", [128, S], BF16) for h in range(2)]
    lrb = sb(es1b, "lrb", [128, 2 * 16 * 128], BF16)
    pTs = [sb(es1b, f"pT{i}", [128, 512], BF16) for i in range(5)]
    O1n = sb(es1b, "o1n", [128, NT, 256], F32, nbuf=NT)
    mixed = sb(es1b, "mixed", [128, NT, 512], BF16, nbuf=NT)
    lam_t = sb(es1b, "lam_t", [128, 512], F32)
    subw_t = sb(es1b, "subw_t", [128, 256], F32)
    pastm_t = sb(es1b, "pastm_t", [128, 512], F32)
    small = sb(es1b, "small", [128, 64], F32)
    gall = sb(es1b, "gall", [128, 32, 16], F32)
    max8 = sb(es1b, "max8", [128, 32, 8], F32)
    selb = sb(es1b, "selb", [128, 32, 16], BF16)
    selb2 = sb(es1b, "selb2", [128, 32, 16], F32)
    km = sb(es1b, "km", [128, 4, 16], F32)
    kmb = sb(es1b, "kmb", [128, 2, 16], BF16)
    recs = sb(es1b, "recs", [128, 8], F32, nbuf=8)
    dtmp = sb(es1b, "dtmp", [128, 2, 256], F32, nbuf=2)
    djunk = sb(es1b, "djunk", [128, 256], F32)

    old1 = list(w_in_sb.bs)
    for tl in xts + xns + hTs:
        old1.append(tl.b)
    new1 = []
    for tl in RB + [lrb] + pTs + [O1n, mixed, lam_t, subw_t, pastm_t, small, gall, max8, selb, selb2, km, kmb, recs,
                                  dtmp, djunk]:
        new1 += tl.bs
        if tl.b not in new1:
            new1.append(tl.b)
    P.add("dve", lambda e: e.memset(bar_t.ap[:, 6:7], 0.0), reads=[], writes=old1 + [bar_t.b])
    P.add("dve", lambda e: e.memset(bar_t.ap[:, 7:8], 0.0), reads=[bar_t.b], writes=new1)

    P.dma("sp", lam_t.ap, lamv, writes=[lam_t.b], buf=lam_t.b)
    P.dma("sp", subw_t.ap, subw, writes=[subw_t.b], buf=subw_t.b)
    P.dma("sp", pastm_t.ap, pastm, writes=[pastm_t.b], buf=pastm_t.b)
    P.dma("sp", lrb.ap, lrb_d, writes=[lrb.b], buf=lrb.b)
    for h in range(2):
        P.add("pool", lambda e, h=h: e.memset(RB[h].ap, 0.0), writes=[RB[h].b])
        P.dma("sp", RB[h].ap[16:18, :], rbt_d, reads=[], writes=[RB[h].b], buf=RB[h].b)

    P.add("dve", lambda e: e.tensor_tensor(out=djunk.ap[:, 0:128], in0=lam_t.ap[:, 0:128], in1=lam_t.ap[:, 128:256],
                                           op=ALU.mult), reads=[lam_t.b], writes=[djunk.b])
    P.add("dve", lambda e: e.reduce_sum(out=small.ap[:, 0:1], in_=djunk.ap[:, 0:128], axis=AX.X),
          reads=[djunk.b], writes=[small.b])
    P.add("dve", lambda e: e.tensor_tensor(out=djunk.ap[:, 128:256], in0=lam_t.ap[:, 256:384], in1=lam_t.ap[:, 384:512],
                                           op=ALU.mult), reads=[lam_t.b], writes=[djunk.b])
    P.add("dve", lambda e: e.reduce_sum(out=small.ap[:, 1:2], in_=djunk.ap[:, 128:256], axis=AX.X),
          reads=[djunk.b], writes=[small.b])
    P.add("act", lambda e: e.activation(out=small.ap[:, 2:4], in_=small.ap[:, 0:2], func=AF.Exp),
          reads=[small.b], writes=[small.b])
    P.add("dve", lambda e: e.scalar_tensor_tensor(out=small.ap[:, 4:5], in0=small.ap[:, 3:4], scalar=-0.2,
                                                  in1=small.ap[:, 2:3], op0=ALU.add, op1=ALU.subtract),
          reads=[small.b], writes=[small.b])
    neglam = small.ap[:, 4:5]


    def _dbg_exit(tl, shape2):
        d = nc.dram_tensor("dbg_x", shape2, tl.ap.dtype, kind="ExternalOutput").ap()
        b = Buf("dbg_x")
        src = tl.ap
        if len(src.shape) == 3:
            src = src.rearrange("p a b -> p (a b)")
        P.dma("sp", d, src, reads=tl.bs + [tl.b], writes=[b], buf=b)
        P.emit([b])
        return nc

    if dbg and stage == 11:
        return _dbg_exit(small, [128, 64])
    kb_tab = [vecs_t.ap[:, 32 + 32 * i:64 + 32 * i] for i in range(3)]

    for h in range(2):
        P.add("dve", lambda e, h=h: e.tensor_scalar(out=km.ap[:, 0, :], in0=ksum.ap[:, h, :], scalar1=1.0 / 256,
                                                    scalar2=None, op0=ALU.mult), reads=[ksum.b], writes=[km.b])
        P.add("dve", lambda e: e.tensor_copy(out=kmb.ap[:, 0, :], in_=km.ap[:, 0, :]), reads=[km.b], writes=[kmb.b])
        P.add("dve", lambda e: e.tensor_copy(out=km.ap[:, 1, :], in_=kmb.ap[:, 0, :]), reads=[kmb.b], writes=[km.b])
        P.add("dve", lambda e: e.tensor_tensor(out=km.ap[:, 2, :], in0=km.ap[:, 0, :], in1=km.ap[:, 1, :],
                                               op=ALU.subtract), reads=[km.b], writes=[km.b])
        P.add("dve", lambda e: e.tensor_copy(out=kmb.ap[:, 1, :], in_=km.ap[:, 2, :]), reads=[km.b], writes=[kmb.b])
        pg = PA[2]
        for i in range(NT):
            for part in range(2):
                P.add("pe", lambda e, i=i, part=part, h=h: e.matmul(
                    pg.ap[:, i * 16:(i + 1) * 16], lhsT=QT[h].ap[:, i * 128:(i + 1) * 128], rhs=kmb.ap[:, part, :],
                    start=(part == 0), stop=(part == 1)), reads=[QT[h].bs[i // 4], kmb.b], writes=[pg.b])
        P.add("dve", lambda e: e.tensor_tensor(out=gall.ap.rearrange("p a b -> p (a b)"), in0=pg.ap, in1=pastm_t.ap,
                                               op=ALU.add), reads=[pg.b, pastm_t.b], writes=[gall.b])
        if dbg and stage == 12:
            return _dbg_exit(gall, [128, 512])
        for i in range(NT):
            P.add("dve", lambda e, i=i: e.max(out=max8.ap[:, i, :], in_=gall.ap[:, i, :]),
                  reads=[gall.b], writes=[max8.b])
        P.add("dve", lambda e: e.tensor_tensor(out=selb2.ap, in0=gall.ap,
                                               in1=max8.ap[:, :, 3:4].to_broadcast([128, 32, 16]),
                                               op=ALU.is_lt), reads=[gall.b, max8.b], writes=[selb2.b])
        P.add("dve", lambda e: e.tensor_scalar(out=selb.ap, in0=selb2.ap, scalar1=NEG, scalar2=None, op0=ALU.mult),
              reads=[selb2.b], writes=[selb.b])
        if dbg and stage == 13:
            return _dbg_exit(selb, [128, 512])
        for r in range(4):
            pt = PT[r % 2]
            for k in range(8):
                i = r * 8 + k
                P.add("pe", lambda e, pt=pt, k=k, i=i: e.transpose(
                    out=pt.ap[0:16, k * 128:(k + 1) * 128], in_=selb.ap[:, i, :], identity=ident),
                    reads=[selb.b] + CONST, writes=[pt.b])
            P.add("act", lambda e, pt=pt, r=r, h=h: e.activation(
                out=RB[h].ap[0:16, r * 1024:(r + 1) * 1024], in_=pt.ap[0:16, :], func=AF.Copy),
                reads=[pt.b], writes=[RB[h].b])

    if dbg and stage == 15:
        d = nc.dram_tensor("dbg_rb", [18, S], BF16, kind="ExternalOutput").ap()
        b = Buf("dbg_rb")
        P.dma("sp", d, RB[1].ap, reads=[RB[1].b], writes=[b], buf=b)
        d2 = nc.dram_tensor("dbg_gall", [128, 512], F32, kind="ExternalOutput").ap()
        b2 = Buf("dbg_gall")
        P.dma("sp", d2, gall.ap.rearrange("p a b -> p (a b)"), reads=[gall.b], writes=[b2], buf=b2)
        P.emit([b, b2])
        return nc

    sc_i = [0]

    tasks = []
    SCB = [PA[0], PA[1], PA[6], PA[7]]

    def attn_pass(qT, kT, V, dv1, kbias, rb, lrb_h, out_fn, qts=range(8)):
        Oacc = PA[2:6]
        for qt in qts:
            nkt = 4 * qt + 4
            for kt in range(nkt):
                j = kt - 4 * qt
                q0 = 128 * j if j > 0 else 0
                ps = SCB[sc_i[0] % 4]
                pT = pTs[sc_i[0] % 5]
                sc_i[0] += 1
                qcols = slice(qt * 512 + q0, (qt + 1) * 512)
                more = (rb is not None) or (j >= 0)

                def qk(ps=ps, kt=kt, q0=q0, qcols=qcols, more=more, j=j, qt=qt):
                    P.add("pe", lambda e: e.matmul(
                        ps.ap[:, q0:512], lhsT=kT.ap[:, kt * 128:(kt + 1) * 128], rhs=qT.ap[:, qcols],
                        start=True, stop=not more), reads=[kT.bs[kt // 4], qT.bs[qt]], writes=[ps.b])
                    if rb is not None:
                        n = kt // 2
                        P.add("pe", lambda e: e.matmul(
                            ps.ap[:, q0:512], lhsT=lrb.ap[:, (lrb_h * 16 + n) * 128:(lrb_h * 16 + n + 1) * 128],
                            rhs=rb.ap[:, qcols], start=False, stop=(j < 0)), reads=[rb.b, lrb.b], writes=[ps.b])
                    if j >= 0:
                        P.add("pe", lambda e: e.matmul(
                            ps.ap[:, q0:q0 + 128], lhsT=ident, rhs=tri, start=False, stop=True),
                            reads=CONST, writes=[ps.b])

                def rest(ps=ps, pT=pT, kt=kt, q0=q0, j=j, qt=qt):
                    m = j + 28
                    P.add("act", lambda e: e.activation(
                        out=pT.ap[:, q0:512], in_=ps.ap[:, q0:512], func=AF.Exp, bias=kbias[:, m:m + 1], scale=1.0),
                        reads=[ps.b] + CONST, writes=[pT.b])
                    for sub in range(q0 // 128, 4):
                        last = (kt == 4 * qt + sub)
                        P.add("pe", lambda e, sub=sub, last=last: e.matmul(
                            Oacc[sub].ap[:, 0:dv1], lhsT=pT.ap[:, sub * 128:(sub + 1) * 128], rhs=V.ap[:, kt, 0:dv1],
                            start=(kt == 0), stop=last), reads=[pT.b, V.bs[kt // 4]], writes=[Oacc[sub].b])
                        if last:
                            out_fn(qt * 4 + sub, Oacc[sub])

                tasks.append((qk, rest))

    DEPTH = 3

    def run_tasks():
        n = len(tasks)
        for i in range(min(DEPTH, n)):
            tasks[i][0]()
        for i in range(n):
            if i + DEPTH < n:
                tasks[i + DEPTH][0]()
            tasks[i][1]()
        tasks.clear()

    def moba_out(h):
        def f(tile, oa):
            rb_ = recs.bs[tile % 8]
            rc = recs.ap[:, tile % 8:tile % 8 + 1]
            P.add("dve", lambda e: e.reciprocal(out=rc, in_=oa.ap[:, 128:129]), reads=[oa.b], writes=[rb_])
            P.add("dve", lambda e: e.tensor_scalar(out=mixed.ap[:, tile, h * 128:(h + 1) * 128], in0=oa.ap[:, 0:128],
                                                   scalar1=rc, scalar2=None, op0=ALU.mult),
                  reads=[oa.b, rb_], writes=[mixed.bs[tile]])
        return f

    def diff1_out(tile, oa):
        rb_ = recs.bs[tile % 8]
        rc = recs.ap[:, tile % 8:tile % 8 + 1]
        P.add("dve", lambda e: e.reciprocal(out=rc, in_=oa.ap[:, 256:257]), reads=[oa.b], writes=[rb_])
        P.add("dve", lambda e: e.tensor_scalar(out=O1n.ap[:, tile, :], in0=oa.ap[:, 0:256], scalar1=rc, scalar2=None,
                                               op0=ALU.mult), reads=[oa.b, rb_], writes=[O1n.bs[tile]])

    def diff2_out(tile, oa):
        rb_ = recs.bs[tile % 8]
        rc = recs.ap[:, tile % 8:tile % 8 + 1]
        P.add("dve", lambda e: e.reciprocal(out=rc, in_=oa.ap[:, 256:257]), reads=[oa.b], writes=[rb_])
        P.add("dve", lambda e: e.tensor_tensor(out=rc, in0=rc, in1=neglam, op=ALU.mult),
              reads=[rb_, small.b], writes=[rb_])
        P.add("dve", lambda e: e.scalar_tensor_tensor(out=O1n.ap[:, tile, :], in0=oa.ap[:, 0:256], scalar=rc,
                                                      in1=O1n.ap[:, tile, :], op0=ALU.mult, op1=ALU.add),
              reads=[oa.b, rb_, O1n.bs[tile]], writes=[O1n.bs[tile]])
        if tile % 8 == 7:
            t0 = tile - 7
            sb_ = dtmp.bs[(tile // 8) % 2]
            sq = dtmp.ap[:, (tile // 8) % 2, 0:8]
            rs = dtmp.ap[:, (tile // 8) % 2, 8:16]
            for t in range(t0, t0 + 8):
                P.add("act", lambda e, t=t: e.activation(out=djunk.ap, in_=O1n.ap[:, t, :], func=AF.Square,
                                                         accum_out=sq[:, t - t0:t - t0 + 1]),
                      reads=[O1n.bs[t]], writes=[djunk.b, sb_])
            P.add("act", lambda e: e.activation(out=rs, in_=sq, func=AF.Ln, scale=1.0 / 256, bias=epsT.ap),
                  reads=[sb_, epsT.b], writes=[sb_])
            P.add("act", lambda e: e.activation(out=rs, in_=rs, func=AF.Exp, scale=-0.5), reads=[sb_], writes=[sb_])
            P.add("dve", lambda e: e.tensor_scalar(out=rs, in0=rs, scalar1=0.8, scalar2=None, op0=ALU.mult),
                  reads=[sb_], writes=[sb_])
            for t in range(t0, t0 + 8):
                P.add("dve", lambda e, t=t: e.scalar_tensor_tensor(
                    out=mixed.ap[:, t, 256:512], in0=O1n.ap[:, t, :], scalar=rs[:, t - t0:t - t0 + 1],
                    in1=subw_t.ap, op0=ALU.mult, op1=ALU.mult),
                    reads=[O1n.bs[t], sb_, subw_t.b], writes=[mixed.bs[t]])

    def exchange(j):
        P.dma("sp", bounce[j].ap().rearrange("(t p) f -> p t f", p=128), mixed.ap[:, j * 8:(j + 1) * 8, :],
              reads=mixed.bs[j * 8:(j + 1) * 8], writes=[bounce_b[j]], buf=bounce_b[j])
        P.add("pool", lambda e: e.collective_compute(
            "AllGather", ALU.bypass, replica_groups=[[0, 1, 2, 3], [4, 5, 6, 7]],
            ins=[bounce[j].ap().opt()], outs=[agbig.ap()[j * 4096:(j + 1) * 4096, :].opt()]),
            reads=[bounce_b[j]], writes=[agout_b[j]], kind="cc", buf=agout_b[j])

    passes = [(QT[0], KT[0], VA[0], 129, kb_tab[0], RB[0], 0, moba_out(0)),
              (QT[1], KT[1], VA[1], 129, kb_tab[1], RB[1], 1, moba_out(1)),
              (QT[2], KT[2], VD, 257, kb_tab[2], None, 0, diff1_out),
              (QT[3], KT[3], VD, 257, kb_tab[2], None, 0, diff2_out)]
    if not (dbg and stage == 17):
        for j in range(4):
            for pz in passes:
                attn_pass(*pz, qts=(2 * j, 2 * j + 1))
            if not (dbg and stage == 2):
                tasks.append((lambda: None, lambda j=j: exchange(j)))
        run_tasks()
    if dbg and stage == 17:
        attn_pass(QT[0], KT[0], VA[0], 129, kb_tab[0], RB[0], 0, moba_out(0))
        run_tasks()
        d = nc.dram_tensor("dbg_mixed", [128, NT * 512], BF16, kind="ExternalOutput").ap()
        b = Buf("dbg_mixed")
        P.dma("sp", d, mixed.ap.rearrange("p t c -> p (t c)"), reads=mixed.bs, writes=[b], buf=b)
        P.emit([b])
        return nc

    if dbg and stage == 2:
        d = nc.dram_tensor("dbg_mixed", [128, NT * 512], BF16, kind="ExternalOutput").ap()
        b = Buf("dbg_mixed")
        P.dma("sp", d, mixed.ap.rearrange("p t c -> p (t c)"), reads=mixed.bs, writes=[b], buf=b)
        P.emit([b])
        return nc

    es1b.close()
    es1.close()

    allb = Buf("phase_barrier")

    def barrier():
        for en in ("pe", "act", "dve", "pool", "sp"):
            pass

    es2 = ExitStack()
    x1 = sb(es2, "x1", [128, 8, D], F32, nbuf=8)
    ss2 = sb(es2, "ss2", [128, 16], F32, nbuf=16)
    h2T = sb(es2, "h2T", [128, 16, 1024], BF16, nbuf=8)
    xn2 = [sb(es2, f"xn2_{i}", [128, D], BF16) for i in range(2)]
    selI = cbf_t.ap[:, 256:768]

    es2a = ExitStack()
    mixT = sb(es2a, "mixT", [128, 16, 1024], BF16, nbuf=16)
    agb = [sb(es2a, f"agb{i}", [128, 32, 512], BF16) for i in range(1)]
    wob = [sb(es2a, f"wob{i}", [128, 16, 512], BF16) for i in range(2)]

    p1_bufs = []
    for tl in QT + KT + VA + [VD, ksum, ss1, rstd1, mixed, O1n, lam_t, subw_t, pastm_t, small, gall, max8, selb, selb2,
                              km, kmb, recs, dtmp, djunk, lrb] + RB + pTs + xts + xns + hTs + [w_in_sb]:
        p1_bufs += tl.bs
        if tl.b not in p1_bufs:
            p1_bufs.append(tl.b)
    P.add("dve", lambda e: e.memset(bar_t.ap[:, 0:1], 0.0), reads=[], writes=p1_bufs + [bar_t.b])
    new_bufs = x1.bs + ss2.bs + mixT.bs + h2T.bs + [t_.b for t_ in agb + wob + xn2]
    P.add("dve", lambda e: e.memset(bar_t.ap[:, 1:2], 0.0), reads=[bar_t.b], writes=new_bufs)

    for i in range(8):
        P.dma("sp", x1.ap[:, i, :], xres[i * 128:(i + 1) * 128, :], writes=[x1.bs[i]], buf=x1.bs[i])

    idx_t = sb(es2a, "idx_t", [128, 32], mybir.dt.int32)
    P.add("dve", lambda e: e.memset(bar_t.ap[:, 2:3], 0.0), reads=[bar_t.b], writes=[idx_t.b])
    P.dma("sp", idx_t.ap, idxg_d, writes=[idx_t.b], buf=idx_t.b)
    ab = agb[0]
    for ri in range(32):
        P.add("pool", lambda e, ri=ri: e.indirect_dma_start(
            out=ab.ap[:, ri, :], out_offset=None, in_=agbig.ap(),
            in_offset=bass.IndirectOffsetOnAxis(ap=idx_t.ap[:, ri:ri + 1], axis=0)),
            reads=[idx_t.b] + agout_b, writes=[ab.b], kind="d", buf=ab.b)
    ev = 0
    for r in range(4):
        for fc in range(4):
            pt = PT[ev % 2]
            for i in range(8):
                P.add("pe", lambda e, pt=pt, r=r, fc=fc, i=i: e.transpose(
                    out=pt.ap[:, i * 128:(i + 1) * 128], in_=ab.ap[:, r * 8 + i, fc * 128:(fc + 1) * 128],
                    identity=ident), reads=[ab.b] + CONST, writes=[pt.b])
            if ev % 2 == 0:
                P.add("act", lambda e, pt=pt, r=r, fc=fc: e.activation(
                    out=mixT.ap[:, r * 4 + fc, :], in_=pt.ap, func=AF.Copy), reads=[pt.b],
                    writes=[mixT.bs[r * 4 + fc]])
            else:
                P.add("dve", lambda e, pt=pt, r=r, fc=fc: e.tensor_copy(out=mixT.ap[:, r * 4 + fc, :], in_=pt.ap),
                      reads=[pt.b], writes=[mixT.bs[r * 4 + fc]])
            ev += 1

    def rms_rstd(ssap, ssb, n):
        P.add("act", lambda e: e.activation(out=ssap, in_=ssap, func=AF.Sqrt, scale=1.0 / n, bias=epsT.ap),
              reads=[ssb, epsT.b], writes=[ssb])
        P.add("dve", lambda e: e.reciprocal(out=ssap, in_=ssap), reads=[ssb], writes=[ssb])

    def norm2_pre(i):
        xn = xn2[i % 2]
        ssap = ss2.ap[:, i:i + 1]
        P.add("act", lambda e: e.activation(out=xn.ap, in_=x1.ap[:, i, :], func=AF.Square, accum_out=ssap),
              reads=[x1.bs[i]], writes=[xn.b, ss2.bs[i]])
        rms_rstd(ssap, ss2.bs[i], D)
        P.add("dve", lambda e: e.tensor_scalar(out=xn.ap, in0=x1.ap[:, i, :], scalar1=ssap, scalar2=None,
                                               op0=ALU.mult), reads=[x1.bs[i], ss2.bs[i]], writes=[xn.b])

    def norm2_pe(i):
        xn = xn2[i % 2]
        for r in range(2):
            pt = PT[r]
            for k in range(8):
                P.add("pe", lambda e, pt=pt, k=k, r=r: e.transpose(
                    out=pt.ap[:, k * 128:(k + 1) * 128], in_=xn.ap[:, (r * 8 + k) * 128:(r * 8 + k + 1) * 128],
                    identity=ident), reads=[xn.b] + CONST, writes=[pt.b])
            P.add("dve", lambda e, pt=pt, r=r: e.tensor_tensor(
                out=h2T.ap[:, r * 8:(r + 1) * 8, i * 128:(i + 1) * 128],
                in0=pt.ap.rearrange("p (k n) -> p k n", k=8),
                in1=wffn[:, r * 8:(r + 1) * 8].unsqueeze(2).to_broadcast([128, 8, 128]),
                op=ALU.mult), reads=[pt.b] + CONST, writes=[h2T.bs[i]])

    pa_i = 0
    w_out_v = w_out_p.rearrange("(k p) c -> p k c", p=128)
    for dc in range(4):
        wb = wob[dc % 2]
        P.dma("pool", wb.ap, w_out_v[:, :, dc * 512:(dc + 1) * 512], writes=[wb.b], buf=wb.b)
        for i in range(8):
            pa = PA[pa_i % 2]
            pa_i += 1
            for k in range(16):
                P.add("pe", lambda e, pa=pa, k=k, i=i, wb=wb: e.matmul(
                    pa.ap, lhsT=mixT.ap[:, k, i * 128:(i + 1) * 128], rhs=wb.ap[:, k, :],
                    start=(k == 0), stop=(k == 15)), reads=[mixT.bs[k], wb.b], writes=[pa.b])
            P.add("dve", lambda e, pa=pa, i=i, dc=dc: e.tensor_tensor(
                out=x1.ap[:, i, dc * 512:(dc + 1) * 512], in0=pa.ap, in1=x1.ap[:, i, dc * 512:(dc + 1) * 512],
                op=ALU.add), reads=[pa.b, x1.bs[i]], writes=[x1.bs[i]])
            if dc == 3:
                if i >= 2:
                    norm2_pe(i - 2)
                norm2_pre(i)
    norm2_pe(6)
    norm2_pe(7)
    es2a.close()

    if dbg and stage == 3:
        d = nc.dram_tensor("dbg_x1", [128, 8 * D], F32, kind="ExternalOutput").ap()
        b = Buf("dbg_x1")
        P.dma("sp", d, x1.ap.rearrange("p t c -> p (t c)"), reads=x1.bs, writes=[b], buf=b)
        P.emit([b])
        return nc

    es2b = ExitStack()
    wr_sb = sb(es2b, "wr_sb", [128, 16, 36], BF16)
    rb_sb = sb(es2b, "rb_sb", [128, 36], F32)
    lg = sb(es2b, "lg", [128, 8, 36], F32, nbuf=8)
    rt = sb(es2b, "rt", [128, 8, 64], F32, nbuf=8)
    comb = sb(es2b, "comb", [128, 8, 32], F32, nbuf=8)
    es2w = ExitStack()
    wg = [sb(es2w, f"wg{i}", [128, 16, DE], BF16) for i in range(2)]
    wu = [sb(es2w, f"wu{i}", [128, 16, DE], BF16) for i in range(2)]
    wd = [sb(es2w, "wd0", [128, 4, D], BF16)] * 2
    sa = [sb(es2w, f"sa{i}", [128, 512], BF16) for i in range(2)]
    actT = [sb(es2w, f"actT{i}", [128, 4, 512], BF16, nbuf=4) for i in range(2)]
    new_bufs = lg.bs + rt.bs + comb.bs + [wr_sb.b, rb_sb.b, lg.b, rt.b]
    for tl in wg + wu + wd + sa:
        new_bufs.append(tl.b)
    for tl in actT:
        new_bufs += tl.bs
    old = mixT.bs + [t_.b for t_ in agb + wob]
    P.add("dve", lambda e: e.memset(bar_t.ap[:, 2:3], 0.0), reads=[], writes=old + [bar_t.b])
    P.add("dve", lambda e: e.memset(bar_t.ap[:, 3:4], 0.0), reads=[bar_t.b], writes=new_bufs)

    P.dma("pool", wr_sb.ap, w_r.rearrange("(k p) c -> p k c", p=128), writes=[wr_sb.b], buf=wr_sb.b)
    P.dma("sp", rb_sb.ap, rbias, writes=[rb_sb.b], buf=rb_sb.b)

    def load_expert(e_):
        P.dma("pool", wg[e_ % 2].ap, w_gate[e_].rearrange("(k p) f -> p k f", p=128), writes=[wg[e_ % 2].b],
              buf=wg[e_ % 2].b)
        P.dma("pool", wu[e_ % 2].ap, w_up[e_].rearrange("(k p) f -> p k f", p=128), writes=[wu[e_ % 2].b],
              buf=wu[e_ % 2].b)

    def load_down(e_):
        P.dma("pool", wd[0].ap, w_down[e_].rearrange("(k p) d -> p k d", p=128), writes=[wd[0].b], buf=wd[0].b)

    load_expert(0)
    load_down(0)
    load_expert(1)

    Lb = lg.b
    Rb = rt.b
    L3 = lg.ap
    R3 = rt.ap
    for i in range(8):
        pa = PA[2 + i % 2]
        for k in range(16):
            P.add("pe", lambda e, pa=pa, k=k, i=i: e.matmul(
                pa.ap[:, 0:36], lhsT=h2T.ap[:, k, i * 128:(i + 1) * 128], rhs=wr_sb.ap[:, k, :],
                start=(k == 0), stop=(k == 15)), reads=[h2T.bs[i], wr_sb.b], writes=[pa.b])
        P.add("dve", lambda e, pa=pa, i=i: e.tensor_tensor(out=L3[:, i, :], in0=pa.ap[:, 0:36], in1=rb_sb.ap,
                                                           op=ALU.add), reads=[pa.b, rb_sb.b], writes=[Lb])

    def bc(ap, w):
        return ap.to_broadcast([128, 8, w])

    def tt(out, in0, in1, op, rd=(), wr=None):
        P.add("dve", lambda e: e.tensor_tensor(out=out, in0=in0, in1=in1, op=op), reads=[Lb, Rb] + list(rd),
              writes=[Rb] if wr is None else wr)

    tt(R3[:, :, 8:10], L3[:, :, 0:2], L3[:, :, 2:4], ALU.max)
    tt(R3[:, :, 0:1], R3[:, :, 8:9], R3[:, :, 9:10], ALU.max)
    tt(R3[:, :, 4:8], L3[:, :, 0:4], bc(R3[:, :, 0:1], 4), ALU.is_ge)
    tt(R3[:, :, 8:12], L3[:, :, 0:4], bc(R3[:, :, 0:1], 4), ALU.subtract)
    P.add("act", lambda e: e.activation(out=R3[:, :, 8:12], in_=R3[:, :, 8:12], func=AF.Exp), reads=[Rb], writes=[Rb])
    tt(R3[:, :, 12:14], R3[:, :, 8:10], R3[:, :, 10:12], ALU.add)
    tt(R3[:, :, 1:2], R3[:, :, 12:13], R3[:, :, 13:14], ALU.add)
    tt(R3[:, :, 16:24], L3[:, :, 4:12], bc(R3[:, :, 4:5], 8), ALU.mult)
    for g_ in range(1, 4):
        tt(R3[:, :, 32:40], L3[:, :, 4 + 8 * g_:12 + 8 * g_], bc(R3[:, :, 4 + g_:5 + g_], 8), ALU.mult)
        tt(R3[:, :, 16:24], R3[:, :, 16:24], R3[:, :, 32:40], ALU.add)
    tt(R3[:, :, 24:28], R3[:, :, 16:20], R3[:, :, 20:24], ALU.max)
    tt(R3[:, :, 28:30], R3[:, :, 24:26], R3[:, :, 26:28], ALU.max)
    tt(R3[:, :, 2:3], R3[:, :, 28:29], R3[:, :, 29:30], ALU.max)
    tt(R3[:, :, 24:32], R3[:, :, 16:24], bc(R3[:, :, 2:3], 8), ALU.is_ge)
    P.add("dve", lambda e: e.scalar_tensor_tensor(out=R3[:, :, 32:40], in0=R3[:, :, 24:32], scalar=-1e30,
                                                  in1=R3[:, :, 16:24], op0=ALU.mult, op1=ALU.add),
          reads=[Rb], writes=[Rb])
    tt(R3[:, :, 40:44], R3[:, :, 32:36], R3[:, :, 36:40], ALU.max)
    tt(R3[:, :, 44:46], R3[:, :, 40:42], R3[:, :, 42:44], ALU.max)
    tt(R3[:, :, 3:4], R3[:, :, 44:45], R3[:, :, 45:46], ALU.max)
    tt(R3[:, :, 40:48], R3[:, :, 32:40], bc(R3[:, :, 3:4], 8), ALU.is_ge)
    tt(R3[:, :, 48:49], R3[:, :, 3:4], R3[:, :, 2:3], ALU.subtract)
    P.add("act", lambda e: e.activation(out=R3[:, :, 49:50], in_=R3[:, :, 48:49], func=AF.Exp), reads=[Rb], writes=[Rb])
    P.add("dve", lambda e: e.tensor_scalar(out=R3[:, :, 50:51], in0=R3[:, :, 49:50], scalar1=1.0, scalar2=None,
                                           op0=ALU.add), reads=[Rb], writes=[Rb])
    tt(R3[:, :, 50:51], R3[:, :, 50:51], R3[:, :, 1:2], ALU.mult)
    P.add("dve", lambda e: e.reciprocal(out=R3[:, :, 51:52], in_=R3[:, :, 50:51]), reads=[Rb], writes=[Rb])
    tt(R3[:, :, 52:53], R3[:, :, 51:52], R3[:, :, 49:50], ALU.mult)
    tt(R3[:, :, 56:64], R3[:, :, 24:32], bc(R3[:, :, 51:52], 8), ALU.mult)
    tt(R3[:, :, 32:40], R3[:, :, 40:48], bc(R3[:, :, 52:53], 8), ALU.mult)
    tt(R3[:, :, 56:64], R3[:, :, 56:64], R3[:, :, 32:40], ALU.add)
    for g_ in range(4):
        tt(comb.ap[:, :, 8 * g_:8 * g_ + 8], R3[:, :, 56:64], bc(R3[:, :, 4 + g_:5 + g_], 8), ALU.mult,
           wr=comb.bs)

    it = 0
    for ex in range(N_EXP):
        wgb, wub, wdb = wg[ex % 2], wu[ex % 2], wd[ex % 2]
        for tg in range(2):
            aT = actT[tg]
            for fc in range(4):
                pa_g = PA[0]
                pa_u = PA[1]
                s_ = sa[it % 2]
                it += 1
                for k in range(16):
                    P.add("pe", lambda e, k=k, fc=fc, tg=tg, wgb=wgb: e.matmul(
                        pa_g.ap, lhsT=wgb.ap[:, k, fc * 128:(fc + 1) * 128], rhs=h2T.ap[:, k, tg * 512:(tg + 1) * 512],
                        start=(k == 0), stop=(k == 15)), reads=[wgb.b] + h2T.bs[tg * 4:tg * 4 + 4], writes=[pa_g.b])
                for k in range(16):
                    P.add("pe", lambda e, k=k, fc=fc, tg=tg, wub=wub: e.matmul(
                        pa_u.ap, lhsT=wub.ap[:, k, fc * 128:(fc + 1) * 128], rhs=h2T.ap[:, k, tg * 512:(tg + 1) * 512],
                        start=(k == 0), stop=(k == 15)), reads=[wub.b] + h2T.bs[tg * 4:tg * 4 + 4], writes=[pa_u.b])
                P.add("act", lambda e, s_=s_: e.activation(out=s_.ap, in_=pa_g.ap, func=AF.Silu),
                      reads=[pa_g.b], writes=[s_.b])
                P.add("dve", lambda e, s_=s_, aT=aT, fc=fc: e.tensor_tensor(
                    out=aT.ap[:, fc, :], in0=pa_u.ap, in1=s_.ap, op=ALU.mult), reads=[pa_u.b, s_.b], writes=[aT.bs[fc]])
            for ii in range(4):
                i = tg * 4 + ii
                for dc in range(4):
                    pa = PA[2 + (ii * 4 + dc) % 4]
                    for fc in range(4):
                        P.add("pe", lambda e, pa=pa, fc=fc, ii=ii, dc=dc, aT=aT, wdb=wdb: e.matmul(
                            pa.ap, lhsT=aT.ap[:, fc, ii * 128:(ii + 1) * 128], rhs=wdb.ap[:, fc, dc * 512:(dc + 1) * 512],
                            start=(fc == 0), stop=(fc == 3)), reads=[aT.bs[fc], wdb.b], writes=[pa.b])
                    P.add("dve", lambda e, pa=pa, i=i, dc=dc, ex=ex: e.scalar_tensor_tensor(
                        out=x1.ap[:, i, dc * 512:(dc + 1) * 512], in0=pa.ap, scalar=comb.ap[:, i, ex:ex + 1],
                        in1=x1.ap[:, i, dc * 512:(dc + 1) * 512], op0=ALU.mult, op1=ALU.add),
                        reads=[pa.b, x1.bs[i], comb.bs[i]], writes=[x1.bs[i]])
        if ex + 1 < N_EXP:
            load_down(ex + 1)
        if ex + 2 < N_EXP:
            load_expert(ex + 2)

    oldw = [wg[0].b, wg[1].b, wu[0].b, wu[1].b, wd[0].b, sa[0].b, sa[1].b] + actT[0].bs + actT[1].bs
    P.add("dve", lambda e: e.memset(bar_t.ap[:, 4:5], 0.0), reads=[], writes=oldw + [bar_t.b])
    es2w.close()
    es2c = ExitStack()
    wfin_t = sb(es2c, "wfin_t", [128, D], F32)
    P.add("dve", lambda e: e.memset(bar_t.ap[:, 5:6], 0.0), reads=[bar_t.b], writes=[wfin_t.b])
    P.dma("sp", wfin_t.ap, wfin, writes=[wfin_t.b], buf=wfin_t.b)
    for i in range(8):
        xn = xn2[i % 2]
        ssap = ss2.ap[:, 8 + i:9 + i]
        P.add("act", lambda e, xn=xn, i=i, ssap=ssap: e.activation(out=xn.ap, in_=x1.ap[:, i, :], func=AF.Square,
                                                                  accum_out=ssap),
              reads=[x1.bs[i]], writes=[xn.b, ss2.bs[8 + i]])
        rms_rstd(ssap, ss2.bs[8 + i], D)
        P.add("dve", lambda e, i=i, ssap=ssap: e.scalar_tensor_tensor(
            out=x1.ap[:, i, :], in0=x1.ap[:, i, :], scalar=ssap, in1=wfin_t.ap, op0=ALU.mult, op1=ALU.mult),
            reads=[x1.bs[i], ss2.bs[8 + i], wfin_t.b], writes=[x1.bs[i]])
        P.dma("sp", out[i * 128:(i + 1) * 128, :], x1.ap[:, i, :], reads=[x1.bs[i]], writes=[out_b], buf=out_b)
    P.emit([out_b])
    return nc


def _bf(a):
    return np.asarray(a, dtype=np.float32).astype(ml_dtypes.bfloat16)


def _consts(g):
    slopes_a = [2.0 ** -(i + 1) for i in range(8)]
    slopes_b = [2.0 ** (-2 * (i + 1)) for i in range(4)]
    p = np.arange(128, dtype=np.float64)[:, None]
    m = np.arange(32, dtype=np.float64)[None, :]
    kb = []
    for h in range(2):
        kb.append(slopes_a[2 * g + h] * (p + 128.0 * (m - 28)))
    kb.append(slopes_b[g] * (p + 128.0 * (m - 28) - 256.0))
    kb = np.concatenate(kb, axis=1).astype(np.float32)
    ident = np.eye(128, dtype=np.float32)
    kk = np.arange(128)[:, None]
    qq = np.arange(128)[None, :]
    tri = np.where(kk > qq, NEG, 0.0).astype(np.float32)
    selI = np.zeros((128, 4, 128), np.float32)
    selI[:, g, :] = ident
    cbf = _bf(np.concatenate([ident, tri, selI.reshape(128, 512)], axis=1))
    lrb = np.zeros((128, 2, 16, 128), np.float32)
    for h in range(2):
        for n in range(16):
            lrb[n, h, n, :] = 1.0
        lrb[16:18, h, :, :] = -slopes_a[2 * g + h]
    lrb = _bf(lrb.reshape(128, 2 * 16 * 128))
    t = np.arange(S) % 512
    rbt = _bf(np.stack([t % 256, t - t % 256]).astype(np.float32))
    pm = np.zeros((32, 16), np.float32)
    for i in range(32):
        j = i // 2
        pm[i, j] = 1e30
        pm[i, j + 1:] = -1e30
    pastm = np.broadcast_to(pm.reshape(1, 512), (128, 512)).copy()
    selE = np.zeros((32, N_EXP, 128), np.float32)
    for e in range(N_EXP):
        selE[e, e, :] = 1.0
    selE = _bf(selE.reshape(32, N_EXP * 128))
    return kb, cbf, lrb, rbt, pastm, selE


def make_in_maps(x, norm_mix_w, w_in, lambda_q1, lambda_k1, lambda_q2, lambda_k2, diff_subln_w,
                 w_out, norm_ffn_w, w_router_group, b_router_group, w_router_expert, b_router_expert,
                 w_gate, w_up, w_down, norm_final_w):
    f = lambda a: np.ascontiguousarray(np.asarray(a, dtype=np.float32))
    x = f(x); w_in = f(w_in)[0]; w_out = f(w_out)[0]
    w_gate = f(w_gate)[0]; w_up = f(w_up)[0]; w_down = f(w_down)[0]
    wmix = f(norm_mix_w)[0].reshape(16, 128).T
    wffn = f(norm_ffn_w)[0].reshape(16, 128).T
    lam = np.concatenate([f(lambda_q1)[0], f(lambda_k1)[0], f(lambda_q2)[0], f(lambda_k2)[0]])
    lamv = np.ascontiguousarray(np.broadcast_to(lam[None, :], (128, 512)))
    subw = np.ascontiguousarray(np.broadcast_to(f(diff_subln_w)[0][None, :], (128, 256)))
    rbias = np.concatenate([f(b_router_group)[0], f(b_router_expert)[0]])
    rbias = np.ascontiguousarray(np.broadcast_to(rbias[None, :], (128, 36)))
    wfin = np.ascontiguousarray(np.broadcast_to(f(norm_final_w)[None, :], (128, D)))
    w_r = np.ascontiguousarray(np.concatenate([f(w_router_group)[0], f(w_router_expert)[0]], axis=1))
    in_maps = []
    for c in range(8):
        b, g = c // 4, c % 4
        cols = []
        for h in (2 * g, 2 * g + 1):
            cols.append(np.arange(h * 128, (h + 1) * 128))
        for h in (2 * g, 2 * g + 1):
            cols.append(1024 + np.arange(h * 128, (h + 1) * 128))
        cols.append(3072 + g * 256 + np.arange(256))
        cols.append(4096 + g * 256 + np.arange(256))
        for h in (2 * g, 2 * g + 1):
            cols.append(2048 + np.arange(h * 128, (h + 1) * 128))
        cols.append(5120 + g * 256 + np.arange(256))
        cols = np.concatenate(cols)
        w_in_c = np.ascontiguousarray(w_in[:, cols])
        rows = np.concatenate([np.concatenate([np.arange(256 * r, 256 * r + 256), 1024 + np.arange(256 * r, 256 * r + 256)])
                               for r in range(4)])
        kb, cbf, lrb, rbt, pastm, selE = _consts(g)
        ri = np.arange(32)[None, :]
        idxg = (g * 4096 + (ri // 8) * 1024 + (ri % 8) * 128 + np.arange(128)[:, None]).astype(np.int32)
        vecs = np.ascontiguousarray(np.concatenate([wmix, wffn, kb], axis=1).astype(np.float32))
        in_maps.append({
            "xb": x[b], "xres": np.ascontiguousarray(x[b, g * 1024:(g + 1) * 1024]),
            "w_in_c": w_in_c, "w_out_p": np.ascontiguousarray(w_out[rows]), "w_r": w_r,
            "w_gate": w_gate, "w_up": w_up, "w_down": w_down,
            "vecs": vecs, "lamv": lamv, "subw": subw, "rbias": rbias, "wfin": wfin, "pastm": pastm,
            "cbf": cbf, "lrb": lrb, "rbt": rbt, "idxg": np.ascontiguousarray(idxg),
        })
    return in_maps


_NC = None


def kernel(**inputs):
    global _NC
    in_maps = make_in_maps(**inputs)
    if _NC is None:
        _NC = build()
    res = run_bass_kernel_spmd(_NC, in_maps, core_ids=list(range(8)))
    outp = np.empty((2, S, D), np.float32)
    for c in range(8):
        b, g = c // 4, c % 4
        outp[b, g * 1024:(g + 1) * 1024] = res.results[c]["out"]
    return outp
```

```python
import numpy as np
import ml_dtypes
from contextlib import ExitStack
import concourse.bass as bass
import concourse.mybir as mybir
from concourse.bass_utils import run_bass_kernel_spmd

F32 = mybir.dt.float32
BF16 = mybir.dt.bfloat16
AF = mybir.ActivationFunctionType
ALU = mybir.AluOpType
AX = mybir.AxisListType

D = 2048
S = 4096
NT = 32
EPS = 1e-6
NEG = -30000.0
N_EXP = 32
DE = 512
SEM_BLK = 2000


class Buf:
    __slots__ = ("name", "last_w", "readers", "dma_sem", "dma_cnt")

    def __init__(self, name):
        self.name = name
        self.last_w = None
        self.readers = {}
        self.dma_sem = None
        self.dma_cnt = 0


class Op:
    __slots__ = ("eng", "fn", "deps", "kind", "buf", "dma_val", "sig", "semidx", "idx")


class Prog:
    def __init__(self, nc):
        self.nc = nc
        self.ops = []
        self.engs = {"pe": nc.tensor, "act": nc.scalar, "dve": nc.vector, "pool": nc.gpsimd, "sp": nc.sync}

    def add(self, eng, fn, reads=(), writes=(), kind="c", buf=None):
        op = Op()
        op.eng, op.fn, op.kind, op.buf = eng, fn, kind, buf
        op.sig = False
        op.semidx = 0
        op.idx = len(self.ops)
        deps = set()
        for b in reads:
            if b.last_w is not None:
                deps.add(b.last_w)
        for b in writes:
            if b.last_w is not None:
                deps.add(b.last_w)
            for r in b.readers.values():
                if isinstance(r, list):
                    deps.update(r)
                else:
                    deps.add(r)
        deps.discard(op.idx)
        op.deps = deps
        key = eng if kind == "c" else "dma"
        for b in reads:
            if key == "dma":
                b.readers.setdefault("dma", []).append(op.idx)
            else:
                b.readers[key] = op.idx
        for b in writes:
            b.last_w = op.idx
            b.readers = {}
        if kind == "d":
            buf.dma_cnt += 1
            op.dma_val = 16 * buf.dma_cnt
        elif kind == "cc":
            buf.dma_cnt += 1
            op.dma_val = buf.dma_cnt
        self.ops.append(op)
        return op

    def dma(self, eng, out, in_, reads=(), writes=(), buf=None):
        return self.add(eng, lambda e: e.dma_start(out=out, in_=in_), reads, writes, kind="d", buf=buf)

    def emit(self, final_bufs):
        nc = self.nc
        ops = self.ops
        for op in ops:
            for d in op.deps:
                p = ops[d]
                if p.kind == "c":
                    if p.eng == op.eng and op.kind == "c" and op.eng == "pe":
                        continue
                    p.sig = True
        cnt = {e: 0 for e in self.engs}
        for op in ops:
            if op.kind == "c" and op.sig:
                cnt[op.eng] += 1
                op.semidx = cnt[op.eng]
        sems = {}

        def eng_sem(e, idx):
            blk = (idx - 1) // SEM_BLK
            k = (e, blk)
            if k not in sems:
                sems[k] = nc.alloc_semaphore(f"s_{e}_{blk}")
            return sems[k], (idx - 1) % SEM_BLK + 1

        def buf_sem(b):
            if b.dma_sem is None:
                b.dma_sem = nc.alloc_semaphore(f"d_{b.name}")
            return b.dma_sem

        waited_eng = {e: {p: 0 for p in self.engs} for e in self.engs}
        waited_buf = {e: {} for e in self.engs}
        for op in ops:
            e = self.engs[op.eng]
            need_eng = {}
            need_buf = {}
            for d in op.deps:
                p = ops[d]
                if p.kind == "c":
                    if p.eng == op.eng and op.kind == "c" and op.eng == "pe":
                        continue
                    if p.semidx > need_eng.get(p.eng, 0):
                        need_eng[p.eng] = p.semidx
                else:
                    if p.dma_val > need_buf.get(id(p.buf), (None, 0))[1]:
                        need_buf[id(p.buf)] = (p.buf, p.dma_val)
            for pe_, idx in need_eng.items():
                if waited_eng[op.eng][pe_] < idx:
                    s, v = eng_sem(pe_, idx)
                    e.wait_ge(s, v)
                    waited_eng[op.eng][pe_] = idx
            for bid, (b, v) in need_buf.items():
                if waited_buf[op.eng].get(bid, 0) < v:
                    e.wait_ge(buf_sem(b), v)
                    waited_buf[op.eng][bid] = v
            ins = op.fn(e)
            if op.kind == "c":
                if op.sig:
                    s, v = eng_sem(op.eng, op.semidx)
                    ins.then_inc(s, 1)
            elif op.kind == "d":
                ins.then_inc(buf_sem(op.buf), 16)
            else:
                ins.then_inc(buf_sem(op.buf))
        sp = nc.sync
        for b in final_bufs:
            sp.wait_ge(buf_sem(b), 16 * b.dma_cnt)


class T:
    def __init__(self, ap, name, nbuf=1):
        self.ap = ap
        self.b = Buf(name)
        self.bs = [Buf(f"{name}_{i}") for i in range(nbuf)] if nbuf > 1 else [self.b]


def build(stage=99, dbg=False):
    nc = bass.Bass("TRN2", target_bir_lowering=False)
    P = Prog(nc)
    def din(name, shape, dt=F32):
        return nc.dram_tensor(name, shape, dt, kind="ExternalInput").ap()

    xb = din("xb", [S, D])
    xres = din("xres", [1024, D])
    w_in_c = din("w_in_c", [D, 1536])
    w_out_p = din("w_out_p", [D, D])
    w_r = din("w_r", [D, 36])
    if stage == 99:
        w_gate = din("w_gate", [N_EXP, D, DE])
        w_up = din("w_up", [N_EXP, D, DE])
        w_down = din("w_down", [N_EXP, DE, D])
    vecs = din("vecs", [128, 16 + 16 + 96])
    lamv = din("lamv", [128, 512])
    subw = din("subw", [128, 256])
    rbias = din("rbias", [128, 36])
    wfin = din("wfin", [128, D])
    pastm = din("pastm", [128, 512])
    cbf = din("cbf", [128, 256 + 512], BF16)
    lrb_d = din("lrb", [128, 2 * 16 * 128], BF16)
    rbt_d = din("rbt", [2, S], BF16)
    out = nc.dram_tensor("out", [1024, D], F32, kind="ExternalOutput").ap()
    bounce = [nc.dram_tensor(f"bounce{j}", [1024, 512], BF16) for j in range(4)]
    agbig = nc.dram_tensor("agbig", [4 * 4096, 512], BF16)
    idxg_d = nc.dram_tensor("idxg", [128, 32], mybir.dt.int32, kind="ExternalInput").ap()
    bounce_b = [Buf(f"bounce{j}") for j in range(4)]
    agout_b = [Buf(f"agout{j}") for j in range(4)]
    out_b = Buf("outd")
    dbg_outs = {}

    PA = [T(nc.alloc_psum_tensor(f"pa{i}", [128, 512], F32).ap(), f"pa{i}") for i in range(8)]
    PT = []
    for i in range(2):
        v = T(PA[6 + i].ap.bitcast(BF16), f"ptv{i}")
        v.b = PA[6 + i].b
        v.bs = [v.b]
        PT.append(v)

    es_all = ExitStack()

    def sb(es, name, shape, dt, nbuf=1):
        h = es.enter_context(nc.sbuf_tensor("sb_" + name, shape, dt))
        return T(h.ap() if hasattr(h, "ap") and callable(h.ap) else h, name, nbuf)

    vecs_t = sb(es_all, "vecs", [128, 128], F32)
    cbf_t = sb(es_all, "cbf", [128, 768], BF16)
    P.dma("sp", vecs_t.ap, vecs, writes=[vecs_t.b], buf=vecs_t.b)
    P.dma("sp", cbf_t.ap, cbf, writes=[cbf_t.b], buf=cbf_t.b)
    epsT = sb(es_all, "epsT", [128, 1], F32)
    bar_t = sb(es_all, "bar_t", [128, 8], F32)
    P.add("dve", lambda e: e.memset(epsT.ap, EPS), writes=[epsT.b])
    wmix = vecs_t.ap[:, 0:16]
    wffn = vecs_t.ap[:, 16:32]
    ident = cbf_t.ap[:, 0:128]
    tri = cbf_t.ap[:, 128:256]
    CONST = [vecs_t.b, cbf_t.b]

    es1 = ExitStack()
    QT = [sb(es1, f"qt{i}", [128, S], BF16, nbuf=8) for i in range(4)]
    KT = [sb(es1, f"kt{i}", [128, S], BF16, nbuf=8) for i in range(4)]
    VA = [sb(es1, f"va{i}", [128, NT, 129], BF16, nbuf=8) for i in range(2)]
    VD = sb(es1, "vd", [128, NT, 257], BF16, nbuf=8)
    ksum = sb(es1, "ksum", [128, 2, 16], F32)
    ss1 = sb(es1, "ss1", [128, NT], F32, nbuf=NT)
    rstd1 = sb(es1, "rstd1", [128, NT], F32, nbuf=NT)

    P.add("dve", lambda e: e.memset(ksum.ap, 0.0), writes=[ksum.b])
    P.add("pool", lambda e: e.memset(VA[0].ap[:, :, 128:129], 1.0), writes=VA[0].bs)
    P.add("pool", lambda e: e.memset(VA[1].ap[:, :, 128:129], 1.0), writes=VA[1].bs)
    P.add("pool", lambda e: e.memset(VD.ap[:, :, 256:257], 1.0), writes=VD.bs)

    es1a = ExitStack()
    w_in_sb = sb(es1a, "w_in_sb", [128, 16, 1536], BF16, nbuf=9)
    xts = [sb(es1a, f"xt{i}", [128, D], F32) for i in range(2)]
    xns = [sb(es1a, f"xn{i}", [128, D], BF16) for i in range(2)]
    hTs = [sb(es1a, f"hT{i}", [128, 16, 512], BF16) for i in range(2)]

    w_in_v = w_in_c.rearrange("(k p) c -> p k c", p=128)
    for c in range(8):
        P.dma("pool", w_in_sb.ap[:, :, c * 128:(c + 1) * 128], w_in_v[:, :, c * 128:(c + 1) * 128],
              writes=[w_in_sb.bs[c]], buf=w_in_sb.bs[c])
    P.dma("pool", w_in_sb.ap[:, :, 1024:1536], w_in_v[:, :, 1024:1536], writes=[w_in_sb.bs[8]], buf=w_in_sb.bs[8])

    QSCALE = 128.0 ** -0.5
    pa_ctr = [0]

    def norm_T(g, tt):
        hT = hTs[g % 2]
        t = g * 4 + tt
        xt = xts[t % 2]
        xn = xns[t % 2]
        P.dma("sp", xt.ap, xb[t * 128:(t + 1) * 128, :], writes=[xt.b], buf=xt.b)
        P.add("act", lambda e: e.activation(out=xn.ap, in_=xt.ap, func=AF.Square, accum_out=ss1.ap[:, t:t + 1]),
              reads=[xt.b], writes=[xn.b, ss1.bs[t]])
        P.add("act", lambda e: e.activation(out=rstd1.ap[:, t:t + 1], in_=ss1.ap[:, t:t + 1], func=AF.Sqrt,
                                            scale=1.0 / D, bias=epsT.ap),
              reads=[ss1.bs[t], epsT.b], writes=[rstd1.bs[t]])
        P.add("dve", lambda e: e.reciprocal(out=rstd1.ap[:, t:t + 1], in_=rstd1.ap[:, t:t + 1]),
              reads=[rstd1.bs[t]], writes=[rstd1.bs[t]])
        P.add("dve", lambda e: e.tensor_scalar(out=xn.ap, in0=xt.ap, scalar1=rstd1.ap[:, t:t + 1], scalar2=None,
                                               op0=ALU.mult), reads=[xt.b, rstd1.bs[t]], writes=[xn.b])

    def norm_pe(g, tt):
        hT = hTs[g % 2]
        t = g * 4 + tt
        xn = xns[t % 2]
        for r in range(2):
            pt = PT[r]
            for k in range(8):
                P.add("pe", lambda e, pt=pt, k=k, r=r: e.transpose(
                    out=pt.ap[:, k * 128:(k + 1) * 128], in_=xn.ap[:, (r * 8 + k) * 128:(r * 8 + k + 1) * 128],
                    identity=ident), reads=[xn.b] + CONST, writes=[pt.b])
            P.add("dve", lambda e, pt=pt, r=r: e.tensor_tensor(
                out=hT.ap[:, r * 8:(r + 1) * 8, tt * 128:(tt + 1) * 128],
                in0=pt.ap.rearrange("p (k n) -> p k n", k=8),
                in1=wmix[:, r * 8:(r + 1) * 8].unsqueeze(2).to_broadcast([128, 8, 128]),
                op=ALU.mult), reads=[pt.b] + CONST, writes=[hT.b])

    def chunk_T(g, c):
        hT = hTs[g % 2]
        pa = PA[pa_ctr[0] % 2]
        pa_ctr[0] += 1
        for k in range(16):
            P.add("pe", lambda e, k=k: e.matmul(
                pa.ap, lhsT=w_in_sb.ap[:, k, c * 128:(c + 1) * 128], rhs=hT.ap[:, k, :],
                start=(k == 0), stop=(k == 15)), reads=[hT.b, w_in_sb.bs[c]], writes=[pa.b])
        cols = slice(g * 512, (g + 1) * 512)
        if c in (0, 1, 4, 5):
            dst = QT[c if c < 2 else c - 2]
            P.add("act", lambda e: e.activation(out=dst.ap[:, cols], in_=pa.ap, func=AF.Copy, scale=QSCALE),
                  reads=[pa.b], writes=[dst.bs[g]])
        elif c in (2, 3):
            dst = KT[c - 2]
            for hf in range(2):
                P.add("act", lambda e, hf=hf: e.activation(
                    out=dst.ap[:, g * 512 + hf * 256:g * 512 + (hf + 1) * 256],
                    in_=pa.ap[:, hf * 256:(hf + 1) * 256], func=AF.Identity,
                    accum_out=ksum.ap[:, c - 2, 2 * g + hf:2 * g + hf + 1]),
                    reads=[pa.b], writes=[dst.bs[g], ksum.b])
        else:
            dst = KT[c - 4]
            P.add("act", lambda e: e.activation(out=dst.ap[:, cols], in_=pa.ap, func=AF.Copy),
                  reads=[pa.b], writes=[dst.bs[g]])

    def chunk_V(g, tt):
        hT = hTs[g % 2]
        t = g * 4 + tt
        pa = PA[pa_ctr[0] % 2]
        pa_ctr[0] += 1
        for k in range(16):
            P.add("pe", lambda e, k=k: e.matmul(
                pa.ap, lhsT=hT.ap[:, k, tt * 128:(tt + 1) * 128], rhs=w_in_sb.ap[:, k, 1024:1536],
                start=(k == 0), stop=(k == 15)), reads=[hT.b, w_in_sb.bs[8]], writes=[pa.b])
        P.add("dve", lambda e: e.tensor_copy(out=VA[0].ap[:, t, 0:128], in_=pa.ap[:, 0:128]),
              reads=[pa.b], writes=[VA[0].bs[g]])
        P.add("dve", lambda e: e.tensor_copy(out=VA[1].ap[:, t, 0:128], in_=pa.ap[:, 128:256]),
              reads=[pa.b], writes=[VA[1].bs[g]])
        P.add("dve", lambda e: e.tensor_copy(out=VD.ap[:, t, 0:256], in_=pa.ap[:, 256:512]),
              reads=[pa.b], writes=[VD.bs[g]])

    norm_T(0, 0)
    norm_T(0, 1)
    norm_pe(0, 0)
    norm_T(0, 2)
    norm_pe(0, 1)
    norm_T(0, 3)
    norm_pe(0, 2)
    norm_pe(0, 3)
    for g in range(8):
        ci = 0
        for c in range(12):
            if c < 8:
                chunk_T(g, c)
            else:
                chunk_V(g, c - 8)
            ci += 1
            if g + 1 < 8:
                if ci in (1, 4, 7, 10):
                    norm_T(g + 1, (ci - 1) // 3)
                if ci in (3, 6, 9, 12):
                    norm_pe(g + 1, (ci - 3) // 3)
    es1a.close()

    if dbg and stage == 1:
        for nm, tt_ in (("qt0", QT[0]), ("kt1", KT[1]), ("qt3", QT[3])):
            d = nc.dram_tensor("dbg_" + nm, [128, S], BF16, kind="ExternalOutput").ap()
            b = Buf("dbg_" + nm)
            P.dma("sp", d, tt_.ap, reads=tt_.bs, writes=[b], buf=b)
            dbg_outs[nm] = b
        d = nc.dram_tensor("dbg_vd", [128, NT * 257], BF16, kind="ExternalOutput").ap()
        b = Buf("dbg_vd")
        P.dma("sp", d, VD.ap.rearrange("p t c -> p (t c)"), reads=VD.bs, writes=[b], buf=b)
        dbg_outs["vd"] = b
        d = nc.dram_tensor("dbg_ksum", [128, 32], F32, kind="ExternalOutput").ap()
        b = Buf("dbg_ksum")
        P.dma("sp", d, ksum.ap.rearrange("p a c -> p (a c)"), reads=[ksum.b], writes=[b], buf=b)
        dbg_outs["ksum"] = b
        P.emit(list(dbg_outs.values()))
        return nc

    es1b = ExitStack()
    RB = [sb(es1b, f"rb{h}", [128, S], BF16) for h in range(2)]
    lrb = sb(es1b, "lrb", [128, 2 * 16 * 128], BF16)
    pTs = [sb(es1b, f"pT{i}", [128, 512], BF16) for i in range(5)]
    O1n = sb(es1b, "o1n", [128, NT, 256], F32, nbuf=NT)
    mixed = sb(es1b, "mixed", [128, NT, 512], BF16, nbuf=NT)
    lam_t = sb(es1b, "lam_t", [128, 512], F32)
    subw_t = sb(es1b, "subw_t", [128, 256], F32)
    pastm_t = sb(es1b, "pastm_t", [128, 512], F32)
    small = sb(es1b, "small", [128, 64], F32)
    gall = sb(es1b, "gall", [128, 32, 16], F32)
    max8 = sb(es1b, "max8", [128, 32, 8], F32)
    selb = sb(es1b, "selb", [128, 32, 16], BF16)
    selb2 = sb(es1b, "selb2", [128, 32, 16], F32)
    km = sb(es1b, "km", [128, 4, 16], F32)
    kmb = sb(es1b, "kmb", [128, 2, 16], BF16)
    recs = sb(es1b, "recs", [128, 8], F32, nbuf=8)
    dtmp = sb(es1b, "dtmp", [128, 2, 256], F32, nbuf=2)
    djunk = sb(es1b, "djunk", [128, 256], F32)

    old1 = list(w_in_sb.bs)
    for tl in xts + xns + hTs:
        old1.append(tl.b)
    new1 = []
    for tl in RB + [lrb] + pTs + [O1n, mixed, lam_t, subw_t, pastm_t, small, gall, max8, selb, selb2, km, kmb, recs,
                                  dtmp, djunk]:
        new1 += tl.bs
        if tl.b not in new1:
            new1.append(tl.b)
    P.add("dve", lambda e: e.memset(bar_t.ap[:, 6:7], 0.0), reads=[], writes=old1 + [bar_t.b])
    P.add("dve", lambda e: e.memset(bar_t.ap[:, 7:8], 0.0), reads=[bar_t.b], writes=new1)

    P.dma("sp", lam_t.ap, lamv, writes=[lam_t.b], buf=lam_t.b)
    P.dma("sp", subw_t.ap, subw, writes=[subw_t.b], buf=subw_t.b)
    P.dma("sp", pastm_t.ap, pastm, writes=[pastm_t.b], buf=pastm_t.b)
    P.dma("sp", lrb.ap, lrb_d, writes=[lrb.b], buf=lrb.b)
    for h in range(2):
        P.add("pool", lambda e, h=h: e.memset(RB[h].ap, 0.0), writes=[RB[h].b])
        P.dma("sp", RB[h].ap[16:18, :], rbt_d, reads=[], writes=[RB[h].b], buf=RB[h].b)

    P.add("dve", lambda e: e.tensor_tensor(out=djunk.ap[:, 0:128], in0=lam_t.ap[:, 0:128], in1=lam_t.ap[:, 128:256],
                                           op=ALU.mult), reads=[lam_t.b], writes=[djunk.b])
    P.add("dve", lambda e: e.reduce_sum(out=small.ap[:, 0:1], in_=djunk.ap[:, 0:128], axis=AX.X),
          reads=[djunk.b], writes=[small.b])
    P.add("dve", lambda e: e.tensor_tensor(out=djunk.ap[:, 128:256], in0=lam_t.ap[:, 256:384], in1=lam_t.ap[:, 384:512],
                                           op=ALU.mult), reads=[lam_t.b], writes=[djunk.b])
    P.add("dve", lambda e: e.reduce_sum(out=small.ap[:, 1:2], in_=djunk.ap[:, 128:256], axis=AX.X),
          reads=[djunk.b], writes=[small.b])
    P.add("act", lambda e: e.activation(out=small.ap[:, 2:4], in_=small.ap[:, 0:2], func=AF.Exp),
          reads=[small.b], writes=[small.b])
    P.add("dve", lambda e: e.scalar_tensor_tensor(out=small.ap[:, 4:5], in0=small.ap[:, 3:4], scalar=-0.2,
                                                  in1=small.ap[:, 2:3], op0=ALU.add, op1=ALU.subtract),
          reads=[small.b], writes=[small.b])
    neglam = small.ap[:, 4:5]


    def _dbg_exit(tl, shape2):
        d = nc.dram_tensor("dbg_x", shape2, tl.ap.dtype, kind="ExternalOutput").ap()
        b = Buf("dbg_x")
        src = tl.ap
        if len(src.shape) == 3:
            src = src.rearrange("p a b -> p (a b)")
        P.dma("sp", d, src, reads=tl.bs + [tl.b], writes=[b], buf=b)
        P.emit([b])
        return nc

    if dbg and stage == 11:
        return _dbg_exit(small, [128, 64])
    kb_tab = [vecs_t.ap[:, 32 + 32 * i:64 + 32 * i] for i in range(3)]

    for h in range(2):
        P.add("dve", lambda e, h=h: e.tensor_scalar(out=km.ap[:, 0, :], in0=ksum.ap[:, h, :], scalar1=1.0 / 256,
                                                    scalar2=None, op0=ALU.mult), reads=[ksum.b], writes=[km.b])
        P.add("dve", lambda e: e.tensor_copy(out=kmb.ap[:, 0, :], in_=km.ap[:, 0, :]), reads=[km.b], writes=[kmb.b])
        P.add("dve", lambda e: e.tensor_copy(out=km.ap[:, 1, :], in_=kmb.ap[:, 0, :]), reads=[kmb.b], writes=[km.b])
        P.add("dve", lambda e: e.tensor_tensor(out=km.ap[:, 2, :], in0=km.ap[:, 0, :], in1=km.ap[:, 1, :],
                                               op=ALU.subtract), reads=[km.b], writes=[km.b])
        P.add("dve", lambda e: e.tensor_copy(out=kmb.ap[:, 1, :], in_=km.ap[:, 2, :]), reads=[km.b], writes=[kmb.b])
        pg = PA[2]
        for i in range(NT):
            for part in range(2):
                P.add("pe", lambda e, i=i, part=part, h=h: e.matmul(
                    pg.ap[:, i * 16:(i + 1) * 16], lhsT=QT[h].ap[:, i * 128:(i + 1) * 128], rhs=kmb.ap[:, part, :],
                    start=(part == 0), stop=(part == 1)), reads=[QT[h].bs[i // 4], kmb.b], writes=[pg.b])
        P.add("dve", lambda e: e.tensor_tensor(out=gall.ap.rearrange("p a b -> p (a b)"), in0=pg.ap, in1=pastm_t.ap,
                                               op=ALU.add), reads=[pg.b, pastm_t.b], writes=[gall.b])
        if dbg and stage == 12:
            return _dbg_exit(gall, [128, 512])
        for i in range(NT):
            P.add("dve", lambda e, i=i: e.max(out=max8.ap[:, i, :], in_=gall.ap[:, i, :]),
                  reads=[gall.b], writes=[max8.b])
        P.add("dve", lambda e: e.tensor_tensor(out=selb2.ap, in0=gall.ap,
                                               in1=max8.ap[:, :, 3:4].to_broadcast([128, 32, 16]),
                                               op=ALU.is_lt), reads=[gall.b, max8.b], writes=[selb2.b])
        P.add("dve", lambda e: e.tensor_scalar(out=selb.ap, in0=selb2.ap, scalar1=NEG, scalar2=None, op0=ALU.mult),
              reads=[selb2.b], writes=[selb.b])
        if dbg and stage == 13:
            return _dbg_exit(selb, [128, 512])
        for r in range(4):
            pt = PT[r % 2]
            for k in range(8):
                i = r * 8 + k
                P.add("pe", lambda e, pt=pt, k=k, i=i: e.transpose(
                    out=pt.ap[0:16, k * 128:(k + 1) * 128], in_=selb.ap[:, i, :], identity=ident),
                    reads=[selb.b] + CONST, writes=[pt.b])
            P.add("act", lambda e, pt=pt, r=r, h=h: e.activation(
                out=RB[h].ap[0:16, r * 1024:(r + 1) * 1024], in_=pt.ap[0:16, :], func=AF.Copy),
                reads=[pt.b], writes=[RB[h].b])

    if dbg and stage == 15:
        d = nc.dram_tensor("dbg_rb", [18, S], BF16, kind="ExternalOutput").ap()
        b = Buf("dbg_rb")
        P.dma("sp", d, RB[1].ap, reads=[RB[1].b], writes=[b], buf=b)
        d2 = nc.dram_tensor("dbg_gall", [128, 512], F32, kind="ExternalOutput").ap()
        b2 = Buf("dbg_gall")
        P.dma("sp", d2, gall.ap.rearrange("p a b -> p (a b)"), reads=[gall.b], writes=[b2], buf=b2)
        P.emit([b, b2])
        return nc

    sc_i = [0]

    tasks = []
    SCB = [PA[0], PA[1], PA[6], PA[7]]

    def attn_pass(qT, kT, V, dv1, kbias, rb, lrb_h, out_fn, qts=range(8)):
        Oacc = PA[2:6]
        for qt in qts:
            nkt = 4 * qt + 4
            for kt in range(nkt):
                j = kt - 4 * qt
                q0 = 128 * j if j > 0 else 0
                ps = SCB[sc_i[0] % 4]
                pT = pTs[sc_i[0] % 5]
                sc_i[0] += 1
                qcols = slice(qt * 512 + q0, (qt + 1) * 512)
                more = (rb is not None) or (j >= 0)

                def qk(ps=ps, kt=kt, q0=q0, qcols=qcols, more=more, j=j, qt=qt):
                    P.add("pe", lambda e: e.matmul(
                        ps.ap[:, q0:512], lhsT=kT.ap[:, kt * 128:(kt + 1) * 128], rhs=qT.ap[:, qcols],
                        start=True, stop=not more), reads=[kT.bs[kt // 4], qT.bs[qt]], writes=[ps.b])
                    if rb is not None:
                        n = kt // 2
                        P.add("pe", lambda e: e.matmul(
                            ps.ap[:, q0:512], lhsT=lrb.ap[:, (lrb_h * 16 + n) * 128:(lrb_h * 16 + n + 1) * 128],
                            rhs=rb.ap[:, qcols], start=False, stop=(j < 0)), reads=[rb.b, lrb.b], writes=[ps.b])
                    if j >= 0:
                        P.add("pe", lambda e: e.matmul(
                            ps.ap[:, q0:q0 + 128], lhsT=ident, rhs=tri, start=False, stop=True),
                            reads=CONST, writes=[ps.b])

                def rest(ps=ps, pT=pT, kt=kt, q0=q0, j=j, qt=qt):
                    m = j + 28
                    P.add("act", lambda e: e.activation(
                        out=pT.ap[:, q0:512], in_=ps.ap[:, q0:512], func=AF.Exp, bias=kbias[:, m:m + 1], scale=1.0),
                        reads=[ps.b] + CONST, writes=[pT.b])
                    for sub in range(q0 // 128, 4):
                        last = (kt == 4 * qt + sub)
                        P.add("pe", lambda e, sub=sub, last=last: e.matmul(
                            Oacc[sub].ap[:, 0:dv1], lhsT=pT.ap[:, sub * 128:(sub + 1) * 128], rhs=V.ap[:, kt, 0:dv1],
                            start=(kt == 0), stop=last), reads=[pT.b, V.bs[kt // 4]], writes=[Oacc[sub].b])
                        if last:
                            out_fn(qt * 4 + sub, Oacc[sub])

                tasks.append((qk, rest))

    DEPTH = 3

    def run_tasks():
        n = len(tasks)
        for i in range(min(DEPTH, n)):
            tasks[i][0]()
        for i in range(n):
            if i + DEPTH < n:
                tasks[i + DEPTH][0]()
            tasks[i][1]()
        tasks.clear()

    def moba_out(h):
        def f(tile, oa):
            rb_ = recs.bs[tile % 8]
            rc = recs.ap[:, tile % 8:tile % 8 + 1]
            P.add("dve", lambda e: e.reciprocal(out=rc, in_=oa.ap[:, 128:129]), reads=[oa.b], writes=[rb_])
            P.add("dve", lambda e: e.tensor_scalar(out=mixed.ap[:, tile, h * 128:(h + 1) * 128], in0=oa.ap[:, 0:128],
                                                   scalar1=rc, scalar2=None, op0=ALU.mult),
                  reads=[oa.b, rb_], writes=[mixed.bs[tile]])
        return f

    def diff1_out(tile, oa):
        rb_ = recs.bs[tile % 8]
        rc = recs.ap[:, tile % 8:tile % 8 + 1]
        P.add("dve", lambda e: e.reciprocal(out=rc, in_=oa.ap[:, 256:257]), reads=[oa.b], writes=[rb_])
        P.add("dve", lambda e: e.tensor_scalar(out=O1n.ap[:, tile, :], in0=oa.ap[:, 0:256], scalar1=rc, scalar2=None,
                                               op0=ALU.mult), reads=[oa.b, rb_], writes=[O1n.bs[tile]])

    def diff2_out(tile, oa):
        rb_ = recs.bs[tile % 8]
        rc = recs.ap[:, tile % 8:tile % 8 + 1]
        P.add("dve", lambda e: e.reciprocal(out=rc, in_=oa.ap[:, 256:257]), reads=[oa.b], writes=[rb_])
        P.add("dve", lambda e: e.tensor_tensor(out=rc, in0=rc, in1=neglam, op=ALU.mult),
              reads=[rb_, small.b], writes=[rb_])
        P.add("dve", lambda e: e.scalar_tensor_tensor(out=O1n.ap[:, tile, :], in0=oa.ap[:, 0:256], scalar=rc,
                                                      in1=O1n.ap[:, tile, :], op0=ALU.mult, op1=ALU.add),
              reads=[oa.b, rb_, O1n.bs[tile]], writes=[O1n.bs[tile]])
        if tile % 8 == 7:
            t0 = tile - 7
            sb_ = dtmp.bs[(tile // 8) % 2]
            sq = dtmp.ap[:, (tile // 8) % 2, 0:8]
            rs = dtmp.ap[:, (tile // 8) % 2, 8:16]
            for t in range(t0, t0 + 8):
                P.add("act", lambda e, t=t: e.activation(out=djunk.ap, in_=O1n.ap[:, t, :], func=AF.Square,
                                                         accum_out=sq[:, t - t0:t - t0 + 1]),
                      reads=[O1n.bs[t]], writes=[djunk.b, sb_])
            P.add("act", lambda e: e.activation(out=rs, in_=sq, func=AF.Ln, scale=1.0 / 256, bias=epsT.ap),
                  reads=[sb_, epsT.b], writes=[sb_])
            P.add("act", lambda e: e.activation(out=rs, in_=rs, func=AF.Exp, scale=-0.5), reads=[sb_], writes=[sb_])
            P.add("dve", lambda e: e.tensor_scalar(out=rs, in0=rs, scalar1=0.8, scalar2=None, op0=ALU.mult),
                  reads=[sb_], writes=[sb_])
            for t in range(t0, t0 + 8):
                P.add("dve", lambda e, t=t: e.scalar_tensor_tensor(
                    out=mixed.ap[:, t, 256:512], in0=O1n.ap[:, t, :], scalar=rs[:, t - t0:t - t0 + 1],
                    in1=subw_t.ap, op0=ALU.mult, op1=ALU.mult),
                    reads=[O1n.bs[t], sb_, subw_t.b], writes=[mixed.bs[t]])

    def exchange(j):
        P.dma("sp", bounce[j].ap().rearrange("(t p) f -> p t f", p=128), mixed.ap[:, j * 8:(j + 1) * 8, :],
              reads=mixed.bs[j * 8:(j + 1) * 8], writes=[bounce_b[j]], buf=bounce_b[j])
        P.add("pool", lambda e: e.collective_compute(
            "AllGather", ALU.bypass, replica_groups=[[0, 1, 2, 3], [4, 5, 6, 7]],
            ins=[bounce[j].ap().opt()], outs=[agbig.ap()[j * 4096:(j + 1) * 4096, :].opt()]),
            reads=[bounce_b[j]], writes=[agout_b[j]], kind="cc", buf=agout_b[j])

    passes = [(QT[0], KT[0], VA[0], 129, kb_tab[0], RB[0], 0, moba_out(0)),
              (QT[1], KT[1], VA[1], 129, kb_tab[1], RB[1], 1, moba_out(1)),
              (QT[2], KT[2], VD, 257, kb_tab[2], None, 0, diff1_out),
              (QT[3], KT[3], VD, 257, kb_tab[2], None, 0, diff2_out)]
    if not (dbg and stage == 17):
        for j in range(4):
            for pz in passes:
                attn_pass(*pz, qts=(2 * j, 2 * j + 1))
            if not (dbg and stage == 2):
                tasks.append((lambda: None, lambda j=j: exchange(j)))
        run_tasks()
    if dbg and stage == 17:
        attn_pass(QT[0], KT[0], VA[0], 129, kb_tab[0], RB[0], 0, moba_out(0))
        run_tasks()
        d = nc.dram_tensor("dbg_mixed", [128, NT * 512], BF16, kind="ExternalOutput").ap()
        b = Buf("dbg_mixed")
        P.dma("sp", d, mixed.ap.rearrange("p t c -> p (t c)"), reads=mixed.bs, writes=[b], buf=b)
        P.emit([b])
        return nc

    if dbg and stage == 2:
        d = nc.dram_tensor("dbg_mixed", [128, NT * 512], BF16, kind="ExternalOutput").ap()
        b = Buf("dbg_mixed")
        P.dma("sp", d, mixed.ap.rearrange("p t c -> p (t c)"), reads=mixed.bs, writes=[b], buf=b)
        P.emit([b])
        return nc

    es1b.close()
    es1.close()

    allb = Buf("phase_barrier")

    def barrier():
        for en in ("pe", "act", "dve", "pool", "sp"):
            pass

    es2 = ExitStack()
    x1 = sb(es2, "x1", [128, 8, D], F32, nbuf=8)
    ss2 = sb(es2, "ss2", [128, 16], F32, nbuf=16)
    h2T = sb(es2, "h2T", [128, 16, 1024], BF16, nbuf=8)
    xn2 = [sb(es2, f"xn2_{i}", [128, D], BF16) for i in range(2)]
    selI = cbf_t.ap[:, 256:768]

    es2a = ExitStack()
    mixT = sb(es2a, "mixT", [128, 16, 1024], BF16, nbuf=16)
    agb = [sb(es2a, f"agb{i}", [128, 32, 512], BF16) for i in range(1)]
    wob = [sb(es2a, f"wob{i}", [128, 16, 512], BF16) for i in range(2)]

    p1_bufs = []
    for tl in QT + KT + VA + [VD, ksum, ss1, rstd1, mixed, O1n, lam_t, subw_t, pastm_t, small, gall, max8, selb, selb2,
                              km, kmb, recs, dtmp, djunk, lrb] + RB + pTs + xts + xns + hTs + [w_in_sb]:
        p1_bufs += tl.bs
        if tl.b not in p1_bufs:
            p1_bufs.append(tl.b)
    P.add("dve", lambda e: e.memset(bar_t.ap[:, 0:1], 0.0), reads=[], writes=p1_bufs + [bar_t.b])
    new_bufs = x1.bs + ss2.bs + mixT.bs + h2T.bs + [t_.b for t_ in agb + wob + xn2]
    P.add("dve", lambda e: e.memset(bar_t.ap[:, 1:2], 0.0), reads=[bar_t.b], writes=new_bufs)

    for i in range(8):
        P.dma("sp", x1.ap[:, i, :], xres[i * 128:(i + 1) * 128, :], writes=[x1.bs[i]], buf=x1.bs[i])

    idx_t = sb(es2a, "idx_t", [128, 32], mybir.dt.int32)
    P.add("dve", lambda e: e.memset(bar_t.ap[:, 2:3], 0.0), reads=[bar_t.b], writes=[idx_t.b])
    P.dma("sp", idx_t.ap, idxg_d, writes=[idx_t.b], buf=idx_t.b)
    ab = agb[0]
    for ri in range(32):
        P.add("pool", lambda e, ri=ri: e.indirect_dma_start(
            out=ab.ap[:, ri, :], out_offset=None, in_=agbig.ap(),
            in_offset=bass.IndirectOffsetOnAxis(ap=idx_t.ap[:, ri:ri + 1], axis=0)),
            reads=[idx_t.b] + agout_b, writes=[ab.b], kind="d", buf=ab.b)
    ev = 0
    for r in range(4):
        for fc in range(4):
            pt = PT[ev % 2]
            for i in range(8):
                P.add("pe", lambda e, pt=pt, r=r, fc=fc, i=i: e.transpose(
                    out=pt.ap[:, i * 128:(i + 1) * 128], in_=ab.ap[:, r * 8 + i, fc * 128:(fc + 1) * 128],
                    identity=ident), reads=[ab.b] + CONST, writes=[pt.b])
            if ev % 2 == 0:
                P.add("act", lambda e, pt=pt, r=r, fc=fc: e.activation(
                    out=mixT.ap[:, r * 4 + fc, :], in_=pt.ap, func=AF.Copy), reads=[pt.b],
                    writes=[mixT.bs[r * 4 + fc]])
            else:
                P.add("dve", lambda e, pt=pt, r=r, fc=fc: e.tensor_copy(out=mixT.ap[:, r * 4 + fc, :], in_=pt.ap),
                      reads=[pt.b], writes=[mixT.bs[r * 4 + fc]])
            ev += 1

    def rms_rstd(ssap, ssb, n):
        P.add("act", lambda e: e.activation(out=ssap, in_=ssap, func=AF.Sqrt, scale=1.0 / n, bias=epsT.ap),
              reads=[ssb, epsT.b], writes=[ssb])
        P.add("dve", lambda e: e.reciprocal(out=ssap, in_=ssap), reads=[ssb], writes=[ssb])

    def norm2_pre(i):
        xn = xn2[i % 2]
        ssap = ss2.ap[:, i:i + 1]
        P.add("act", lambda e: e.activation(out=xn.ap, in_=x1.ap[:, i, :], func=AF.Square, accum_out=ssap),
              reads=[x1.bs[i]], writes=[xn.b, ss2.bs[i]])
        rms_rstd(ssap, ss2.bs[i], D)
        P.add("dve", lambda e: e.tensor_scalar(out=xn.ap, in0=x1.ap[:, i, :], scalar1=ssap, scalar2=None,
                                               op0=ALU.mult), reads=[x1.bs[i], ss2.bs[i]], writes=[xn.b])

    def norm2_pe(i):
        xn = xn2[i % 2]
        for r in range(2):
            pt = PT[r]
            for k in range(8):
                P.add("pe", lambda e, pt=pt, k=k, r=r: e.transpose(
                    out=pt.ap[:, k * 128:(k + 1) * 128], in_=xn.ap[:, (r * 8 + k) * 128:(r * 8 + k + 1) * 128],
                    identity=ident), reads=[xn.b] + CONST, writes=[pt.b])
            P.add("dve", lambda e, pt=pt, r=r: e.tensor_tensor(
                out=h2T.ap[:, r * 8:(r + 1) * 8, i * 128:(i + 1) * 128],
                in0=pt.ap.rearrange("p (k n) -> p k n", k=8),
                in1=wffn[:, r * 8:(r + 1) * 8].unsqueeze(2).to_broadcast([128, 8, 128]),
                op=ALU.mult), reads=[pt.b] + CONST, writes=[h2T.bs[i]])

    pa_i = 0
    w_out_v = w_out_p.rearrange("(k p) c -> p k c", p=128)
    for dc in range(4):
        wb = wob[dc % 2]
        P.dma("pool", wb.ap, w_out_v[:, :, dc * 512:(dc + 1) * 512], writes=[wb.b], buf=wb.b)
        for i in range(8):
            pa = PA[pa_i % 2]
            pa_i += 1
            for k in range(16):
                P.add("pe", lambda e, pa=pa, k=k, i=i, wb=wb: e.matmul(
                    pa.ap, lhsT=mixT.ap[:, k, i * 128:(i + 1) * 128], rhs=wb.ap[:, k, :],
                    start=(k == 0), stop=(k == 15)), reads=[mixT.bs[k], wb.b], writes=[pa.b])
            P.add("dve", lambda e, pa=pa, i=i, dc=dc: e.tensor_tensor(
                out=x1.ap[:, i, dc * 512:(dc + 1) * 512], in0=pa.ap, in1=x1.ap[:, i, dc * 512:(dc + 1) * 512],
                op=ALU.add), reads=[pa.b, x1.bs[i]], writes=[x1.bs[i]])
            if dc == 3:
                if i >= 2:
                    norm2_pe(i - 2)
                norm2_pre(i)
    norm2_pe(6)
    norm2_pe(7)
    es2a.close()

    if dbg and stage == 3:
        d = nc.dram_tensor("dbg_x1", [128, 8 * D], F32, kind="ExternalOutput").ap()
        b = Buf("dbg_x1")
        P.dma("sp", d, x1.ap.rearrange("p t c -> p (t c)"), reads=x1.bs, writes=[b], buf=b)
        P.emit([b])
        return nc

    es2b = ExitStack()
    wr_sb = sb(es2b, "wr_sb", [128, 16, 36], BF16)
    rb_sb = sb(es2b, "rb_sb", [128, 36], F32)
    lg = sb(es2b, "lg", [128, 8, 36], F32, nbuf=8)
    rt = sb(es2b, "rt", [128, 8, 64], F32, nbuf=8)
    comb = sb(es2b, "comb", [128, 8, 32], F32, nbuf=8)
    wfin_t = sb(es2b, "wfin_t", [128, D], F32)
    es2w = ExitStack()
    wg = [sb(es2w, f"wg{i}", [128, 16, DE], BF16) for i in range(2)]
    wu = [sb(es2w, f"wu{i}", [128, 16, DE], BF16) for i in range(2)]
    wd = [sb(es2w, "wd0", [128, 4, D], BF16)] * 2
    sa = [sb(es2w, f"sa{i}", [128, 512], BF16) for i in range(2)]
    actT = [sb(es2w, "actT0", [128, 4, 512], BF16, nbuf=4)] * 2
    new_bufs = lg.bs + rt.bs + comb.bs + [wr_sb.b, rb_sb.b, lg.b, rt.b, wfin_t.b]
    for tl in wg + wu + wd + sa:
        new_bufs.append(tl.b)
    for tl in actT:
        new_bufs += tl.bs
    old = mixT.bs + [t_.b for t_ in agb + wob]
    P.add("dve", lambda e: e.memset(bar_t.ap[:, 2:3], 0.0), reads=[], writes=old + [bar_t.b])
    P.add("dve", lambda e: e.memset(bar_t.ap[:, 3:4], 0.0), reads=[bar_t.b], writes=new_bufs)

    P.dma("pool", wr_sb.ap, w_r.rearrange("(k p) c -> p k c", p=128), writes=[wr_sb.b], buf=wr_sb.b)
    P.dma("sp", rb_sb.ap, rbias, writes=[rb_sb.b], buf=rb_sb.b)
    P.dma("sp", wfin_t.ap, wfin, writes=[wfin_t.b], buf=wfin_t.b)

    def final_norm(i):
        xn = xn2[i % 2]
        ssap = ss2.ap[:, 8 + i:9 + i]
        P.add("act", lambda e: e.activation(out=xn.ap, in_=x1.ap[:, i, :], func=AF.Square, accum_out=ssap),
              reads=[x1.bs[i]], writes=[xn.b, ss2.bs[8 + i]])
        rms_rstd(ssap, ss2.bs[8 + i], D)
        P.add("dve", lambda e: e.scalar_tensor_tensor(
            out=x1.ap[:, i, :], in0=x1.ap[:, i, :], scalar=ssap, in1=wfin_t.ap, op0=ALU.mult, op1=ALU.mult),
            reads=[x1.bs[i], ss2.bs[8 + i], wfin_t.b], writes=[x1.bs[i]])
        P.dma("sp", out[i * 128:(i + 1) * 128, :], x1.ap[:, i, :], reads=[x1.bs[i]], writes=[out_b], buf=out_b)

    def load_expert(e_):
        P.dma("pool", wg[e_ % 2].ap, w_gate[e_].rearrange("(k p) f -> p k f", p=128), writes=[wg[e_ % 2].b],
              buf=wg[e_ % 2].b)
        P.dma("pool", wu[e_ % 2].ap, w_up[e_].rearrange("(k p) f -> p k f", p=128), writes=[wu[e_ % 2].b],
              buf=wu[e_ % 2].b)

    def load_down(e_):
        P.dma("pool", wd[0].ap, w_down[e_].rearrange("(k p) d -> p k d", p=128), writes=[wd[0].b], buf=wd[0].b)

    load_expert(0)
    load_down(0)
    load_expert(1)

    Lb = lg.b
    Rb = rt.b
    L3 = lg.ap
    R3 = rt.ap
    for i in range(8):
        pa = PA[2 + i % 2]
        for k in range(16):
            P.add("pe", lambda e, pa=pa, k=k, i=i: e.matmul(
                pa.ap[:, 0:36], lhsT=h2T.ap[:, k, i * 128:(i + 1) * 128], rhs=wr_sb.ap[:, k, :],
                start=(k == 0), stop=(k == 15)), reads=[h2T.bs[i], wr_sb.b], writes=[pa.b])
        P.add("dve", lambda e, pa=pa, i=i: e.tensor_tensor(out=L3[:, i, :], in0=pa.ap[:, 0:36], in1=rb_sb.ap,
                                                           op=ALU.add), reads=[pa.b, rb_sb.b], writes=[Lb])

    def bc(ap, w):
        return ap.to_broadcast([128, 8, w])

    def tt(out, in0, in1, op, rd=(), wr=None):
        P.add("dve", lambda e: e.tensor_tensor(out=out, in0=in0, in1=in1, op=op), reads=[Lb, Rb] + list(rd),
              writes=[Rb] if wr is None else wr)

    tt(R3[:, :, 8:10], L3[:, :, 0:2], L3[:, :, 2:4], ALU.max)
    tt(R3[:, :, 0:1], R3[:, :, 8:9], R3[:, :, 9:10], ALU.max)
    tt(R3[:, :, 4:8], L3[:, :, 0:4], bc(R3[:, :, 0:1], 4), ALU.is_ge)
    tt(R3[:, :, 8:12], L3[:, :, 0:4], bc(R3[:, :, 0:1], 4), ALU.subtract)
    P.add("act", lambda e: e.activation(out=R3[:, :, 8:12], in_=R3[:, :, 8:12], func=AF.Exp), reads=[Rb], writes=[Rb])
    tt(R3[:, :, 12:14], R3[:, :, 8:10], R3[:, :, 10:12], ALU.add)
    tt(R3[:, :, 1:2], R3[:, :, 12:13], R3[:, :, 13:14], ALU.add)
    tt(R3[:, :, 16:24], L3[:, :, 4:12], bc(R3[:, :, 4:5], 8), ALU.mult)
    for g_ in range(1, 4):
        tt(R3[:, :, 32:40], L3[:, :, 4 + 8 * g_:12 + 8 * g_], bc(R3[:, :, 4 + g_:5 + g_], 8), ALU.mult)
        tt(R3[:, :, 16:24], R3[:, :, 16:24], R3[:, :, 32:40], ALU.add)
    tt(R3[:, :, 24:28], R3[:, :, 16:20], R3[:, :, 20:24], ALU.max)
    tt(R3[:, :, 28:30], R3[:, :, 24:26], R3[:, :, 26:28], ALU.max)
    tt(R3[:, :, 2:3], R3[:, :, 28:29], R3[:, :, 29:30], ALU.max)
    tt(R3[:, :, 24:32], R3[:, :, 16:24], bc(R3[:, :, 2:3], 8), ALU.is_ge)
    P.add("dve", lambda e: e.scalar_tensor_tensor(out=R3[:, :, 32:40], in0=R3[:, :, 24:32], scalar=-1e30,
                                                  in1=R3[:, :, 16:24], op0=ALU.mult, op1=ALU.add),
          reads=[Rb], writes=[Rb])
    tt(R3[:, :, 40:44], R3[:, :, 32:36], R3[:, :, 36:40], ALU.max)
    tt(R3[:, :, 44:46], R3[:, :, 40:42], R3[:, :, 42:44], ALU.max)
    tt(R3[:, :, 3:4], R3[:, :, 44:45], R3[:, :, 45:46], ALU.max)
    tt(R3[:, :, 40:48], R3[:, :, 32:40], bc(R3[:, :, 3:4], 8), ALU.is_ge)
    tt(R3[:, :, 48:49], R3[:, :, 3:4], R3[:, :, 2:3], ALU.subtract)
    P.add("act", lambda e: e.activation(out=R3[:, :, 49:50], in_=R3[:, :, 48:49], func=AF.Exp), reads=[Rb], writes=[Rb])
    P.add("dve", lambda e: e.tensor_scalar(out=R3[:, :, 50:51], in0=R3[:, :, 49:50], scalar1=1.0, scalar2=None,
                                           op0=ALU.add), reads=[Rb], writes=[Rb])
    tt(R3[:, :, 50:51], R3[:, :, 50:51], R3[:, :, 1:2], ALU.mult)
    P.add("dve", lambda e: e.reciprocal(out=R3[:, :, 51:52], in_=R3[:, :, 50:51]), reads=[Rb], writes=[Rb])
    tt(R3[:, :, 52:53], R3[:, :, 51:52], R3[:, :, 49:50], ALU.mult)
    tt(R3[:, :, 56:64], R3[:, :, 24:32], bc(R3[:, :, 51:52], 8), ALU.mult)
    tt(R3[:, :, 32:40], R3[:, :, 40:48], bc(R3[:, :, 52:53], 8), ALU.mult)
    tt(R3[:, :, 56:64], R3[:, :, 56:64], R3[:, :, 32:40], ALU.add)
    for g_ in range(4):
        tt(comb.ap[:, :, 8 * g_:8 * g_ + 8], R3[:, :, 56:64], bc(R3[:, :, 4 + g_:5 + g_], 8), ALU.mult,
           wr=comb.bs)

    it = 0
    for ex in range(N_EXP):
        wgb, wub, wdb = wg[ex % 2], wu[ex % 2], wd[ex % 2]
        for tg in range(2):
            aT = actT[tg]
            for fc in range(4):
                pa_g = PA[0]
                pa_u = PA[1]
                s_ = sa[it % 2]
                it += 1
                for k in range(16):
                    P.add("pe", lambda e, k=k, fc=fc, tg=tg, wgb=wgb: e.matmul(
                        pa_g.ap, lhsT=wgb.ap[:, k, fc * 128:(fc + 1) * 128], rhs=h2T.ap[:, k, tg * 512:(tg + 1) * 512],
                        start=(k == 0), stop=(k == 15)), reads=[wgb.b] + h2T.bs[tg * 4:tg * 4 + 4], writes=[pa_g.b])
                for k in range(16):
                    P.add("pe", lambda e, k=k, fc=fc, tg=tg, wub=wub: e.matmul(
                        pa_u.ap, lhsT=wub.ap[:, k, fc * 128:(fc + 1) * 128], rhs=h2T.ap[:, k, tg * 512:(tg + 1) * 512],
                        start=(k == 0), stop=(k == 15)), reads=[wub.b] + h2T.bs[tg * 4:tg * 4 + 4], writes=[pa_u.b])
                P.add("act", lambda e, s_=s_: e.activation(out=s_.ap, in_=pa_g.ap, func=AF.Silu),
                      reads=[pa_g.b], writes=[s_.b])
                P.add("dve", lambda e, s_=s_, aT=aT, fc=fc: e.tensor_tensor(
                    out=aT.ap[:, fc, :], in0=pa_u.ap, in1=s_.ap, op=ALU.mult), reads=[pa_u.b, s_.b], writes=[aT.bs[fc]])
            for ii in range(4):
                i = tg * 4 + ii
                for dc in range(4):
                    pa = PA[2 + (ii * 4 + dc) % 4]
                    for fc in range(4):
                        P.add("pe", lambda e, pa=pa, fc=fc, ii=ii, dc=dc, aT=aT, wdb=wdb: e.matmul(
                            pa.ap, lhsT=aT.ap[:, fc, ii * 128:(ii + 1) * 128], rhs=wdb.ap[:, fc, dc * 512:(dc + 1) * 512],
                            start=(fc == 0), stop=(fc == 3)), reads=[aT.bs[fc], wdb.b], writes=[pa.b])
                    P.add("dve", lambda e, pa=pa, i=i, dc=dc, ex=ex: e.scalar_tensor_tensor(
                        out=x1.ap[:, i, dc * 512:(dc + 1) * 512], in0=pa.ap, scalar=comb.ap[:, i, ex:ex + 1],
                        in1=x1.ap[:, i, dc * 512:(dc + 1) * 512], op0=ALU.mult, op1=ALU.add),
                        reads=[pa.b, x1.bs[i], comb.bs[i]], writes=[x1.bs[i]])
                    if ex == N_EXP - 1 and dc == 3:
                        final_norm(i)
        if ex + 1 < N_EXP:
            load_down(ex + 1)
        if ex + 2 < N_EXP:
            load_expert(ex + 2)

    P.emit([out_b])
    return nc


def _bf(a):
    return np.asarray(a, dtype=np.float32).astype(ml_dtypes.bfloat16)


def _consts(g):
    slopes_a = [2.0 ** -(i + 1) for i in range(8)]
    slopes_b = [2.0 ** (-2 * (i + 1)) for i in range(4)]
    p = np.arange(128, dtype=np.float64)[:, None]
    m = np.arange(32, dtype=np.float64)[None, :]
    kb = []
    for h in range(2):
        kb.append(slopes_a[2 * g + h] * (p + 128.0 * (m - 28)))
    kb.append(slopes_b[g] * (p + 128.0 * (m - 28) - 256.0))
    kb = np.concatenate(kb, axis=1).astype(np.float32)
    ident = np.eye(128, dtype=np.float32)
    kk = np.arange(128)[:, None]
    qq = np.arange(128)[None, :]
    tri = np.where(kk > qq, NEG, 0.0).astype(np.float32)
    selI = np.zeros((128, 4, 128), np.float32)
    selI[:, g, :] = ident
    cbf = _bf(np.concatenate([ident, tri, selI.reshape(128, 512)], axis=1))
    lrb = np.zeros((128, 2, 16, 128), np.float32)
    for h in range(2):
        for n in range(16):
            lrb[n, h, n, :] = 1.0
        lrb[16:18, h, :, :] = -slopes_a[2 * g + h]
    lrb = _bf(lrb.reshape(128, 2 * 16 * 128))
    t = np.arange(S) % 512
    rbt = _bf(np.stack([t % 256, t - t % 256]).astype(np.float32))
    pm = np.zeros((32, 16), np.float32)
    for i in range(32):
        j = i // 2
        pm[i, j] = 1e30
        pm[i, j + 1:] = -1e30
    pastm = np.broadcast_to(pm.reshape(1, 512), (128, 512)).copy()
    selE = np.zeros((32, N_EXP, 128), np.float32)
    for e in range(N_EXP):
        selE[e, e, :] = 1.0
    selE = _bf(selE.reshape(32, N_EXP * 128))
    return kb, cbf, lrb, rbt, pastm, selE


def make_in_maps(x, norm_mix_w, w_in, lambda_q1, lambda_k1, lambda_q2, lambda_k2, diff_subln_w,
                 w_out, norm_ffn_w, w_router_group, b_router_group, w_router_expert, b_router_expert,
                 w_gate, w_up, w_down, norm_final_w):
    f = lambda a: np.ascontiguousarray(np.asarray(a, dtype=np.float32))
    x = f(x); w_in = f(w_in)[0]; w_out = f(w_out)[0]
    w_gate = f(w_gate)[0]; w_up = f(w_up)[0]; w_down = f(w_down)[0]
    wmix = f(norm_mix_w)[0].reshape(16, 128).T
    wffn = f(norm_ffn_w)[0].reshape(16, 128).T
    lam = np.concatenate([f(lambda_q1)[0], f(lambda_k1)[0], f(lambda_q2)[0], f(lambda_k2)[0]])
    lamv = np.ascontiguousarray(np.broadcast_to(lam[None, :], (128, 512)))
    subw = np.ascontiguousarray(np.broadcast_to(f(diff_subln_w)[0][None, :], (128, 256)))
    rbias = np.concatenate([f(b_router_group)[0], f(b_router_expert)[0]])
    rbias = np.ascontiguousarray(np.broadcast_to(rbias[None, :], (128, 36)))
    wfin = np.ascontiguousarray(np.broadcast_to(f(norm_final_w)[None, :], (128, D)))
    w_r = np.ascontiguousarray(np.concatenate([f(w_router_group)[0], f(w_router_expert)[0]], axis=1))
    in_maps = []
    for c in range(8):
        b, g = c // 4, c % 4
        cols = []
        for h in (2 * g, 2 * g + 1):
            cols.append(np.arange(h * 128, (h + 1) * 128))
        for h in (2 * g, 2 * g + 1):
            cols.append(1024 + np.arange(h * 128, (h + 1) * 128))
        cols.append(3072 + g * 256 + np.arange(256))
        cols.append(4096 + g * 256 + np.arange(256))
        for h in (2 * g, 2 * g + 1):
            cols.append(2048 + np.arange(h * 128, (h + 1) * 128))
        cols.append(5120 + g * 256 + np.arange(256))
        cols = np.concatenate(cols)
        w_in_c = np.ascontiguousarray(w_in[:, cols])
        rows = np.concatenate([np.concatenate([np.arange(256 * r, 256 * r + 256), 1024 + np.arange(256 * r, 256 * r + 256)])
                               for r in range(4)])
        kb, cbf, lrb, rbt, pastm, selE = _consts(g)
        ri = np.arange(32)[None, :]
        idxg = (g * 4096 + (ri // 8) * 1024 + (ri % 8) * 128 + np.arange(128)[:, None]).astype(np.int32)
        vecs = np.ascontiguousarray(np.concatenate([wmix, wffn, kb], axis=1).astype(np.float32))
        in_maps.append({
            "xb": x[b], "xres": np.ascontiguousarray(x[b, g * 1024:(g + 1) * 1024]),
            "w_in_c": w_in_c, "w_out_p": np.ascontiguousarray(w_out[rows]), "w_r": w_r,
            "w_gate": w_gate, "w_up": w_up, "w_down": w_down,
            "vecs": vecs, "lamv": lamv, "subw": subw, "rbias": rbias, "wfin": wfin, "pastm": pastm,
            "cbf": cbf, "lrb": lrb, "rbt": rbt, "idxg": np.ascontiguousarray(idxg),
        })
    return in_maps


_NC = None


def kernel(**inputs):
    global _NC
    in_maps = make_in_maps(**inputs)
    if _NC is None:
        _NC = build()
    res = run_bass_kernel_spmd(_NC, in_maps, core_ids=list(range(8)))
    outp = np.empty((2, S, D), np.float32)
    for c in range(8):
        b, g = c // 4, c % 4
        outp[b, g * 1024:(g + 1) * 1024] = res.results[c]["out"]
    return outp
```

```python
import numpy as np
import ml_dtypes
from contextlib import ExitStack
import concourse.bass as bass
import concourse.mybir as mybir
from concourse.bass_utils import run_bass_kernel_spmd

F32 = mybir.dt.float32
BF16 = mybir.dt.bfloat16
AF = mybir.ActivationFunctionType
ALU = mybir.AluOpType
AX = mybir.AxisListType

D = 2048
S = 4096
NT = 32
EPS = 1e-6
NEG = -30000.0
N_EXP = 32
DE = 512
SEM_BLK = 2000


class Buf:
    __slots__ = ("name", "last_w", "readers", "dma_sem", "dma_cnt")

    def __init__(self, name):
        self.name = name
        self.last_w = None
        self.readers = {}
        self.dma_sem = None
        self.dma_cnt = 0


class Op:
    __slots__ = ("eng", "fn", "deps", "kind", "buf", "dma_val", "sig", "semidx", "idx")


class Prog:
    def __init__(self, nc):
        self.nc = nc
        self.ops = []
        self.engs = {"pe": nc.tensor, "act": nc.scalar, "dve": nc.vector, "pool": nc.gpsimd, "sp": nc.sync}

    def add(self, eng, fn, reads=(), writes=(), kind="c", buf=None):
        op = Op()
        op.eng, op.fn, op.kind, op.buf = eng, fn, kind, buf
        op.sig = False
        op.semidx = 0
        op.idx = len(self.ops)
        deps = set()
        for b in reads:
            if b.last_w is not None:
                deps.add(b.last_w)
        for b in writes:
            if b.last_w is not None:
                deps.add(b.last_w)
            for r in b.readers.values():
                if isinstance(r, list):
                    deps.update(r)
                else:
                    deps.add(r)
        deps.discard(op.idx)
        op.deps = deps
        key = eng if kind == "c" else "dma"
        for b in reads:
            if key == "dma":
                b.readers.setdefault("dma", []).append(op.idx)
            else:
                b.readers[key] = op.idx
        for b in writes:
            b.last_w = op.idx
            b.readers = {}
        if kind == "d":
            buf.dma_cnt += 1
            op.dma_val = 16 * buf.dma_cnt
        elif kind == "cc":
            buf.dma_cnt += 1
            op.dma_val = buf.dma_cnt
        self.ops.append(op)
        return op

    def dma(self, eng, out, in_, reads=(), writes=(), buf=None):
        return self.add(eng, lambda e: e.dma_start(out=out, in_=in_), reads, writes, kind="d", buf=buf)

    def emit(self, final_bufs):
        nc = self.nc
        ops = self.ops
        for op in ops:
            for d in op.deps:
                p = ops[d]
                if p.kind == "c":
                    if p.eng == op.eng and op.kind == "c" and op.eng == "pe":
                        continue
                    p.sig = True
        cnt = {e: 0 for e in self.engs}
        for op in ops:
            if op.kind == "c" and op.sig:
                cnt[op.eng] += 1
                op.semidx = cnt[op.eng]
        sems = {}

        def eng_sem(e, idx):
            blk = (idx - 1) // SEM_BLK
            k = (e, blk)
            if k not in sems:
                sems[k] = nc.alloc_semaphore(f"s_{e}_{blk}")
            return sems[k], (idx - 1) % SEM_BLK + 1

        def buf_sem(b):
            if b.dma_sem is None:
                b.dma_sem = nc.alloc_semaphore(f"d_{b.name}")
            return b.dma_sem

        waited_eng = {e: {p: 0 for p in self.engs} for e in self.engs}
        waited_buf = {e: {} for e in self.engs}
        for op in ops:
            e = self.engs[op.eng]
            need_eng = {}
            need_buf = {}
            for d in op.deps:
                p = ops[d]
                if p.kind == "c":
                    if p.eng == op.eng and op.kind == "c" and op.eng == "pe":
                        continue
                    if p.semidx > need_eng.get(p.eng, 0):
                        need_eng[p.eng] = p.semidx
                else:
                    if p.dma_val > need_buf.get(id(p.buf), (None, 0))[1]:
                        need_buf[id(p.buf)] = (p.buf, p.dma_val)
            for pe_, idx in need_eng.items():
                if waited_eng[op.eng][pe_] < idx:
                    s, v = eng_sem(pe_, idx)
                    e.wait_ge(s, v)
                    waited_eng[op.eng][pe_] = idx
            for bid, (b, v) in need_buf.items():
                if waited_buf[op.eng].get(bid, 0) < v:
                    e.wait_ge(buf_sem(b), v)
                    waited_buf[op.eng][bid] = v
            ins = op.fn(e)
            if op.kind == "c":
                if op.sig:
                    s, v = eng_sem(op.eng, op.semidx)
                    ins.then_inc(s, 1)
            elif op.kind == "d":
                ins.then_inc(buf_sem(op.buf), 16)
            else:
                ins.then_inc(buf_sem(op.buf))
        sp = nc.sync
        for b in final_bufs:
            sp.wait_ge(buf_sem(b), 16 * b.dma_cnt)


class T:
    def __init__(self, ap, name, nbuf=1):
        self.ap = ap
        self.b = Buf(name)
        self.bs = [Buf(f"{name}_{i}") for i in range(nbuf)] if nbuf > 1 else [self.b]


def build(stage=99, dbg=False):
    nc = bass.Bass("TRN2", target_bir_lowering=False)
    P = Prog(nc)
    def din(name, shape, dt=F32):
        return nc.dram_tensor(name, shape, dt, kind="ExternalInput").ap()

    xb = din("xb", [S, D])
    xres = din("xres", [1024, D])
    w_in_c = din("w_in_c", [D, 1536])
    w_out_p = din("w_out_p", [D, D])
    w_r = din("w_r", [D, 36])
    if stage == 99:
        w_gate = din("w_gate", [N_EXP, D, DE])
        w_up = din("w_up", [N_EXP, D, DE])
        w_down = din("w_down", [N_EXP, DE, D])
    vecs = din("vecs", [128, 16 + 16 + 96])
    lamv = din("lamv", [128, 512])
    subw = din("subw", [128, 256])
    rbias = din("rbias", [128, 36])
    wfin = din("wfin", [128, D])
    pastm = din("pastm", [128, 512])
    cbf = din("cbf", [128, 256 + 512], BF16)
    lrb_d = din("lrb", [128, 2 * 16 * 128], BF16)
    rbt_d = din("rbt", [2, S], BF16)
    out = nc.dram_tensor("out", [1024, D], F32, kind="ExternalOutput").ap()
    bounce = [nc.dram_tensor(f"bounce{j}", [1024, 512], BF16) for j in range(4)]
    agbig = nc.dram_tensor("agbig", [4 * 4096, 512], BF16)
    idxg_d = nc.dram_tensor("idxg", [128, 32], mybir.dt.int32, kind="ExternalInput").ap()
    bounce_b = [Buf(f"bounce{j}") for j in range(4)]
    agout_b = [Buf(f"agout{j}") for j in range(4)]
    out_b = Buf("outd")
    dbg_outs = {}

    PA = [T(nc.alloc_psum_tensor(f"pa{i}", [128, 512], F32).ap(), f"pa{i}") for i in range(8)]
    PT = []
    for i in range(2):
        v = T(PA[6 + i].ap.bitcast(BF16), f"ptv{i}")
        v.b = PA[6 + i].b
        v.bs = [v.b]
        PT.append(v)

    es_all = ExitStack()

    def sb(es, name, shape, dt, nbuf=1):
        h = es.enter_context(nc.sbuf_tensor("sb_" + name, shape, dt))
        return T(h.ap() if hasattr(h, "ap") and callable(h.ap) else h, name, nbuf)

    vecs_t = sb(es_all, "vecs", [128, 128], F32)
    cbf_t = sb(es_all, "cbf", [128, 768], BF16)
    P.dma("sp", vecs_t.ap, vecs, writes=[vecs_t.b], buf=vecs_t.b)
    P.dma("sp", cbf_t.ap, cbf, writes=[cbf_t.b], buf=cbf_t.b)
    epsT = sb(es_all, "epsT", [128, 1], F32)
    bar_t = sb(es_all, "bar_t", [128, 8], F32)
    P.add("dve", lambda e: e.memset(epsT.ap, EPS), writes=[epsT.b])
    wmix = vecs_t.ap[:, 0:16]
    wffn = vecs_t.ap[:, 16:32]
    ident = cbf_t.ap[:, 0:128]
    tri = cbf_t.ap[:, 128:256]
    CONST = [vecs_t.b, cbf_t.b]

    es1 = ExitStack()
    QT = [sb(es1, f"qt{i}", [128, S], BF16, nbuf=8) for i in range(4)]
    KT = [sb(es1, f"kt{i}", [128, S], BF16, nbuf=8) for i in range(4)]
    VA = [sb(es1, f"va{i}", [128, NT, 129], BF16, nbuf=8) for i in range(2)]
    VD = sb(es1, "vd", [128, NT, 257], BF16, nbuf=8)
    ksum = sb(es1, "ksum", [128, 2, 16], F32)
    ss1 = sb(es1, "ss1", [128, NT], F32, nbuf=NT)
    rstd1 = sb(es1, "rstd1", [128, NT], F32, nbuf=NT)

    P.add("dve", lambda e: e.memset(ksum.ap, 0.0), writes=[ksum.b])
    P.add("pool", lambda e: e.memset(VA[0].ap[:, :, 128:129], 1.0), writes=VA[0].bs)
    P.add("pool", lambda e: e.memset(VA[1].ap[:, :, 128:129], 1.0), writes=VA[1].bs)
    P.add("pool", lambda e: e.memset(VD.ap[:, :, 256:257], 1.0), writes=VD.bs)

    es1a = ExitStack()
    w_in_sb = sb(es1a, "w_in_sb", [128, 16, 1536], BF16, nbuf=9)
    xts = [sb(es1a, f"xt{i}", [128, D], F32) for i in range(2)]
    xns = [sb(es1a, f"xn{i}", [128, D], BF16) for i in range(2)]
    hTs = [sb(es1a, f"hT{i}", [128, 16, 512], BF16) for i in range(2)]

    w_in_v = w_in_c.rearrange("(k p) c -> p k c", p=128)
    for c in range(8):
        P.dma("pool", w_in_sb.ap[:, :, c * 128:(c + 1) * 128], w_in_v[:, :, c * 128:(c + 1) * 128],
              writes=[w_in_sb.bs[c]], buf=w_in_sb.bs[c])
    P.dma("pool", w_in_sb.ap[:, :, 1024:1536], w_in_v[:, :, 1024:1536], writes=[w_in_sb.bs[8]], buf=w_in_sb.bs[8])

    QSCALE = 128.0 ** -0.5
    pa_ctr = [0]

    def norm_T(g, tt):
        hT = hTs[g % 2]
        t = g * 4 + tt
        xt = xts[t % 2]
        xn = xns[t % 2]
        P.dma("sp", xt.ap, xb[t * 128:(t + 1) * 128, :], writes=[xt.b], buf=xt.b)
        P.add("act", lambda e: e.activation(out=xn.ap, in_=xt.ap, func=AF.Square, accum_out=ss1.ap[:, t:t + 1]),
              reads=[xt.b], writes=[xn.b, ss1.bs[t]])
        P.add("act", lambda e: e.activation(out=rstd1.ap[:, t:t + 1], in_=ss1.ap[:, t:t + 1], func=AF.Sqrt,
                                            scale=1.0 / D, bias=epsT.ap),
              reads=[ss1.bs[t], epsT.b], writes=[rstd1.bs[t]])
        P.add("dve", lambda e: e.reciprocal(out=rstd1.ap[:, t:t + 1], in_=rstd1.ap[:, t:t + 1]),
              reads=[rstd1.bs[t]], writes=[rstd1.bs[t]])
        P.add("dve", lambda e: e.tensor_scalar(out=xn.ap, in0=xt.ap, scalar1=rstd1.ap[:, t:t + 1], scalar2=None,
                                               op0=ALU.mult), reads=[xt.b, rstd1.bs[t]], writes=[xn.b])

    def norm_pe(g, tt):
        hT = hTs[g % 2]
        t = g * 4 + tt
        xn = xns[t % 2]
        for r in range(2):
            pt = PT[r]
            for k in range(8):
                P.add("pe", lambda e, pt=pt, k=k, r=r: e.transpose(
                    out=pt.ap[:, k * 128:(k + 1) * 128], in_=xn.ap[:, (r * 8 + k) * 128:(r * 8 + k + 1) * 128],
                    identity=ident), reads=[xn.b] + CONST, writes=[pt.b])
            P.add("dve", lambda e, pt=pt, r=r: e.tensor_tensor(
                out=hT.ap[:, r * 8:(r + 1) * 8, tt * 128:(tt + 1) * 128],
                in0=pt.ap.rearrange("p (k n) -> p k n", k=8),
                in1=wmix[:, r * 8:(r + 1) * 8].unsqueeze(2).to_broadcast([128, 8, 128]),
                op=ALU.mult), reads=[pt.b] + CONST, writes=[hT.b])

    def chunk_T(g, c):
        hT = hTs[g % 2]
        pa = PA[pa_ctr[0] % 2]
        pa_ctr[0] += 1
        for k in range(16):
            P.add("pe", lambda e, k=k: e.matmul(
                pa.ap, lhsT=w_in_sb.ap[:, k, c * 128:(c + 1) * 128], rhs=hT.ap[:, k, :],
                start=(k == 0), stop=(k == 15)), reads=[hT.b, w_in_sb.bs[c]], writes=[pa.b])
        cols = slice(g * 512, (g + 1) * 512)
        if c in (0, 1, 4, 5):
            dst = QT[c if c < 2 else c - 2]
            P.add("act", lambda e: e.activation(out=dst.ap[:, cols], in_=pa.ap, func=AF.Copy, scale=QSCALE),
                  reads=[pa.b], writes=[dst.bs[g]])
        elif c in (2, 3):
            dst = KT[c - 2]
            for hf in range(2):
                P.add("act", lambda e, hf=hf: e.activation(
                    out=dst.ap[:, g * 512 + hf * 256:g * 512 + (hf + 1) * 256],
                    in_=pa.ap[:, hf * 256:(hf + 1) * 256], func=AF.Identity,
                    accum_out=ksum.ap[:, c - 2, 2 * g + hf:2 * g + hf + 1]),
                    reads=[pa.b], writes=[dst.bs[g], ksum.b])
        else:
            dst = KT[c - 4]
            P.add("act", lambda e: e.activation(out=dst.ap[:, cols], in_=pa.ap, func=AF.Copy),
                  reads=[pa.b], writes=[dst.bs[g]])

    def chunk_V(g, tt):
        hT = hTs[g % 2]
        t = g * 4 + tt
        pa = PA[pa_ctr[0] % 2]
        pa_ctr[0] += 1
        for k in range(16):
            P.add("pe", lambda e, k=k: e.matmul(
                pa.ap, lhsT=hT.ap[:, k, tt * 128:(tt + 1) * 128], rhs=w_in_sb.ap[:, k, 1024:1536],
                start=(k == 0), stop=(k == 15)), reads=[hT.b, w_in_sb.bs[8]], writes=[pa.b])
        P.add("dve", lambda e: e.tensor_copy(out=VA[0].ap[:, t, 0:128], in_=pa.ap[:, 0:128]),
              reads=[pa.b], writes=[VA[0].bs[g]])
        P.add("dve", lambda e: e.tensor_copy(out=VA[1].ap[:, t, 0:128], in_=pa.ap[:, 128:256]),
              reads=[pa.b], writes=[VA[1].bs[g]])
        P.add("dve", lambda e: e.tensor_copy(out=VD.ap[:, t, 0:256], in_=pa.ap[:, 256:512]),
              reads=[pa.b], writes=[VD.bs[g]])

    norm_T(0, 0)
    norm_T(0, 1)
    norm_pe(0, 0)
    norm_T(0, 2)
    norm_pe(0, 1)
    norm_T(0, 3)
    norm_pe(0, 2)
    norm_pe(0, 3)
    for g in range(8):
        ci = 0
        for c in range(12):
            if c < 8:
                chunk_T(g, c)
            else:
                chunk_V(g, c - 8)
            ci += 1
            if g + 1 < 8:
                if ci in (1, 4, 7, 10):
                    norm_T(g + 1, (ci - 1) // 3)
                if ci in (3, 6, 9, 12):
                    norm_pe(g + 1, (ci - 3) // 3)
    es1a.close()

    if dbg and stage == 1:
        for nm, tt_ in (("qt0", QT[0]), ("kt1", KT[1]), ("qt3", QT[3])):
            d = nc.dram_tensor("dbg_" + nm, [128, S], BF16, kind="ExternalOutput").ap()
            b = Buf("dbg_" + nm)
            P.dma("sp", d, tt_.ap, reads=tt_.bs, writes=[b], buf=b)
            dbg_outs[nm] = b
        d = nc.dram_tensor("dbg_vd", [128, NT * 257], BF16, kind="ExternalOutput").ap()
        b = Buf("dbg_vd")
        P.dma("sp", d, VD.ap.rearrange("p t c -> p (t c)"), reads=VD.bs, writes=[b], buf=b)
        dbg_outs["vd"] = b
        d = nc.dram_tensor("dbg_ksum", [128, 32], F32, kind="ExternalOutput").ap()
        b = Buf("dbg_ksum")
        P.dma("sp", d, ksum.ap.rearrange("p a c -> p (a c)"), reads=[ksum.b], writes=[b], buf=b)
        dbg_outs["ksum"] = b
        P.emit(list(dbg_outs.values()))
        return nc

    es1b = ExitStack()
    RB = [sb(es1b, f"rb{h}", [128, S], BF16) for h in range(2)]
    lrb = sb(es1b, "lrb", [128, 2 * 16 * 128], BF16)
    pTs = [sb(es1b, f"pT{i}", [128, 512], BF16) for i in range(5)]
    O1n = sb(es1b, "o1n", [128, NT, 256], F32, nbuf=NT)
    mixed = sb(es1b, "mixed", [128, NT, 512], BF16, nbuf=NT)
    lam_t = sb(es1b, "lam_t", [128, 512], F32)
    subw_t = sb(es1b, "subw_t", [128, 256], F32)
    pastm_t = sb(es1b, "pastm_t", [128, 512], F32)
    small = sb(es1b, "small", [128, 64], F32)
    gall = sb(es1b, "gall", [128, 32, 16], F32)
    max8 = sb(es1b, "max8", [128, 32, 8], F32)
    selb = sb(es1b, "selb", [128, 32, 16], BF16)
    selb2 = sb(es1b, "selb2", [128, 32, 16], F32)
    km = sb(es1b, "km", [128, 4, 16], F32)
    kmb = sb(es1b, "kmb", [128, 2, 16], BF16)
    recs = sb(es1b, "recs", [128, 8], F32, nbuf=8)
    dtmp = sb(es1b, "dtmp", [128, 2, 256], F32, nbuf=2)
    djunk = sb(es1b, "djunk", [128, 256], F32)

    old1 = list(w_in_sb.bs)
    for tl in xts + xns + hTs:
        old1.append(tl.b)
    new1 = []
    for tl in RB + [lrb] + pTs + [O1n, mixed, lam_t, subw_t, pastm_t, small, gall, max8, selb, selb2, km, kmb, recs,
                                  dtmp, djunk]:
        new1 += tl.bs
        if tl.b not in new1:
            new1.append(tl.b)
    P.add("dve", lambda e: e.memset(bar_t.ap[:, 6:7], 0.0), reads=[], writes=old1 + [bar_t.b])
    P.add("dve", lambda e: e.memset(bar_t.ap[:, 7:8], 0.0), reads=[bar_t.b], writes=new1)

    P.dma("sp", lam_t.ap, lamv, writes=[lam_t.b], buf=lam_t.b)
    P.dma("sp", subw_t.ap, subw, writes=[subw_t.b], buf=subw_t.b)
    P.dma("sp", pastm_t.ap, pastm, writes=[pastm_t.b], buf=pastm_t.b)
    P.dma("sp", lrb.ap, lrb_d, writes=[lrb.b], buf=lrb.b)
    for h in range(2):
        P.add("pool", lambda e, h=h: e.memset(RB[h].ap, 0.0), writes=[RB[h].b])
        P.dma("sp", RB[h].ap[16:18, :], rbt_d, reads=[], writes=[RB[h].b], buf=RB[h].b)

    P.add("dve", lambda e: e.tensor_tensor(out=djunk.ap[:, 0:128], in0=lam_t.ap[:, 0:128], in1=lam_t.ap[:, 128:256],
                                           op=ALU.mult), reads=[lam_t.b], writes=[djunk.b])
    P.add("dve", lambda e: e.reduce_sum(out=small.ap[:, 0:1], in_=djunk.ap[:, 0:128], axis=AX.X),
          reads=[djunk.b], writes=[small.b])
    P.add("dve", lambda e: e.tensor_tensor(out=djunk.ap[:, 128:256], in0=lam_t.ap[:, 256:384], in1=lam_t.ap[:, 384:512],
                                           op=ALU.mult), reads=[lam_t.b], writes=[djunk.b])
    P.add("dve", lambda e: e.reduce_sum(out=small.ap[:, 1:2], in_=djunk.ap[:, 128:256], axis=AX.X),
          reads=[djunk.b], writes=[small.b])
    P.add("act", lambda e: e.activation(out=small.ap[:, 2:4], in_=small.ap[:, 0:2], func=AF.Exp),
          reads=[small.b], writes=[small.b])
    P.add("dve", lambda e: e.scalar_tensor_tensor(out=small.ap[:, 4:5], in0=small.ap[:, 3:4], scalar=-0.2,
                                                  in1=small.ap[:, 2:3], op0=ALU.add, op1=ALU.subtract),
          reads=[small.b], writes=[small.b])
    neglam = small.ap[:, 4:5]


    def _dbg_exit(tl, shape2):
        d = nc.dram_tensor("dbg_x", shape2, tl.ap.dtype, kind="ExternalOutput").ap()
        b = Buf("dbg_x")
        src = tl.ap
        if len(src.shape) == 3:
            src = src.rearrange("p a b -> p (a b)")
        P.dma("sp", d, src, reads=tl.bs + [tl.b], writes=[b], buf=b)
        P.emit([b])
        return nc

    if dbg and stage == 11:
        return _dbg_exit(small, [128, 64])
    kb_tab = [vecs_t.ap[:, 32 + 32 * i:64 + 32 * i] for i in range(3)]

    for h in range(2):
        P.add("dve", lambda e, h=h: e.tensor_scalar(out=km.ap[:, 0, :], in0=ksum.ap[:, h, :], scalar1=1.0 / 256,
                                                    scalar2=None, op0=ALU.mult), reads=[ksum.b], writes=[km.b])
        P.add("dve", lambda e: e.tensor_copy(out=kmb.ap[:, 0, :], in_=km.ap[:, 0, :]), reads=[km.b], writes=[kmb.b])
        P.add("dve", lambda e: e.tensor_copy(out=km.ap[:, 1, :], in_=kmb.ap[:, 0, :]), reads=[kmb.b], writes=[km.b])
        P.add("dve", lambda e: e.tensor_tensor(out=km.ap[:, 2, :], in0=km.ap[:, 0, :], in1=km.ap[:, 1, :],
                                               op=ALU.subtract), reads=[km.b], writes=[km.b])
        P.add("dve", lambda e: e.tensor_copy(out=kmb.ap[:, 1, :], in_=km.ap[:, 2, :]), reads=[km.b], writes=[kmb.b])
        pg = PA[2]
        for i in range(NT):
            for part in range(2):
                P.add("pe", lambda e, i=i, part=part, h=h: e.matmul(
                    pg.ap[:, i * 16:(i + 1) * 16], lhsT=QT[h].ap[:, i * 128:(i + 1) * 128], rhs=kmb.ap[:, part, :],
                    start=(part == 0), stop=(part == 1)), reads=[QT[h].bs[i // 4], kmb.b], writes=[pg.b])
        P.add("dve", lambda e: e.tensor_tensor(out=gall.ap.rearrange("p a b -> p (a b)"), in0=pg.ap, in1=pastm_t.ap,
                                               op=ALU.add), reads=[pg.b, pastm_t.b], writes=[gall.b])
        if dbg and stage == 12:
            return _dbg_exit(gall, [128, 512])
        for i in range(NT):
            P.add("dve", lambda e, i=i: e.max(out=max8.ap[:, i, :], in_=gall.ap[:, i, :]),
                  reads=[gall.b], writes=[max8.b])
        P.add("dve", lambda e: e.tensor_tensor(out=selb2.ap, in0=gall.ap,
                                               in1=max8.ap[:, :, 3:4].to_broadcast([128, 32, 16]),
                                               op=ALU.is_lt), reads=[gall.b, max8.b], writes=[selb2.b])
        P.add("dve", lambda e: e.tensor_scalar(out=selb.ap, in0=selb2.ap, scalar1=NEG, scalar2=None, op0=ALU.mult),
              reads=[selb2.b], writes=[selb.b])
        if dbg and stage == 13:
            return _dbg_exit(selb, [128, 512])
        for r in range(4):
            pt = PT[r % 2]
            for k in range(8):
                i = r * 8 + k
                P.add("pe", lambda e, pt=pt, k=k, i=i: e.transpose(
                    out=pt.ap[0:16, k * 128:(k + 1) * 128], in_=selb.ap[:, i, :], identity=ident),
                    reads=[selb.b] + CONST, writes=[pt.b])
            P.add("act", lambda e, pt=pt, r=r, h=h: e.activation(
                out=RB[h].ap[0:16, r * 1024:(r + 1) * 1024], in_=pt.ap[0:16, :], func=AF.Copy),
                reads=[pt.b], writes=[RB[h].b])

    if dbg and stage == 15:
        d = nc.dram_tensor("dbg_rb", [18, S], BF16, kind="ExternalOutput").ap()
        b = Buf("dbg_rb")
        P.dma("sp", d, RB[1].ap, reads=[RB[1].b], writes=[b], buf=b)
        d2 = nc.dram_tensor("dbg_gall", [128, 512], F32, kind="ExternalOutput").ap()
        b2 = Buf("dbg_gall")
        P.dma("sp", d2, gall.ap.rearrange("p a b -> p (a b)"), reads=[gall.b], writes=[b2], buf=b2)
        P.emit([b, b2])
        return nc

    sc_i = [0]

    tasks = []
    SCB = [PA[0], PA[1], PA[6], PA[7]]

    def attn_pass(qT, kT, V, dv1, kbias, rb, lrb_h, out_fn, qts=range(8)):
        Oacc = PA[2:6]
        for qt in qts:
            nkt = 4 * qt + 4
            for kt in range(nkt):
                j = kt - 4 * qt
                q0 = 128 * j if j > 0 else 0
                ps = SCB[sc_i[0] % 4]
                pT = pTs[sc_i[0] % 5]
                sc_i[0] += 1
                qcols = slice(qt * 512 + q0, (qt + 1) * 512)
                more = (rb is not None) or (j >= 0)

                def qk(ps=ps, kt=kt, q0=q0, qcols=qcols, more=more, j=j, qt=qt):
                    P.add("pe", lambda e: e.matmul(
                        ps.ap[:, q0:512], lhsT=kT.ap[:, kt * 128:(kt + 1) * 128], rhs=qT.ap[:, qcols],
                        start=True, stop=not more), reads=[kT.bs[kt // 4], qT.bs[qt]], writes=[ps.b])
                    if rb is not None:
                        n = kt // 2
                        P.add("pe", lambda e: e.matmul(
                            ps.ap[:, q0:512], lhsT=lrb.ap[:, (lrb_h * 16 + n) * 128:(lrb_h * 16 + n + 1) * 128],
                            rhs=rb.ap[:, qcols], start=False, stop=(j < 0)), reads=[rb.b, lrb.b], writes=[ps.b])
                    if j >= 0:
                        P.add("pe", lambda e: e.matmul(
                            ps.ap[:, q0:q0 + 128], lhsT=ident, rhs=tri, start=False, stop=True),
                            reads=CONST, writes=[ps.b])

                def rest(ps=ps, pT=pT, kt=kt, q0=q0, j=j, qt=qt):
                    m = j + 28
                    P.add("act", lambda e: e.activation(
                        out=pT.ap[:, q0:512], in_=ps.ap[:, q0:512], func=AF.Exp, bias=kbias[:, m:m + 1], scale=1.0),
                        reads=[ps.b] + CONST, writes=[pT.b])
                    for sub in range(q0 // 128, 4):
                        last = (kt == 4 * qt + sub)
                        P.add("pe", lambda e, sub=sub, last=last: e.matmul(
                            Oacc[sub].ap[:, 0:dv1], lhsT=pT.ap[:, sub * 128:(sub + 1) * 128], rhs=V.ap[:, kt, 0:dv1],
                            start=(kt == 0), stop=last), reads=[pT.b, V.bs[kt // 4]], writes=[Oacc[sub].b])
                        if last:
                            out_fn(qt * 4 + sub, Oacc[sub])

                tasks.append((qk, rest))

    DEPTH = 3

    def run_tasks():
        n = len(tasks)
        for i in range(min(DEPTH, n)):
            tasks[i][0]()
        for i in range(n):
            if i + DEPTH < n:
                tasks[i + DEPTH][0]()
            tasks[i][1]()
        tasks.clear()

    def moba_out(h):
        def f(tile, oa):
            rb_ = recs.bs[tile % 8]
            rc = recs.ap[:, tile % 8:tile % 8 + 1]
            P.add("dve", lambda e: e.reciprocal(out=rc, in_=oa.ap[:, 128:129]), reads=[oa.b], writes=[rb_])
            P.add("dve", lambda e: e.tensor_scalar(out=mixed.ap[:, tile, h * 128:(h + 1) * 128], in0=oa.ap[:, 0:128],
                                                   scalar1=rc, scalar2=None, op0=ALU.mult),
                  reads=[oa.b, rb_], writes=[mixed.bs[tile]])
        return f

    def diff1_out(tile, oa):
        rb_ = recs.bs[tile % 8]
        rc = recs.ap[:, tile % 8:tile % 8 + 1]
        P.add("dve", lambda e: e.reciprocal(out=rc, in_=oa.ap[:, 256:257]), reads=[oa.b], writes=[rb_])
        P.add("dve", lambda e: e.tensor_scalar(out=O1n.ap[:, tile, :], in0=oa.ap[:, 0:256], scalar1=rc, scalar2=None,
                                               op0=ALU.mult), reads=[oa.b, rb_], writes=[O1n.bs[tile]])

    def diff2_out(tile, oa):
        rb_ = recs.bs[tile % 8]
        rc = recs.ap[:, tile % 8:tile % 8 + 1]
        P.add("dve", lambda e: e.reciprocal(out=rc, in_=oa.ap[:, 256:257]), reads=[oa.b], writes=[rb_])
        P.add("dve", lambda e: e.tensor_tensor(out=rc, in0=rc, in1=neglam, op=ALU.mult),
              reads=[rb_, small.b], writes=[rb_])
        P.add("dve", lambda e: e.scalar_tensor_tensor(out=O1n.ap[:, tile, :], in0=oa.ap[:, 0:256], scalar=rc,
                                                      in1=O1n.ap[:, tile, :], op0=ALU.mult, op1=ALU.add),
              reads=[oa.b, rb_, O1n.bs[tile]], writes=[O1n.bs[tile]])
        if tile % 8 == 7:
            t0 = tile - 7
            sb_ = dtmp.bs[(tile // 8) % 2]
            sq = dtmp.ap[:, (tile // 8) % 2, 0:8]
            rs = dtmp.ap[:, (tile // 8) % 2, 8:16]
            for t in range(t0, t0 + 8):
                P.add("act", lambda e, t=t: e.activation(out=djunk.ap, in_=O1n.ap[:, t, :], func=AF.Square,
                                                         accum_out=sq[:, t - t0:t - t0 + 1]),
                      reads=[O1n.bs[t]], writes=[djunk.b, sb_])
            P.add("act", lambda e: e.activation(out=rs, in_=sq, func=AF.Ln, scale=1.0 / 256, bias=epsT.ap),
                  reads=[sb_, epsT.b], writes=[sb_])
            P.add("act", lambda e: e.activation(out=rs, in_=rs, func=AF.Exp, scale=-0.5), reads=[sb_], writes=[sb_])
            P.add("dve", lambda e: e.tensor_scalar(out=rs, in0=rs, scalar1=0.8, scalar2=None, op0=ALU.mult),
                  reads=[sb_], writes=[sb_])
            for t in range(t0, t0 + 8):
                P.add("dve", lambda e, t=t: e.scalar_tensor_tensor(
                    out=mixed.ap[:, t, 256:512], in0=O1n.ap[:, t, :], scalar=rs[:, t - t0:t - t0 + 1],
                    in1=subw_t.ap, op0=ALU.mult, op1=ALU.mult),
                    reads=[O1n.bs[t], sb_, subw_t.b], writes=[mixed.bs[t]])

    def exchange(j):
        P.dma("sp", bounce[j].ap().rearrange("(t p) f -> p t f", p=128), mixed.ap[:, j * 8:(j + 1) * 8, :],
              reads=mixed.bs[j * 8:(j + 1) * 8], writes=[bounce_b[j]], buf=bounce_b[j])
        P.add("pool", lambda e: e.collective_compute(
            "AllGather", ALU.bypass, replica_groups=[[0, 1, 2, 3], [4, 5, 6, 7]],
            ins=[bounce[j].ap().opt()], outs=[agbig.ap()[j * 4096:(j + 1) * 4096, :].opt()]),
            reads=[bounce_b[j]], writes=[agout_b[j]], kind="cc", buf=agout_b[j])

    passes = [(QT[0], KT[0], VA[0], 129, kb_tab[0], RB[0], 0, moba_out(0)),
              (QT[1], KT[1], VA[1], 129, kb_tab[1], RB[1], 1, moba_out(1)),
              (QT[2], KT[2], VD, 257, kb_tab[2], None, 0, diff1_out),
              (QT[3], KT[3], VD, 257, kb_tab[2], None, 0, diff2_out)]
    if not (dbg and stage == 17):
        for j in range(4):
            for pz in passes:
                attn_pass(*pz, qts=(2 * j, 2 * j + 1))
            if not (dbg and stage == 2):
                tasks.append((lambda: None, lambda j=j: exchange(j)))
        run_tasks()
    if dbg and stage == 17:
        attn_pass(QT[0], KT[0], VA[0], 129, kb_tab[0], RB[0], 0, moba_out(0))
        run_tasks()
        d = nc.dram_tensor("dbg_mixed", [128, NT * 512], BF16, kind="ExternalOutput").ap()
        b = Buf("dbg_mixed")
        P.dma("sp", d, mixed.ap.rearrange("p t c -> p (t c)"), reads=mixed.bs, writes=[b], buf=b)
        P.emit([b])
        return nc

    if dbg and stage == 2:
        d = nc.dram_tensor("dbg_mixed", [128, NT * 512], BF16, kind="ExternalOutput").ap()
        b = Buf("dbg_mixed")
        P.dma("sp", d, mixed.ap.rearrange("p t c -> p (t c)"), reads=mixed.bs, writes=[b], buf=b)
        P.emit([b])
        return nc

    es1b.close()
    es1.close()

    allb = Buf("phase_barrier")

    def barrier():
        for en in ("pe", "act", "dve", "pool", "sp"):
            pass

    es2 = ExitStack()
    x1 = sb(es2, "x1", [128, 8, D], F32, nbuf=8)
    ss2 = sb(es2, "ss2", [128, 16], F32, nbuf=16)
    h2T = sb(es2, "h2T", [128, 16, 1024], BF16, nbuf=8)
    xn2 = [sb(es2, f"xn2_{i}", [128, D], BF16) for i in range(2)]
    selI = cbf_t.ap[:, 256:768]

    es2a = ExitStack()
    mixT = sb(es2a, "mixT", [128, 16, 1024], BF16, nbuf=16)
    agb = [sb(es2a, f"agb{i}", [128, 32, 512], BF16) for i in range(1)]
    wob = [sb(es2a, f"wob{i}", [128, 16, 512], BF16) for i in range(2)]

    p1_bufs = []
    for tl in QT + KT + VA + [VD, ksum, ss1, rstd1, mixed, O1n, lam_t, subw_t, pastm_t, small, gall, max8, selb, selb2,
                              km, kmb, recs, dtmp, djunk, lrb] + RB + pTs + xts + xns + hTs + [w_in_sb]:
        p1_bufs += tl.bs
        if tl.b not in p1_bufs:
            p1_bufs.append(tl.b)
    P.add("dve", lambda e: e.memset(bar_t.ap[:, 0:1], 0.0), reads=[], writes=p1_bufs + [bar_t.b])
    new_bufs = x1.bs + ss2.bs + mixT.bs + h2T.bs + [t_.b for t_ in agb + wob + xn2]
    P.add("dve", lambda e: e.memset(bar_t.ap[:, 1:2], 0.0), reads=[bar_t.b], writes=new_bufs)

    for i in range(8):
        P.dma("sp", x1.ap[:, i, :], xres[i * 128:(i + 1) * 128, :], writes=[x1.bs[i]], buf=x1.bs[i])

    idx_t = sb(es2a, "idx_t", [128, 32], mybir.dt.int32)
    P.add("dve", lambda e: e.memset(bar_t.ap[:, 2:3], 0.0), reads=[bar_t.b], writes=[idx_t.b])
    P.dma("sp", idx_t.ap, idxg_d, writes=[idx_t.b], buf=idx_t.b)
    ab = agb[0]
    for ri in range(32):
        P.add("pool", lambda e, ri=ri: e.indirect_dma_start(
            out=ab.ap[:, ri, :], out_offset=None, in_=agbig.ap(),
            in_offset=bass.IndirectOffsetOnAxis(ap=idx_t.ap[:, ri:ri + 1], axis=0)),
            reads=[idx_t.b] + agout_b, writes=[ab.b], kind="d", buf=ab.b)
    ev = 0
    for r in range(4):
        for fc in range(4):
            pt = PT[ev % 2]
            for i in range(8):
                P.add("pe", lambda e, pt=pt, r=r, fc=fc, i=i: e.transpose(
                    out=pt.ap[:, i * 128:(i + 1) * 128], in_=ab.ap[:, r * 8 + i, fc * 128:(fc + 1) * 128],
                    identity=ident), reads=[ab.b] + CONST, writes=[pt.b])
            if ev % 2 == 0:
                P.add("act", lambda e, pt=pt, r=r, fc=fc: e.activation(
                    out=mixT.ap[:, r * 4 + fc, :], in_=pt.ap, func=AF.Copy), reads=[pt.b],
                    writes=[mixT.bs[r * 4 + fc]])
            else:
                P.add("dve", lambda e, pt=pt, r=r, fc=fc: e.tensor_copy(out=mixT.ap[:, r * 4 + fc, :], in_=pt.ap),
                      reads=[pt.b], writes=[mixT.bs[r * 4 + fc]])
            ev += 1

    def rms_rstd(ssap, ssb, n):
        P.add("act", lambda e: e.activation(out=ssap, in_=ssap, func=AF.Sqrt, scale=1.0 / n, bias=epsT.ap),
              reads=[ssb, epsT.b], writes=[ssb])
        P.add("dve", lambda e: e.reciprocal(out=ssap, in_=ssap), reads=[ssb], writes=[ssb])

    def norm2_pre(i):
        xn = xn2[i % 2]
        ssap = ss2.ap[:, i:i + 1]
        P.add("act", lambda e: e.activation(out=xn.ap, in_=x1.ap[:, i, :], func=AF.Square, accum_out=ssap),
              reads=[x1.bs[i]], writes=[xn.b, ss2.bs[i]])
        rms_rstd(ssap, ss2.bs[i], D)
        P.add("dve", lambda e: e.tensor_scalar(out=xn.ap, in0=x1.ap[:, i, :], scalar1=ssap, scalar2=None,
                                               op0=ALU.mult), reads=[x1.bs[i], ss2.bs[i]], writes=[xn.b])

    def norm2_pe(i):
        xn = xn2[i % 2]
        for r in range(2):
            pt = PT[r]
            for k in range(8):
                P.add("pe", lambda e, pt=pt, k=k, r=r: e.transpose(
                    out=pt.ap[:, k * 128:(k + 1) * 128], in_=xn.ap[:, (r * 8 + k) * 128:(r * 8 + k + 1) * 128],
                    identity=ident), reads=[xn.b] + CONST, writes=[pt.b])
            P.add("dve", lambda e, pt=pt, r=r: e.tensor_tensor(
                out=h2T.ap[:, r * 8:(r + 1) * 8, i * 128:(i + 1) * 128],
                in0=pt.ap.rearrange("p (k n) -> p k n", k=8),
                in1=wffn[:, r * 8:(r + 1) * 8].unsqueeze(2).to_broadcast([128, 8, 128]),
                op=ALU.mult), reads=[pt.b] + CONST, writes=[h2T.bs[i]])

    pa_i = 0
    w_out_v = w_out_p.rearrange("(k p) c -> p k c", p=128)
    for dc in range(4):
        wb = wob[dc % 2]
        P.dma("pool", wb.ap, w_out_v[:, :, dc * 512:(dc + 1) * 512], writes=[wb.b], buf=wb.b)
        for i in range(8):
            pa = PA[pa_i % 2]
            pa_i += 1
            for k in range(16):
                P.add("pe", lambda e, pa=pa, k=k, i=i, wb=wb: e.matmul(
                    pa.ap, lhsT=mixT.ap[:, k, i * 128:(i + 1) * 128], rhs=wb.ap[:, k, :],
                    start=(k == 0), stop=(k == 15)), reads=[mixT.bs[k], wb.b], writes=[pa.b])
            P.add("dve", lambda e, pa=pa, i=i, dc=dc: e.tensor_tensor(
                out=x1.ap[:, i, dc * 512:(dc + 1) * 512], in0=pa.ap, in1=x1.ap[:, i, dc * 512:(dc + 1) * 512],
                op=ALU.add), reads=[pa.b, x1.bs[i]], writes=[x1.bs[i]])
            if dc == 3:
                if i >= 2:
                    norm2_pe(i - 2)
                norm2_pre(i)
    norm2_pe(6)
    norm2_pe(7)
    es2a.close()

    if dbg and stage == 3:
        d = nc.dram_tensor("dbg_x1", [128, 8 * D], F32, kind="ExternalOutput").ap()
        b = Buf("dbg_x1")
        P.dma("sp", d, x1.ap.rearrange("p t c -> p (t c)"), reads=x1.bs, writes=[b], buf=b)
        P.emit([b])
        return nc

    es2b = ExitStack()
    wr_sb = sb(es2b, "wr_sb", [128, 16, 36], BF16)
    rb_sb = sb(es2b, "rb_sb", [128, 36], F32)
    lg = sb(es2b, "lg", [128, 8, 36], F32, nbuf=8)
    rt = sb(es2b, "rt", [128, 8, 64], F32, nbuf=8)
    comb = sb(es2b, "comb", [128, 8, 32], F32, nbuf=8)
    es2w = ExitStack()
    wg = [sb(es2w, f"wg{i}", [128, 16, DE], BF16, nbuf=4) for i in range(2)]
    wu = [sb(es2w, f"wu{i}", [128, 16, DE], BF16, nbuf=4) for i in range(2)]
    wd = [sb(es2w, "wd0", [128, 4, D], BF16)] * 2
    sa = [sb(es2w, f"sa{i}", [128, 512], BF16) for i in range(2)]
    actT = [sb(es2w, f"actT{i}", [128, 4, 512], BF16, nbuf=4) for i in range(2)]
    new_bufs = lg.bs + rt.bs + comb.bs + [wr_sb.b, rb_sb.b, lg.b, rt.b]
    for tl in wg + wu + wd + sa:
        new_bufs.append(tl.b)
    for tl in wg + wu:
        new_bufs += tl.bs
    for tl in actT:
        new_bufs += tl.bs
    old = mixT.bs + [t_.b for t_ in agb + wob]
    P.add("dve", lambda e: e.memset(bar_t.ap[:, 2:3], 0.0), reads=[], writes=old + [bar_t.b])
    P.add("dve", lambda e: e.memset(bar_t.ap[:, 3:4], 0.0), reads=[bar_t.b], writes=new_bufs)

    P.dma("pool", wr_sb.ap, w_r.rearrange("(k p) c -> p k c", p=128), writes=[wr_sb.b], buf=wr_sb.b)
    P.dma("sp", rb_sb.ap, rbias, writes=[rb_sb.b], buf=rb_sb.b)

    def load_expert(e_, chunked=False):
        g_, u_ = wg[e_ % 2], wu[e_ % 2]
        gv = w_gate[e_].rearrange("(k p) f -> p k f", p=128)
        uv = w_up[e_].rearrange("(k p) f -> p k f", p=128)
        if chunked:
            for fc in range(4):
                cs = slice(fc * 128, (fc + 1) * 128)
                P.dma("pool", g_.ap[:, :, cs], gv[:, :, cs], writes=[g_.bs[fc]], buf=g_.bs[fc])
                P.dma("pool", u_.ap[:, :, cs], uv[:, :, cs], writes=[u_.bs[fc]], buf=u_.bs[fc])
        else:
            P.dma("pool", g_.ap, gv, writes=g_.bs, buf=g_.bs[0])
            P.dma("pool", u_.ap, uv, writes=u_.bs, buf=u_.bs[0])

    def load_down(e_):
        P.dma("pool", wd[0].ap, w_down[e_].rearrange("(k p) d -> p k d", p=128), writes=[wd[0].b], buf=wd[0].b)

    load_expert(0, chunked=True)
    load_down(0)
    load_expert(1)

    Lb = lg.b
    Rb = rt.b
    L3 = lg.ap
    R3 = rt.ap
    for i in range(8):
        pa = PA[2 + i % 2]
        for k in range(16):
            P.add("pe", lambda e, pa=pa, k=k, i=i: e.matmul(
                pa.ap[:, 0:36], lhsT=h2T.ap[:, k, i * 128:(i + 1) * 128], rhs=wr_sb.ap[:, k, :],
                start=(k == 0), stop=(k == 15)), reads=[h2T.bs[i], wr_sb.b], writes=[pa.b])
        P.add("dve", lambda e, pa=pa, i=i: e.tensor_tensor(out=L3[:, i, :], in0=pa.ap[:, 0:36], in1=rb_sb.ap,
                                                           op=ALU.add), reads=[pa.b, rb_sb.b], writes=[Lb])

    def bc(ap, w):
        return ap.to_broadcast([128, 8, w])

    def tt(out, in0, in1, op, rd=(), wr=None):
        P.add("dve", lambda e: e.tensor_tensor(out=out, in0=in0, in1=in1, op=op), reads=[Lb, Rb] + list(rd),
              writes=[Rb] if wr is None else wr)

    tt(R3[:, :, 8:10], L3[:, :, 0:2], L3[:, :, 2:4], ALU.max)
    tt(R3[:, :, 0:1], R3[:, :, 8:9], R3[:, :, 9:10], ALU.max)
    tt(R3[:, :, 4:8], L3[:, :, 0:4], bc(R3[:, :, 0:1], 4), ALU.is_ge)
    tt(R3[:, :, 8:12], L3[:, :, 0:4], bc(R3[:, :, 0:1], 4), ALU.subtract)
    P.add("act", lambda e: e.activation(out=R3[:, :, 8:12], in_=R3[:, :, 8:12], func=AF.Exp), reads=[Rb], writes=[Rb])
    tt(R3[:, :, 12:14], R3[:, :, 8:10], R3[:, :, 10:12], ALU.add)
    tt(R3[:, :, 1:2], R3[:, :, 12:13], R3[:, :, 13:14], ALU.add)
    tt(R3[:, :, 16:24], L3[:, :, 4:12], bc(R3[:, :, 4:5], 8), ALU.mult)
    for g_ in range(1, 4):
        tt(R3[:, :, 32:40], L3[:, :, 4 + 8 * g_:12 + 8 * g_], bc(R3[:, :, 4 + g_:5 + g_], 8), ALU.mult)
        tt(R3[:, :, 16:24], R3[:, :, 16:24], R3[:, :, 32:40], ALU.add)
    tt(R3[:, :, 24:28], R3[:, :, 16:20], R3[:, :, 20:24], ALU.max)
    tt(R3[:, :, 28:30], R3[:, :, 24:26], R3[:, :, 26:28], ALU.max)
    tt(R3[:, :, 2:3], R3[:, :, 28:29], R3[:, :, 29:30], ALU.max)
    tt(R3[:, :, 24:32], R3[:, :, 16:24], bc(R3[:, :, 2:3], 8), ALU.is_ge)
    P.add("dve", lambda e: e.scalar_tensor_tensor(out=R3[:, :, 32:40], in0=R3[:, :, 24:32], scalar=-1e30,
                                                  in1=R3[:, :, 16:24], op0=ALU.mult, op1=ALU.add),
          reads=[Rb], writes=[Rb])
    tt(R3[:, :, 40:44], R3[:, :, 32:36], R3[:, :, 36:40], ALU.max)
    tt(R3[:, :, 44:46], R3[:, :, 40:42], R3[:, :, 42:44], ALU.max)
    tt(R3[:, :, 3:4], R3[:, :, 44:45], R3[:, :, 45:46], ALU.max)
    tt(R3[:, :, 40:48], R3[:, :, 32:40], bc(R3[:, :, 3:4], 8), ALU.is_ge)
    tt(R3[:, :, 48:49], R3[:, :, 3:4], R3[:, :, 2:3], ALU.subtract)
    P.add("act", lambda e: e.activation(out=R3[:, :, 49:50], in_=R3[:, :, 48:49], func=AF.Exp), reads=[Rb], writes=[Rb])
    P.add("dve", lambda e: e.tensor_scalar(out=R3[:, :, 50:51], in0=R3[:, :, 49:50], scalar1=1.0, scalar2=None,
                                           op0=ALU.add), reads=[Rb], writes=[Rb])
    tt(R3[:, :, 50:51], R3[:, :, 50:51], R3[:, :, 1:2], ALU.mult)
    P.add("dve", lambda e: e.reciprocal(out=R3[:, :, 51:52], in_=R3[:, :, 50:51]), reads=[Rb], writes=[Rb])
    tt(R3[:, :, 52:53], R3[:, :, 51:52], R3[:, :, 49:50], ALU.mult)
    tt(R3[:, :, 56:64], R3[:, :, 24:32], bc(R3[:, :, 51:52], 8), ALU.mult)
    tt(R3[:, :, 32:40], R3[:, :, 40:48], bc(R3[:, :, 52:53], 8), ALU.mult)
    tt(R3[:, :, 56:64], R3[:, :, 56:64], R3[:, :, 32:40], ALU.add)
    for g_ in range(4):
        tt(comb.ap[:, :, 8 * g_:8 * g_ + 8], R3[:, :, 56:64], bc(R3[:, :, 4 + g_:5 + g_], 8), ALU.mult,
           wr=comb.bs)

    it = 0
    for ex in range(N_EXP):
        wgb, wub, wdb = wg[ex % 2], wu[ex % 2], wd[ex % 2]
        for tg in range(2):
            aT = actT[tg]
            for fc in range(4):
                pa_g = PA[0]
                pa_u = PA[1]
                s_ = sa[it % 2]
                it += 1
                for k in range(16):
                    P.add("pe", lambda e, k=k, fc=fc, tg=tg, wgb=wgb: e.matmul(
                        pa_g.ap, lhsT=wgb.ap[:, k, fc * 128:(fc + 1) * 128], rhs=h2T.ap[:, k, tg * 512:(tg + 1) * 512],
                        start=(k == 0), stop=(k == 15)), reads=[wgb.bs[fc]] + h2T.bs[tg * 4:tg * 4 + 4], writes=[pa_g.b])
                for k in range(16):
                    P.add("pe", lambda e, k=k, fc=fc, tg=tg, wub=wub: e.matmul(
                        pa_u.ap, lhsT=wub.ap[:, k, fc * 128:(fc + 1) * 128], rhs=h2T.ap[:, k, tg * 512:(tg + 1) * 512],
                        start=(k == 0), stop=(k == 15)), reads=[wub.bs[fc]] + h2T.bs[tg * 4:tg * 4 + 4], writes=[pa_u.b])
                P.add("act", lambda e, s_=s_: e.activation(out=s_.ap, in_=pa_g.ap, func=AF.Silu),
                      reads=[pa_g.b], writes=[s_.b])
                P.add("dve", lambda e, s_=s_, aT=aT, fc=fc: e.tensor_tensor(
                    out=aT.ap[:, fc, :], in0=pa_u.ap, in1=s_.ap, op=ALU.mult), reads=[pa_u.b, s_.b], writes=[aT.bs[fc]])
            for ii in range(4):
                i = tg * 4 + ii
                for dc in range(4):
                    pa = PA[2 + (ii * 4 + dc) % 4]
                    for fc in range(4):
                        P.add("pe", lambda e, pa=pa, fc=fc, ii=ii, dc=dc, aT=aT, wdb=wdb: e.matmul(
                            pa.ap, lhsT=aT.ap[:, fc, ii * 128:(ii + 1) * 128], rhs=wdb.ap[:, fc, dc * 512:(dc + 1) * 512],
                            start=(fc == 0), stop=(fc == 3)), reads=[aT.bs[fc], wdb.b], writes=[pa.b])
                    P.add("dve", lambda e, pa=pa, i=i, dc=dc, ex=ex: e.scalar_tensor_tensor(
                        out=x1.ap[:, i, dc * 512:(dc + 1) * 512], in0=pa.ap, scalar=comb.ap[:, i, ex:ex + 1],
                        in1=x1.ap[:, i, dc * 512:(dc + 1) * 512], op0=ALU.mult, op1=ALU.add),
                        reads=[pa.b, x1.bs[i], comb.bs[i]], writes=[x1.bs[i]])
        if ex + 1 < N_EXP:
            load_down(ex + 1)
        if ex + 2 < N_EXP:
            load_expert(ex + 2)

    oldw = [wg[0].b, wg[1].b, wu[0].b, wu[1].b, wd[0].b, sa[0].b, sa[1].b] + actT[0].bs + actT[1].bs
    oldw += wg[0].bs + wg[1].bs + wu[0].bs + wu[1].bs
    P.add("dve", lambda e: e.memset(bar_t.ap[:, 4:5], 0.0), reads=[], writes=oldw + [bar_t.b])
    es2w.close()
    es2c = ExitStack()
    wfin_t = sb(es2c, "wfin_t", [128, D], F32)
    P.add("dve", lambda e: e.memset(bar_t.ap[:, 5:6], 0.0), reads=[bar_t.b], writes=[wfin_t.b])
    P.dma("sp", wfin_t.ap, wfin, writes=[wfin_t.b], buf=wfin_t.b)
    for i in range(8):
        xn = xn2[i % 2]
        ssap = ss2.ap[:, 8 + i:9 + i]
        P.add("act", lambda e, xn=xn, i=i, ssap=ssap: e.activation(out=xn.ap, in_=x1.ap[:, i, :], func=AF.Square,
                                                                  accum_out=ssap),
              reads=[x1.bs[i]], writes=[xn.b, ss2.bs[8 + i]])
        rms_rstd(ssap, ss2.bs[8 + i], D)
        P.add("dve", lambda e, i=i, ssap=ssap: e.scalar_tensor_tensor(
            out=x1.ap[:, i, :], in0=x1.ap[:, i, :], scalar=ssap, in1=wfin_t.ap, op0=ALU.mult, op1=ALU.mult),
            reads=[x1.bs[i], ss2.bs[8 + i], wfin_t.b], writes=[x1.bs[i]])
        P.dma("sp", out[i * 128:(i + 1) * 128, :], x1.ap[:, i, :], reads=[x1.bs[i]], writes=[out_b], buf=out_b)
    P.emit([out_b])
    return nc


def _bf(a):
    return np.asarray(a, dtype=np.float32).astype(ml_dtypes.bfloat16)


def _consts(g):
    slopes_a = [2.0 ** -(i + 1) for i in range(8)]
    slopes_b = [2.0 ** (-2 * (i + 1)) for i in range(4)]
    p = np.arange(128, dtype=np.float64)[:, None]
    m = np.arange(32, dtype=np.float64)[None, :]
    kb = []
    for h in range(2):
        kb.append(slopes_a[2 * g + h] * (p + 128.0 * (m - 28)))
    kb.append(slopes_b[g] * (p + 128.0 * (m - 28) - 256.0))
    kb = np.concatenate(kb, axis=1).astype(np.float32)
    ident = np.eye(128, dtype=np.float32)
    kk = np.arange(128)[:, None]
    qq = np.arange(128)[None, :]
    tri = np.where(kk > qq, NEG, 0.0).astype(np.float32)
    selI = np.zeros((128, 4, 128), np.float32)
    selI[:, g, :] = ident
    cbf = _bf(np.concatenate([ident, tri, selI.reshape(128, 512)], axis=1))
    lrb = np.zeros((128, 2, 16, 128), np.float32)
    for h in range(2):
        for n in range(16):
            lrb[n, h, n, :] = 1.0
        lrb[16:18, h, :, :] = -slopes_a[2 * g + h]
    lrb = _bf(lrb.reshape(128, 2 * 16 * 128))
    t = np.arange(S) % 512
    rbt = _bf(np.stack([t % 256, t - t % 256]).astype(np.float32))
    pm = np.zeros((32, 16), np.float32)
    for i in range(32):
        j = i // 2
        pm[i, j] = 1e30
        pm[i, j + 1:] = -1e30
    pastm = np.broadcast_to(pm.reshape(1, 512), (128, 512)).copy()
    selE = np.zeros((32, N_EXP, 128), np.float32)
    for e in range(N_EXP):
        selE[e, e, :] = 1.0
    selE = _bf(selE.reshape(32, N_EXP * 128))
    return kb, cbf, lrb, rbt, pastm, selE


def make_in_maps(x, norm_mix_w, w_in, lambda_q1, lambda_k1, lambda_q2, lambda_k2, diff_subln_w,
                 w_out, norm_ffn_w, w_router_group, b_router_group, w_router_expert, b_router_expert,
                 w_gate, w_up, w_down, norm_final_w):
    f = lambda a: np.ascontiguousarray(np.asarray(a, dtype=np.float32))
    x = f(x); w_in = f(w_in)[0]; w_out = f(w_out)[0]
    w_gate = f(w_gate)[0]; w_up = f(w_up)[0]; w_down = f(w_down)[0]
    wmix = f(norm_mix_w)[0].reshape(16, 128).T
    wffn = f(norm_ffn_w)[0].reshape(16, 128).T
    lam = np.concatenate([f(lambda_q1)[0], f(lambda_k1)[0], f(lambda_q2)[0], f(lambda_k2)[0]])
    lamv = np.ascontiguousarray(np.broadcast_to(lam[None, :], (128, 512)))
    subw = np.ascontiguousarray(np.broadcast_to(f(diff_subln_w)[0][None, :], (128, 256)))
    rbias = np.concatenate([f(b_router_group)[0], f(b_router_expert)[0]])
    rbias = np.ascontiguousarray(np.broadcast_to(rbias[None, :], (128, 36)))
    wfin = np.ascontiguousarray(np.broadcast_to(f(norm_final_w)[None, :], (128, D)))
    w_r = np.ascontiguousarray(np.concatenate([f(w_router_group)[0], f(w_router_expert)[0]], axis=1))
    in_maps = []
    for c in range(8):
        b, g = c // 4, c % 4
        cols = []
        for h in (2 * g, 2 * g + 1):
            cols.append(np.arange(h * 128, (h + 1) * 128))
        for h in (2 * g, 2 * g + 1):
            cols.append(1024 + np.arange(h * 128, (h + 1) * 128))
        cols.append(3072 + g * 256 + np.arange(256))
        cols.append(4096 + g * 256 + np.arange(256))
        for h in (2 * g, 2 * g + 1):
            cols.append(2048 + np.arange(h * 128, (h + 1) * 128))
        cols.append(5120 + g * 256 + np.arange(256))
        cols = np.concatenate(cols)
        w_in_c = np.ascontiguousarray(w_in[:, cols])
        rows = np.concatenate([np.concatenate([np.arange(256 * r, 256 * r + 256), 1024 + np.arange(256 * r, 256 * r + 256)])
                               for r in range(4)])
        kb, cbf, lrb, rbt, pastm, selE = _consts(g)
        ri = np.arange(32)[None, :]
        idxg = (g * 4096 + (ri // 8) * 1024 + (ri % 8) * 128 + np.arange(128)[:, None]).astype(np.int32)
        vecs = np.ascontiguousarray(np.concatenate([wmix, wffn, kb], axis=1).astype(np.float32))
        in_maps.append({
            "xb": x[b], "xres": np.ascontiguousarray(x[b, g * 1024:(g + 1) * 1024]),
            "w_in_c": w_in_c, "w_out_p": np.ascontiguousarray(w_out[rows]), "w_r": w_r,
            "w_gate": w_gate, "w_up": w_up, "w_down": w_down,
            "vecs": vecs, "lamv": lamv, "subw": subw, "rbias": rbias, "wfin": wfin, "pastm": pastm,
            "cbf": cbf, "lrb": lrb, "rbt": rbt, "idxg": np.ascontiguousarray(idxg),
        })
    return in_maps


_NC = None


def kernel(**inputs):
    global _NC
    in_maps = make_in_maps(**inputs)
    if _NC is None:
        _NC = build()
    res = run_bass_kernel_spmd(_NC, in_maps, core_ids=list(range(8)))
    outp = np.empty((2, S, D), np.float32)
    for c in range(8):
        b, g = c // 4, c % 4
        outp[b, g * 1024:(g + 1) * 1024] = res.results[c]["out"]
    return outp
```
